# Optimizing a Trainium2 kernel written in Bass

```python
import jax
import jax.numpy as jnp
from jax import lax
import numpy as np

D_MODEL = 1024
BATCH = 8
SEQ = 2048
DEPTH = 2

CTX_LEN = 256
GRID_W = 64
N_EVEN = (DEPTH + 1) // 2
N_ODD = DEPTH // 2
EPS = 1e-6
CONV_K = 4
MIX_WIDTH = D_MODEL
RG_WIDTH = MIX_WIDTH // 2
RG_BLOCKS = 8
RG_BLOCK = RG_WIDTH // RG_BLOCKS
RG_C = 8.0
DN_HEADS = 4
DN_DK = MIX_WIDTH // (2 * DN_HEADS)
DN_DV = MIX_WIDTH // (2 * DN_HEADS)
DN_CHUNK = 64
HG_HEADS = 4
HG_DF = 128
HG_DI = MIX_WIDTH // (2 * HG_HEADS)
GLA_HEADS = 4
GLA_DK = 64
GLA_DV = MIX_WIDTH // (2 * GLA_HEADS)
GLA_RANK = 16
GLA_GATE_NORM = 16.0
GLA_CHUNK = 32
D_FF = 2816
N_EXPERTS = 8
TOP_K = 2
D_EXPERT = 2816

E_SIZES = (RG_WIDTH, RG_WIDTH, DN_HEADS * (2 * DN_DK + DN_DV), DN_HEADS * DN_DV, 2 * DN_HEADS, 2 * DN_HEADS)
E_IN = sum(E_SIZES)
O_SIZES = (HG_HEADS * HG_DF, HG_HEADS * HG_DF, HG_HEADS * HG_DF, HG_HEADS * HG_DI, HG_HEADS * HG_DI,
           GLA_HEADS * GLA_DK, GLA_HEADS * GLA_DK, GLA_HEADS * GLA_DV, GLA_HEADS * GLA_DV, 2 * GLA_RANK)
O_IN = sum(O_SIZES)

kernel_name = 'bidir_hybrid_rglru_deltanet_hgrn2_gla_moe'


def rmsnorm(x, g):
    xf = x.astype(jnp.float32)
    y = xf * lax.rsqrt(jnp.mean(jnp.square(xf), axis=-1, keepdims=True) + EPS)
    return (y * g.astype(jnp.float32)).astype(x.dtype)


def modulate(h, g, shift, scale):
    return rmsnorm(h, g) * (1.0 + scale) + shift


def split_cols(u, sizes):
    out, start = [], 0
    for s in sizes:
        out.append(u[..., start:start + s])
        start += s
    return out


def to_heads(t, n):
    b, l, _ = t.shape
    return t.reshape(b, l, n, -1).transpose(0, 2, 1, 3)


def from_heads(t):
    b, n, l, d = t.shape
    return t.transpose(0, 2, 1, 3).reshape(b, l, n * d)


def l2norm(t):
    return t * lax.rsqrt(jnp.sum(t * t, axis=-1, keepdims=True) + EPS)


def gated_rmsnorm(o, z, g):
    o = o * lax.rsqrt(jnp.mean(o * o, axis=-1, keepdims=True) + EPS) * g
    return o * jax.nn.silu(z)


def flip_seq(t, d, axis):
    return jnp.flip(t, axis=axis) if d == 1 else t


def dwconv_centred(x, w, b=None):
    k, ch = w.shape
    y = lax.conv_general_dilated(x, w[:, None, :].astype(x.dtype), (1,), [(k // 2, k - 1 - k // 2)],
                                 dimension_numbers=('NWC', 'WIO', 'NWC'), feature_group_count=ch)
    return y if b is None else y + b


def rglru_scan(u, gate_w, gate_b, lam, h0):
    b, l, w = u.shape
    gates = jnp.einsum('blhi,ghij->gblhj', u.reshape(b, l, RG_BLOCKS, RG_BLOCK), gate_w).reshape(2, b, l, w)
    gates = gates + gate_b[:, None, None, :]
    r, i = jax.nn.sigmoid(gates[0]), jax.nn.sigmoid(gates[1])
    log_a = -RG_C * r * jax.nn.softplus(-lam)
    a = jnp.exp(log_a)
    xin = jnp.sqrt(-jnp.expm1(2.0 * log_a)) * (i * u)
    xin = xin.at[:, 0].add(a[:, 0] * h0)

    def comb(left, right):
        return left[0] * right[0], right[0] * left[1] + right[1]

    return lax.associative_scan(comb, (a, xin), axis=1)[1]


def rglru_bidir(u_lat, u_ctx, gate_w, gate_b, lam):
    h0 = jnp.zeros((u_ctx.shape[0], u_ctx.shape[2]), jnp.float32)
    y_lat, y_ctx = 0.0, 0.0
    for d in range(2):
        hc = rglru_scan(flip_seq(u_ctx, d, 1), gate_w[d], gate_b[d], lam[d], h0)
        hl = rglru_scan(flip_seq(u_lat, d, 1), gate_w[d], gate_b[d], lam[d], hc[:, -1])
        y_ctx = y_ctx + flip_seq(hc, d, 1)
        y_lat = y_lat + flip_seq(hl, d, 1)
    return y_lat, y_ctx


def gated_delta_chunk(q, k, v, g, beta, s0):
    b, h, l, dk = k.shape
    dv = v.shape[-1]
    n = l // DN_CHUNK
    rs = lambda t: t.reshape(b, h, n, DN_CHUNK, *t.shape[3:])
    q, k, v, g, beta = rs(q), rs(k), rs(v), rs(g), rs(beta)
    gc = jnp.cumsum(g, axis=-1)
    incl = jnp.tril(jnp.ones((DN_CHUNK, DN_CHUNK), bool))
    strict = jnp.tril(jnp.ones((DN_CHUNK, DN_CHUNK), bool), -1)
    decay = jnp.exp(jnp.where(incl, gc[..., :, None] - gc[..., None, :], -jnp.inf))
    kb = k * beta[..., None]
    lower = jnp.where(strict, jnp.einsum('bhnid,bhnjd->bhnij', kb, k) * decay, 0.0)
    eye = jnp.eye(DN_CHUNK, dtype=k.dtype)
    rhs = jnp.concatenate([v * beta[..., None], kb * jnp.exp(gc)[..., None]], axis=-1)
    sol = lax.linalg.triangular_solve(eye + lower, rhs, left_side=True, lower=True, unit_diagonal=True)
    u, w = sol[..., :dv], sol[..., dv:]
    a_qk = jnp.einsum('bhnid,bhnjd->bhnij', q, k) * decay
    q_dec = q * jnp.exp(gc)[..., None]
    k_tail = k * jnp.exp(gc[..., -1:] - gc)[..., None]
    g_tot = jnp.exp(gc[..., -1])

    def step(s, xs):
        u_n, w_n, q_n, a_n, k_n, gt_n = xs
        v_new = u_n - jnp.einsum('bhck,bhkv->bhcv', w_n, s)
        o_n = jnp.einsum('bhck,bhkv->bhcv', q_n, s) + jnp.einsum('bhcj,bhjv->bhcv', a_n, v_new)
        s = s * gt_n[..., None, None] + jnp.einsum('bhck,bhcv->bhkv', k_n, v_new)
        return s, o_n

    xs = tuple(jnp.moveaxis(t, 2, 0) for t in (u, w, q_dec, a_qk, k_tail, g_tot))
    s_fin, o = lax.scan(step, s0, xs)
    return jnp.moveaxis(o, 0, 2).reshape(b, h, l, dv), s_fin


def gla_chunk(q, k, v, logd, s0):
    b, h, l, dk = k.shape
    dv = v.shape[-1]
    n = l // GLA_CHUNK
    rs = lambda t: t.reshape(b, h, n, GLA_CHUNK, t.shape[-1])
    q, k, v, logd = rs(q), rs(k), rs(v), rs(logd)
    bc = jnp.cumsum(logd, axis=-2)
    q_dec = q * jnp.exp(bc)
    incl = jnp.tril(jnp.ones((GLA_CHUNK, GLA_CHUNK), bool))
    a = jnp.where(incl, jnp.einsum('bhnid,bhnjd->bhnij', q_dec, k * jnp.exp(-bc)), 0.0)
    o_intra = jnp.einsum('bhnij,bhnjv->bhniv', a, v)
    k_tail = k * jnp.exp(bc[..., -1:, :] - bc)
    d_tot = jnp.exp(bc[..., -1, :])

    def step(s, xs):
        q_n, k_n, v_n, d_n = xs
        o_n = jnp.einsum('bhck,bhkv->bhcv', q_n, s)
        s = s * d_n[..., None] + jnp.einsum('bhck,bhcv->bhkv', k_n, v_n)
        return s, o_n

    xs = tuple(jnp.moveaxis(t, 2, 0) for t in (q_dec, k_tail, v, d_tot))
    s_fin, o_inter = lax.scan(step, s0, xs)
    o = o_intra + jnp.moveaxis(o_inter, 0, 2)
    return o.reshape(b, h, l, dv), s_fin


def gla_final_state(k, v, logd):
    bc = jnp.cumsum(logd, axis=2)
    return jnp.einsum('bhlk,bhlv->bhkv', k * jnp.exp(bc[:, :, -1:] - bc), v)


def bidir_scan(chunk_fn, state_fn, lat_in, ctx_in, s0, ctx_out):
    y_lat, y_ctx = 0.0, 0.0
    for d in range(2):
        c_args = [flip_seq(t, d, 2) for t in ctx_in[d]]
        if ctx_out:
            o_c, s_c = chunk_fn(*c_args, s0)
            y_ctx = y_ctx + flip_seq(o_c, d, 2)
        else:
            s_c = state_fn(*c_args)
        o_l, _ = chunk_fn(*[flip_seq(t, d, 2) for t in lat_in[d]], s_c)
        y_lat = y_lat + flip_seq(o_l, d, 2)
    return y_lat, (y_ctx if ctx_out else None)


def even_mixer(xl, xc, w_in, w_out, a_conv_w, a_conv_b, a_gate_w, a_gate_b, a_lambda,
               b_conv_w, b_a_log, b_dt_bias, b_norm_g, ctx_out):
    f32 = jnp.float32

    def prep(xs):
        bsz, l, _ = xs.shape
        ax, ay, qkv, z, bb, ba = split_cols(xs @ w_in, E_SIZES)
        ua = dwconv_centred(ax, a_conv_w, a_conv_b).astype(f32)
        qkv = jax.nn.silu(dwconv_centred(qkv, b_conv_w).astype(f32))
        q, k, v = split_cols(qkv, (DN_HEADS * DN_DK, DN_HEADS * DN_DK, DN_HEADS * DN_DV))
        q = l2norm(to_heads(q, DN_HEADS)) * DN_DK ** -0.5
        k = l2norm(to_heads(k, DN_HEADS))
        v = to_heads(v, DN_HEADS)
        beta = jax.nn.sigmoid(bb.astype(f32)).reshape(bsz, l, 2, DN_HEADS).transpose(2, 0, 3, 1)
        g = -jnp.exp(b_a_log.astype(f32)) * jax.nn.softplus(ba.astype(f32).reshape(bsz, l, 2, DN_HEADS) + b_dt_bias)
        g = g.transpose(2, 0, 3, 1)
        return ua, ay, [(q, k, v, g[d], beta[d]) for d in range(2)], z

    ua_l, ay_l, dn_l, z_l = prep(xl)
    ua_c, ay_c, dn_c, z_c = prep(xc)
    ha_l, ha_c = rglru_bidir(ua_l, ua_c, a_gate_w, a_gate_b, a_lambda)
    s0 = jnp.zeros((xc.shape[0], DN_HEADS, DN_DK, DN_DV), f32)
    ob_l, ob_c = bidir_scan(gated_delta_chunk, lambda *t: gated_delta_chunk(*t, s0)[1], dn_l, dn_c, s0, ctx_out)

    def merge(ha, ay, ob, z, dtype):
        ya = ha * jax.nn.gelu(ay.astype(f32))
        yb = from_heads(gated_rmsnorm(ob, to_heads(z.astype(f32), DN_HEADS), b_norm_g))
        return jnp.concatenate([ya, yb], axis=-1).astype(dtype) @ w_out

    y_l = merge(ha_l, ay_l, ob_l, z_l, xl.dtype)
    y_c = merge(ha_c, ay_c, ob_c, z_c, xc.dtype) if ctx_out else None
    return y_l, y_c


def odd_mixer(xl, xc, w_in, w_out, lb, c_norm_g, d_gate_w2, d_gate_b2, d_norm_g, ctx_out):
    f32 = jnp.float32
    lb = lb.astype(f32)

    def prep(xs):
        bsz, l, _ = xs.shape
        cq, cff, cfb, ci, cg, dq, dk, dv, dg, dlr = split_cols((xs @ w_in).astype(f32), O_SIZES)
        q_c = to_heads(jax.nn.silu(cq), HG_HEADS) * HG_DF ** -0.5
        v_c = to_heads(ci, HG_HEADS)
        hg_in = []
        for fl in (cff, cfb):
            log_f = jnp.log(lb + (1.0 - lb) * jax.nn.sigmoid(fl))
            k_c = (1.0 - lb) * jax.nn.sigmoid(-fl)
            hg_in.append((q_c, to_heads(k_c, HG_HEADS), v_c, to_heads(log_f, HG_HEADS)))
        q_d = to_heads(dq, GLA_HEADS) * GLA_DK ** -0.5
        k_d = to_heads(dk, GLA_HEADS)
        v_d = to_heads(dv, GLA_HEADS)
        lr = dlr.reshape(bsz, l, 2, GLA_RANK)
        logd = jax.nn.log_sigmoid(jnp.einsum('blgr,grk->gblk', lr, d_gate_w2) + d_gate_b2[:, None, None, :]) / GLA_GATE_NORM
        gla_in = [(q_d, k_d, v_d, to_heads(logd[d], GLA_HEADS)) for d in range(2)]
        return hg_in, gla_in, cg, dg

    hg_l, gl_l, cg_l, dg_l = prep(xl)
    hg_c, gl_c, cg_c, dg_c = prep(xc)
    state_fn = lambda q, k, v, logd: gla_final_state(k, v, logd)
    s0_c = jnp.zeros((xc.shape[0], HG_HEADS, HG_DF, HG_DI), f32)
    s0_d = jnp.zeros((xc.shape[0], GLA_HEADS, GLA_DK, GLA_DV), f32)
    yc_l, yc_c = bidir_scan(gla_chunk, state_fn, hg_l, hg_c, s0_c, ctx_out)
    yd_l, yd_c = bidir_scan(gla_chunk, state_fn, gl_l, gl_c, s0_d, ctx_out)

    def merge(yc, yd, cg, dg, dtype):
        oc = from_heads(gated_rmsnorm(yc, to_heads(cg, HG_HEADS), c_norm_g))
        od = from_heads(gated_rmsnorm(yd, to_heads(dg, GLA_HEADS), d_norm_g))
        return jnp.concatenate([oc, od], axis=-1).astype(dtype) @ w_out

    y_l = merge(yc_l, yd_l, cg_l, dg_l, xl.dtype)
    y_c = merge(yc_c, yd_c, cg_c, dg_c, xc.dtype) if ctx_out else None
    return y_l, y_c


def swiglu(xs, w_gate, w_up, w_down):
    return (jax.nn.silu(xs @ w_gate) * (xs @ w_up)) @ w_down


def moe_swiglu(xs, router_w, router_b, w_gate, w_up, w_down):
    logits = (xs @ router_w).astype(jnp.float32) + router_b.astype(jnp.float32)
    top_val, top_idx = lax.top_k(logits, TOP_K)
    top_p = jax.nn.softmax(top_val, axis=-1)
    gates = jnp.sum(jax.nn.one_hot(top_idx, N_EXPERTS, dtype=jnp.float32) * top_p[..., None], axis=-2).astype(xs.dtype)
    y = jnp.zeros_like(xs)
    for e in range(N_EXPERTS):
        y = y + gates[..., e:e + 1] * swiglu(xs, w_gate[e], w_up[e], w_down[e])
    return y


def setup_inputs(seed: int = 0) -> dict:
    key = jax.random.key(seed)
    keys = list(jax.random.split(key, 48))

    def nrm(shape, std=1.0):
        return std * jax.random.normal(keys.pop(), shape, jnp.float32)

    def unif(shape, lo, hi):
        return jax.random.uniform(keys.pop(), shape, jnp.float32, lo, hi)

    d = D_MODEL
    a_pow = unif((N_EVEN, 2, RG_WIDTH), 0.9, 0.999) ** (1.0 / RG_C)
    dt = jnp.exp(unif((N_EVEN, 2, DN_HEADS), float(np.log(1e-3)), float(np.log(1e-1))))
    a_log = jnp.log(unif((N_EVEN, 2, DN_HEADS), 1.0, 16.0))
    return {
        'x': nrm((BATCH, SEQ, d)),
        'c': nrm((BATCH, d)),
        'ctx': nrm((BATCH, CTX_LEN, d)),
        'c_ctx': nrm((d,)),
        'ada_w': nrm((DEPTH, d, 6 * d), 0.5 * d ** -0.5),
        'ada_b': nrm((DEPTH, 6 * d), 0.02),
        'norm_mix_g': 1.0 + nrm((DEPTH, d), 0.05),
        'norm_ffn_g': 1.0 + nrm((DEPTH, d), 0.05),
        'final_norm_g': 1.0 + nrm((d,), 0.05),
        'e_w_in': nrm((N_EVEN, d, E_IN), d ** -0.5),
        'e_w_out': nrm((N_EVEN, MIX_WIDTH, d), MIX_WIDTH ** -0.5),
        'e_a_conv_w': nrm((N_EVEN, CONV_K, RG_WIDTH), CONV_K ** -0.5),
        'e_a_conv_b': nrm((N_EVEN, RG_WIDTH), 0.02),
        'e_a_gate_w': nrm((N_EVEN, 2, 2, RG_BLOCKS, RG_BLOCK, RG_BLOCK), RG_BLOCK ** -0.5),
        'e_a_gate_b': nrm((N_EVEN, 2, 2, RG_WIDTH), 0.02),
        'e_a_lambda': jnp.log(a_pow) - jnp.log1p(-a_pow),
        'e_b_conv_w': nrm((N_EVEN, CONV_K, DN_HEADS * (2 * DN_DK + DN_DV)), CONV_K ** -0.5),
        'e_b_a_log': a_log,
        'e_b_dt_bias': dt + jnp.log(-jnp.expm1(-dt)),
        'e_b_norm_g': 1.0 + nrm((N_EVEN, DN_DV), 0.05),
        'e_ffn_w_gate': nrm((N_EVEN, d, D_FF), d ** -0.5),
        'e_ffn_w_up': nrm((N_EVEN, d, D_FF), d ** -0.5),
        'e_ffn_w_down': nrm((N_EVEN, D_FF, d), D_FF ** -0.5),
        'o_w_in': nrm((N_ODD, d, O_IN), d ** -0.5),
        'o_w_out': nrm((N_ODD, MIX_WIDTH, d), MIX_WIDTH ** -0.5),
        'o_lb_logits': nrm((DEPTH, HG_HEADS * HG_DF), 0.1),
        'o_c_norm_g': 1.0 + nrm((N_ODD, HG_DI), 0.05),
        'o_d_gate_w2': nrm((N_ODD, 2, GLA_RANK, GLA_HEADS * GLA_DK), GLA_RANK ** -0.5),
        'o_d_gate_b2': nrm((N_ODD, 2, GLA_HEADS * GLA_DK), 0.02),
        'o_d_norm_g': 1.0 + nrm((N_ODD, GLA_DV), 0.05),
        'o_router_w': nrm((N_ODD, d, N_EXPERTS), d ** -0.5),
        'o_router_b': nrm((N_ODD, N_EXPERTS), 0.01),
        'o_moe_w_gate': nrm((N_ODD, N_EXPERTS, d, D_EXPERT), d ** -0.5),
        'o_moe_w_up': nrm((N_ODD, N_EXPERTS, d, D_EXPERT), d ** -0.5),
        'o_moe_w_down': nrm((N_ODD, N_EXPERTS, D_EXPERT, d), D_EXPERT ** -0.5),
    }


def reference(x, c, ctx, c_ctx, ada_w, ada_b, norm_mix_g, norm_ffn_g, final_norm_g,
              e_w_in, e_w_out, e_a_conv_w, e_a_conv_b, e_a_gate_w, e_a_gate_b, e_a_lambda,
              e_b_conv_w, e_b_a_log, e_b_dt_bias, e_b_norm_g, e_ffn_w_gate, e_ffn_w_up, e_ffn_w_down,
              o_w_in, o_w_out, o_lb_logits, o_c_norm_g, o_d_gate_w2, o_d_gate_b2, o_d_norm_g,
              o_router_w, o_router_b, o_moe_w_gate, o_moe_w_up, o_moe_w_down):
    bsz, seq, dm = x.shape
    rows = seq // GRID_W
    lb_w = jax.nn.softmax(o_lb_logits.astype(jnp.float32), axis=0)
    lower_bounds = jnp.cumsum(lb_w, axis=0) - lb_w[0]
    s_lat = jax.nn.silu(c)
    s_ctx = jax.nn.silu(c_ctx)
    h_lat, h_ctx = x, ctx
    for l in range(DEPTH):
        j = l // 2
        ctx_out = l < DEPTH - 1
        ml = jnp.split((s_lat @ ada_w[l] + ada_b[l])[:, None, :], 6, axis=-1)
        mc = jnp.split(s_ctx @ ada_w[l] + ada_b[l], 6, axis=-1)
        xl = modulate(h_lat, norm_mix_g[l], ml[0], ml[1])
        xc = modulate(h_ctx, norm_mix_g[l], mc[0], mc[1])
        if l % 2 == 0:
            yl, yc = even_mixer(xl, xc, e_w_in[j], e_w_out[j], e_a_conv_w[j], e_a_conv_b[j], e_a_gate_w[j],
                                e_a_gate_b[j], e_a_lambda[j], e_b_conv_w[j], e_b_a_log[j], e_b_dt_bias[j],
                                e_b_norm_g[j], ctx_out)
        else:
            xl_cm = xl.reshape(bsz, rows, GRID_W, dm).swapaxes(1, 2).reshape(bsz, seq, dm)
            yl_cm, yc = odd_mixer(xl_cm, xc, o_w_in[j], o_w_out[j], lower_bounds[l], o_c_norm_g[j],
                                  o_d_gate_w2[j], o_d_gate_b2[j], o_d_norm_g[j], ctx_out)
            yl = yl_cm.reshape(bsz, GRID_W, rows, dm).swapaxes(1, 2).reshape(bsz, seq, dm)
        h_lat = h_lat + ml[2] * yl

        def channel_mixer(xs):
            if l % 2 == 0:
                return swiglu(xs, e_ffn_w_gate[j], e_ffn_w_up[j], e_ffn_w_down[j])
            return moe_swiglu(xs, o_router_w[j], o_router_b[j], o_moe_w_gate[j], o_moe_w_up[j], o_moe_w_down[j])

        h_lat = h_lat + ml[5] * channel_mixer(modulate(h_lat, norm_ffn_g[l], ml[3], ml[4]))
        if ctx_out:
            h_ctx = h_ctx + mc[2] * yc
            h_ctx = h_ctx + mc[5] * channel_mixer(modulate(h_ctx, norm_ffn_g[l], mc[3], mc[4]))
    return rmsnorm(h_lat, final_norm_g)
```

```python
import numpy as np
from contextlib import ExitStack
import concourse.bass as bass
import concourse.mybir as mybir
from concourse.bass_utils import run_bass_kernel_spmd

F32 = mybir.dt.float32
F32R = mybir.dt.float32r
BF16 = mybir.dt.bfloat16
AF = mybir.ActivationFunctionType
ALU = mybir.AluOpType

D = 1024
KC = 8
NCTX = 256
NLAT = 2048
TT = NCTX + NLAT
EPS = 1e-6
E_IN = 3088
O_IN = 4128
DFF = 2816
FC = DFF // 128
NEXP = 8
BLKS = [(0, 256), (256, 768), (768, 1280), (1280, 1792), (1792, 2304)]


class V:
    __slots__ = ("ap", "dep")

    def __init__(self, ap, dep):
        self.ap = ap
        self.dep = dep


class _Idx:
    def __init__(self, t, key):
        self.t = t
        self.key = key

    def __getitem__(self, sl):
        return V(self.t.h[sl], (self.t.name, self.key))


class TT_:
    def __init__(self, h, name):
        self.h = h
        self.name = name

    def __getitem__(self, sl):
        return V(self.h[sl], (self.name, None))

    def __call__(self, key):
        return _Idx(self, key)


def _ap(x):
    return x.ap if isinstance(x, V) else x


def r32v(v):
    return V(v.ap.bitcast(F32R), v.dep) if R32 else v


class Sched:
    ENG = ("pe", "dve", "act", "pool", "sp")
    NDS = 24

    def __init__(self, nc, es):
        self.nc = nc
        self.es = es
        self.eng = dict(pe=nc.tensor, dve=nc.vector, act=nc.scalar, pool=nc.gpsimd, sp=nc.sync)
        self.sem = {e: es.enter_context(nc.semaphore("s_" + e)) for e in self.ENG}
        self.cnt = {e: 0 for e in self.ENG}
        self.seen = {e: {} for e in self.ENG}
        self.dsem = [es.enter_context(nc.semaphore("d%d" % i)) for i in range(self.NDS)]
        self.dval = [0] * self.NDS
        self.dnext = 0
        self.state = {}
        self.uid = 0
        self.out_tokens = []
        self.excl = set()

    def sb(self, es, name, shape, dt):
        self.uid += 1
        nm = "%s_%d" % (name, self.uid)
        return TT_(es.enter_context(self.nc.sbuf_tensor(nm, list(shape), dt)), nm)

    def ps(self, es, name, shape, dt=F32):
        self.uid += 1
        nm = "%s_%d" % (name, self.uid)
        self.excl.add(nm)
        return TT_(es.enter_context(self.nc.psum_tensor(nm, list(shape), dt)), nm)

    def dram(self, name, shape, dt, kind="Internal"):
        t = self.nc.dram_tensor(name, list(shape), dt, kind=kind)
        return TT_(t.ap(), name)

    def _entries(self, dep):
        tid, key = dep
        st = self.state.setdefault(tid, {})
        if None not in st:
            st[None] = [None, {}]
        if key is None:
            return list(st.values())
        if key not in st:
            st[key] = [None, {}]
        return [st[key], st[None]]

    def _wait(self, e, tok):
        if tok is None:
            return
        k, v = tok
        if k == e and e in ("pe", "sp"):
            return
        if self.seen[e].get(k, 0) >= v:
            return
        sem = self.sem[k] if isinstance(k, str) else self.dsem[k[1]]
        self.eng[e].wait_ge(sem, v)
        self.seen[e][k] = v

    def _pre(self, e, R, W):
        for v in R:
            if isinstance(v, V):
                for ent in self._entries(v.dep):
                    self._wait(e, ent[0])
        for v in W:
            if isinstance(v, V):
                for ent in self._entries(v.dep):
                    self._wait(e, ent[0])
                    for k, val in ent[1].items():
                        self._wait(e, (k, val))

    def _post(self, tok, R, W):
        k, val = tok
        for v in R:
            if isinstance(v, V):
                tid, key = v.dep
                ents = self._entries(v.dep)
                tgt = ents if key is None else ents[:1]
                for ent in tgt:
                    if ent[1].get(k, 0) < val:
                        ent[1][k] = val
        for v in W:
            if isinstance(v, V):
                tid, key = v.dep
                ents = self._entries(v.dep)
                tgt = ents if key is None else ents[:1]
                for ent in tgt:
                    ent[0] = tok
                    ent[1] = {}

    dead = False
    last_pool_dma = None

    def op(self, e, fn, R, W):
        if self.dead:
            return
        Rx = [v for v in R if isinstance(v, V) and v.dep[0] in self.excl]
        if Rx:
            R = [v for v in R if not (isinstance(v, V) and v.dep[0] in self.excl)]
            W = list(W) + Rx
        self._pre(e, R, W)
        ins = fn()
        self.cnt[e] += 1
        ins.then_inc(self.sem[e], 1)
        self._post((e, self.cnt[e]), R, W)

    def dma(self, q, out, in_, **kw):
        if self.dead:
            return None
        i = self.dnext
        self.dnext = (i + 1) % self.NDS
        if self.dval[i] > 0:
            self._wait(q, (("d", i), self.dval[i]))
        if q == "pool" and self.last_pool_dma is not None:
            self._wait(q, self.last_pool_dma)
        self._pre(q, [in_], [out])
        ins = self.eng[q].dma_start(out=_ap(out), in_=_ap(in_), **kw)
        self.dval[i] += 16
        ins.then_inc(self.dsem[i], 16)
        tok = (("d", i), self.dval[i])
        if q == "pool":
            self.last_pool_dma = tok
        self._post(tok, [in_], [out])
        return tok

    def scope(self):
        sched = self

        class _Scope(ExitStack):
            def __exit__(self, *a):
                sched.barrier()
                return super().__exit__(*a)

        return _Scope()

    def barrier(self):
        for e in self.ENG:
            for f in self.ENG:
                if f != e and self.cnt[f] > 0:
                    self._wait(e, (f, self.cnt[f]))
            for i in range(self.NDS):
                if self.dval[i] > 0:
                    self._wait(e, (("d", i), self.dval[i]))

    def mm(self, out, lhsT, rhs, start=True, stop=True, r32=False):
        if r32:
            self.op("pe", lambda: self.nc.tensor.matmul(_ap(out), lhsT=_ap(lhsT).bitcast(F32R), rhs=_ap(rhs).bitcast(F32R), start=start, stop=stop),
                    [lhsT, rhs], [out])
            return
        self.op("pe", lambda: self.nc.tensor.matmul(_ap(out), lhsT=_ap(lhsT), rhs=_ap(rhs), start=start, stop=stop),
                [lhsT, rhs], [out])

    def tr(self, out, in_, ident):
        self.op("pe", lambda: self.nc.tensor.transpose(_ap(out), _ap(in_), _ap(ident)), [in_, ident], [out])

    def act(self, out, in_, func, bias=None, scale=None, accum=None):
        kw = {}
        R = [in_]
        if bias is not None:
            kw["bias"] = _ap(bias)
            R.append(bias)
        if scale is not None:
            kw["scale"] = _ap(scale)
            R.append(scale)
        W = [out]
        if accum is not None:
            kw["accum_out"] = _ap(accum)
            W.append(accum)
        self.op("act", lambda: self.nc.scalar.activation(out=_ap(out), in_=_ap(in_), func=func, **kw), R, W)

    def tt(self, e, out, in0, in1, op):
        self.op(e, lambda: self.eng[e].tensor_tensor(out=_ap(out), in0=_ap(in0), in1=_ap(in1), op=op), [in0, in1], [out])

    def ts(self, e, out, in0, s1, s2=None, op0=ALU.mult, op1=None, accum=None):
        kw = {}
        if op1 is not None:
            kw["op1"] = op1
        W = [out]
        if accum is not None:
            kw["accum_out"] = _ap(accum)
            W.append(accum)
        self.op(e, lambda: self.eng[e].tensor_scalar(out=_ap(out), in0=_ap(in0), scalar1=_ap(s1), scalar2=_ap(s2), op0=op0, **kw),
                [in0, s1, s2], W)

    def stt(self, e, out, in0, scalar, in1, op0, op1):
        self.op(e, lambda: self.eng[e].scalar_tensor_tensor(out=_ap(out), in0=_ap(in0), scalar=_ap(scalar), in1=_ap(in1), op0=op0, op1=op1),
                [in0, scalar, in1], [out])

    def cp(self, e, out, in_):
        if e == "act":
            self.op(e, lambda: self.nc.scalar.copy(out=_ap(out), in_=_ap(in_)), [in_], [out])
        else:
            self.op(e, lambda: self.eng[e].tensor_copy(out=_ap(out), in_=_ap(in_)), [in_], [out])

    def scan(self, out, d0, d1, init, op0=ALU.mult, op1=ALU.add):
        self.op("dve", lambda: self.nc.vector.tensor_tensor_scan(out=_ap(out), data0=_ap(d0), data1=_ap(d1), initial=_ap(init), op0=op0, op1=op1),
                [d0, d1, init], [out])

    def memset(self, e, out, val):
        self.op(e, lambda: self.eng[e].memset(_ap(out), val), [], [out])

    def recip(self, out, in_):
        self.op("dve", lambda: self.nc.vector.reciprocal(out=_ap(out), in_=_ap(in_)), [in_], [out])

    def iota_like_select(self, out, in_, pattern, cmp, fill, base, cm):
        self.op("pool", lambda: self.nc.gpsimd.affine_select(out=_ap(out), in_=_ap(in_), pattern=pattern, compare_op=cmp, fill=fill,
                                                              base=base, channel_multiplier=cm), [in_], [out])


def _consts():
    j = np.arange(128)[:, None]
    i = np.arange(128)[None, :]
    c = np.zeros((128, 8, 128), np.float32)
    c[:, 0] = (j == i)
    c[:, 1] = 1.0
    c[:, 2] = (j <= i)
    c[:, 3] = (j < i)
    c[:, 4] = (j >= i)
    c[:, 5] = (j > i)
    same = (j // 64) == (i // 64)
    c[:, 6] = same & (j <= i)
    c[:, 7] = same & (j >= i)
    return c


PROWS = {}


def _pack_rows(inp, b):
    parts = []
    off = [0]

    def add(name, arr):
        a = np.ascontiguousarray(arr, dtype=np.float32).reshape(-1, 128)
        PROWS[name] = (off[0], a.shape[0])
        off[0] += a.shape[0]
        parts.append(a)

    add("c", inp["c"][b])
    add("c_ctx", inp["c_ctx"])
    add("ada_b0", inp["ada_b"][0])
    add("ada_b1", inp["ada_b"][1])
    add("nmix0", inp["norm_mix_g"][0])
    add("nmix1", inp["norm_mix_g"][1])
    add("nffn0", inp["norm_ffn_g"][0])
    add("nffn1", inp["norm_ffn_g"][1])
    add("nfin", inp["final_norm_g"])
    add("a_conv_w", inp["e_a_conv_w"][0])
    add("a_conv_b", inp["e_a_conv_b"][0])
    add("a_gate_b", inp["e_a_gate_b"][0])
    add("a_lambda", inp["e_a_lambda"][0])
    add("b_conv_w", inp["e_b_conv_w"][0])
    add("b_norm_g", inp["e_b_norm_g"][0])
    add("lb_logits", inp["o_lb_logits"])
    add("c_norm_g", inp["o_c_norm_g"][0])
    add("d_gate_b2", inp["o_d_gate_b2"][0])
    add("d_norm_g", inp["o_d_norm_g"][0])
    rows = np.concatenate(parts, 0)
    pad = (-rows.shape[0]) % 128
    if pad:
        rows = np.concatenate([rows, np.zeros((pad, 128), np.float32)], 0)
    return rows


NPROW = 384
R32 = False
DBL_DT = F32
PROWS_STATIC = {'c': (0, 8), 'c_ctx': (8, 8), 'ada_b0': (16, 48), 'ada_b1': (64, 48), 'nmix0': (112, 8), 'nmix1': (120, 8),
                'nffn0': (128, 8), 'nffn1': (136, 8), 'nfin': (144, 8), 'a_conv_w': (152, 16), 'a_conv_b': (168, 4),
                'a_gate_b': (172, 16), 'a_lambda': (188, 8), 'b_conv_w': (196, 48), 'b_norm_g': (244, 1), 'lb_logits': (245, 8),
                'c_norm_g': (253, 1), 'd_gate_b2': (254, 4), 'd_norm_g': (258, 1)}


TAP_SHAPES = {"wgtap": [128, KC, 512], "h0": [128, KC, TT], "mod0": [128, 48, 2], "mod1": [128, 48, 2], "xn0": [128, KC, TT], "rsb": [128, 512],
              "gs": [128, KC, 2], "ha0": [128, TT], "ha3": [128, TT], "ob0": [128, TT], "ob3": [128, TT], "dnrows": [128, TT],
              "dngtb": [128, 8, 18], "dnG": [64, TT], "hmid0": [128, KC, TT], "h1": [128, KC, TT], "xn1": [128, KC, TT],
              "hmid1": [128, KC, TT], "oc0": [128, NLAT], "od0": [128, NLAT], "h2": [128, KC, TT], "gates": [128, 16, 8]}


class _Stop(Exception):
    pass


class Prog:
    cut = None

    ckn = {}

    def ck(self, name):
        k = self.ckn.get(name, 0)
        self.ckn[name] = k + 1
        if self.cut == "%s#%d" % (name, k):
            self.S.dead = True

    def __init__(self, stages, taps=()):
        self.stages = stages
        self.taps = set(taps)
        self.nc = bass.Bass("TRN2", target_bir_lowering=False)
        self.inputs = {}
        self.tap_out = {}

    def din(self, name, shape, dt=F32):
        if name not in self.inputs:
            self.inputs[name] = self.nc.dram_tensor(name, list(shape), dt, kind="ExternalInput").ap()
        return self.inputs[name]

    def tap(self, name, src, shape):
        if name not in self.taps:
            return
        S = self.S
        t = self.tap_out[name]
        q = "sp" if _ap(src).tensor.dtype == F32 else "pool"
        tok = S.dma(q, t, src)
        S.out_tokens.append(tok)

    def build(self):
        nc = self.nc
        with ExitStack() as es:
            S = self.S = Sched(nc, es)
            self.es = es
            for name in sorted(self.taps):
                self.tap_out[name] = nc.dram_tensor("tap_" + name, list(TAP_SHAPES[name]), F32, kind="ExternalOutput").ap()
            try:
                self.setup()
                for l in range(2):
                    if ("L%d" % l) not in self.stages:
                        continue
                    self.layer(l)
                if "FIN" in self.stages:
                    self.final()
            except _Stop:
                pass
            S.barrier()
        return nc

    def setup(self):
        S, nc, es = self.S, self.nc, self.es
        self.pb = [S.ps(es, "pb%d" % i, [128, 512]) for i in range(8)]
        cst = self.cst = S.sb(es, "cst", [128, 8, 128], F32)
        S.dma("sp", cst[:], self.din("consts", [128, 8, 128])[:, :, :])
        cstb = self.cstb = S.sb(es, "cstb", [128, 8, 128], BF16)
        S.cp("dve", cstb[:], cst[:])
        self.ident = cst[:, 0, :]
        ncst = self.ncst = S.sb(es, "ncst", [128, 2, 128], F32)
        S.ts("dve", ncst[:, 0, :], cst[:, 3, :], -1.0, None, op0=ALU.mult)
        S.ts("dve", ncst[:, 1, :], cst[:, 5, :], -1.0, None, op0=ALU.mult)
        self.identb = cstb[:, 0, :]
        self.onesb = cstb[:, 1, :]
        pc = self.pc = S.sb(es, "pc", [128, NPROW], F32)
        prow_d = self.din("prows", [NPROW, 128])
        with S.scope() as es2:
            stg = S.sb(es2, "pstg", [128, NPROW // 128, 128], F32)
            for g in range(NPROW // 128):
                S.dma("sp", stg[:, g, :], prow_d[g * 128:(g + 1) * 128, :])
                pt = self.pb[g % 2]
                S.tr(pt[:, 0:128], stg[:, g, :], self.ident)
                S.cp("dve", pc[:, g * 128:(g + 1) * 128], pt[:, 0:128])
            S.barrier()
        hT = self.hT = S.sb(es, "hT", [128, KC, TT], F32)
        x_d = self.din("x", [NLAT, D])
        ctx_d = self.din("ctx", [NCTX, D])
        with S.scope() as es2:
            xin = [S.sb(es2, "xin%d" % i, [128, D], F32) for i in range(2)]
            for ti in range(TT // 128):
                t0 = ti * 128
                src = ctx_d[t0:t0 + 128, :] if t0 < NCTX else x_d[t0 - NCTX:t0 - NCTX + 128, :]
                xt = xin[ti % 2]
                S.dma("sp", xt[:], src)
                for half in range(2):
                    pt = self.pb[(ti * 2 + half) % 4]
                    for q in range(4):
                        kc = half * 4 + q
                        S.tr(pt[:, q * 128:(q + 1) * 128], xt[:, kc * 128:(kc + 1) * 128], self.ident)
                    S.cp("dve" if half == 0 else "act", hT("t%d" % ti)[:, half * 4:half * 4 + 4, t0:t0 + 128],
                         V(pt.h[:].rearrange("p (q t) -> p q t", q=4), (pt.name, None)))
            S.barrier()
        self.tap("h0", hT[:], [128, KC, TT])
        self.mods()

    def col(self, name, r=0, n=1):
        off, cnt = PROWS_STATIC[name]
        return self.pc[:, off + r:off + r + n]

    def mods(self):
        S, nc, es = self.S, self.nc, self.es
        sT = self.sT = S.sb(es, "sT", [128, KC, 2], F32)
        S.act(sT[:, :, 0], self.col("c", 0, 8), AF.Silu)
        S.act(sT[:, :, 1], self.col("c_ctx", 0, 8), AF.Silu)
        self.mod = [S.sb(es, "mod%d" % l, [128, 48, 2], F32) for l in range(2)]
        with S.scope() as es2:
            wb = [S.sb(es2, "adaw%d" % i, [128, KC, 256], F32) for i in range(2)]
            interleave(self.mods_gen(0, wb))
            if "rglru" not in self.stages:
                interleave(self.mods_gen(1, wb))
        for l in range(2):
            self.tap("mod%d" % l, self.mod[l][:], [128, 48, 2])

    def mods_gen(self, l, wb):
        S = self.S
        ada_d = self.din("ada_w", [2, D, 6 * D])
        md = self.mod[l]
        sT = self.sT
        for g in range(24):
            w = wb[g % 2]
            S.dma("sp", w[:], ada_d[l, :, g * 256:(g + 1) * 256].rearrange("(kc p) n -> p kc n", p=128))
            pt = self.pb[6 + (g % 2)]
            for q in range(2):
                for kc in range(KC):
                    S.mm(pt[:, q * 2:q * 2 + 2], w[:, kc, q * 128:(q + 1) * 128], sT[:, kc, :], start=(kc == 0), stop=(kc == KC - 1))
            yield
            for q in range(2):
                j = g * 2 + q
                S.ts("dve", md[:, j, :], pt[:, q * 2:q * 2 + 2], self.col("ada_b%d" % l, j, 1), None, op0=ALU.add)
            yield


def in_map(inp, b, needed):
    m = {}
    for name in needed:
        if name == "consts":
            m[name] = _consts()
        elif name == "prows":
            m[name] = _pack_rows(inp, b)
        elif name == "x":
            m[name] = np.ascontiguousarray(inp["x"][b])
        elif name == "ctx":
            m[name] = np.ascontiguousarray(inp["ctx"][b])
        elif name == "rb128":
            m[name] = np.ascontiguousarray(np.broadcast_to(inp["o_router_b"][0][None, :], (128, NEXP)))
        elif name == "dn_sc":
            m[name] = np.ascontiguousarray(np.stack([inp["e_b_a_log"][0].reshape(8), inp["e_b_dt_bias"][0].reshape(8)], 1))
        else:
            a = inp[name]
            m[name] = np.ascontiguousarray(a)
    return m


def _layer_methods():
    pass


def modulate(self, l, which, xn, perm=False, nblk=5):
    S, es = self.S, self.es
    hT = self.hT
    g0 = 0 if which == 0 else 3
    gname = ("nmix%d" if which == 0 else "nffn%d") % l
    md = self.mod[l]
    with S.scope() as es2:
        gs = S.sb(es2, "gs", [128, KC, 2], F32)
        sq = [S.sb(es2, "sq%d" % i, [128, 512], BF16) for i in range(2)]
        rsb = S.sb(es2, "rsb", [128, 512], F32)
        tmp = [S.sb(es2, "mtmp%d" % i, [128, 512], F32) for i in range(2)]
        for w in range(2):
            S.ts("dve", gs[:, :, w], md[:, (g0 + 1) * 8:(g0 + 2) * 8, w], 1.0, 32.0, op0=ALU.add, op1=ALU.mult)
            S.tt("dve", gs[:, :, w], gs[:, :, w], self.col(gname, 0, 8), ALU.mult)
        for (t0, t1) in BLKS[5 - nblk:]:
            w = 1 if t0 < NCTX else 0
            n = t1 - t0
            ss = self.pb[0]
            for kc in range(KC):
                s = sq[kc % 2]
                S.act(s[:, :n], hT[:, kc, t0:t1], AF.Square)
                S.mm(ss[:, :n], self.onesb, s[:, :n], start=(kc == 0), stop=(kc == KC - 1))
            S.ts("dve", rsb[:, :n], ss[:, :n], 1024.0 * EPS, None, op0=ALU.add)
            S.act(rsb[:, :n], rsb[:, :n], AF.Sqrt)
            S.recip(rsb[:, :n], rsb[:, :n])
            if t1 == TT:
                self.tap("rsb", rsb[:], [128, 512])
                self.tap("gs", gs[:], [128, KC, 2])
            for kc in range(KC):
                tm = tmp[kc % 2]
                S.tt("dve", tm[:, :n], hT[:, kc, t0:t1], rsb[:, :n], ALU.mult)
                if perm and t0 >= NCTX:
                    r0 = (t0 - NCTX) // 64
                    r1 = (t1 - NCTX) // 64
                    o = V(xn.h[:, kc, NCTX:TT].rearrange("p (c r) -> p r c", c=64, r=32)[:, r0:r1, :], (xn.name, None))
                    i_ = V(tm.h[:, :n].rearrange("p (r c) -> p r c", c=64), (tm.name, None))
                else:
                    o = xn[:, kc, t0:t1]
                    i_ = tm[:, :n]
                S.act(o, i_, AF.Identity, bias=md[:, g0 * 8 + kc, w:w + 1], scale=gs[:, kc, w:w + 1])


def proj(self, xn, w_d, N, consumer, nblk=5, wq="pool"):
    S, es = self.S, self.es
    with S.scope() as es2:
        wb = [S.sb(es2, "pw%d" % i, [128, KC, 512], BF16) for i in range(2)]
        ng = (N + 511) // 512
        pi = 0
        for g in range(ng):
            n0 = g * 512
            nn = min(512, N - n0)
            w = wb[g % 2]
            S.dma(wq, w[:, :, :nn], w_d[:, n0:n0 + nn].rearrange("(kc p) n -> p kc n", p=128))
            for q in range((nn + 127) // 128):
                m = min(128, nn - q * 128)
                for (t0, t1) in BLKS[5 - nblk:]:
                    n = t1 - t0
                    pt = self.pb[2 + (pi % 4)]
                    pi += 1
                    for kc in range(KC):
                        S.mm(pt[0:m, :n], w[:, kc, q * 128:q * 128 + m], xn[:, kc, t0:t1], start=(kc == 0), stop=(kc == KC - 1))
                    consumer(g * 4 + q, m, (t0, t1), pt[0:m, :n])


def conv4(self, out, x, wcols, bias=None):
    S = self.S
    for (a, b) in ((0, NCTX), (NCTX, TT)):
        if bias is None:
            S.ts("dve", out[:, a:b], x[:, a:b], wcols[2], None, op0=ALU.mult)
        else:
            S.ts("dve", out[:, a:b], x[:, a:b], wcols[2], bias, op0=ALU.mult, op1=ALU.add)
        S.stt("dve", out[:, a + 2:b], x[:, a:b - 2], wcols[0], out[:, a + 2:b], ALU.mult, ALU.add)
        S.stt("dve", out[:, a + 1:b], x[:, a:b - 1], wcols[1], out[:, a + 1:b], ALU.mult, ALU.add)
        S.stt("dve", out[:, a:b - 1], x[:, a + 1:b], wcols[3], out[:, a:b - 1], ALU.mult, ALU.add)


def inproj0(self, xn):
    S, es = self.S, self.es
    U0 = self.U0 = S.dram("U0", [25, 128, TT], F32)
    w_d = self.din("e_w_in", [1, D, E_IN])[0]
    with S.scope() as es2:
        pre = [S.sb(es2, "pre%d" % i, [128, TT], F32) for i in range(2)]
        post = [S.sb(es2, "post%d" % i, [128, TT], F32) for i in range(2)]
        cnt = [0]

        def consumer(ci, m, blk, pv):
            t0, t1 = blk
            p = pre[ci % 2]
            S.cp("act", p[0:m, t0:t1], pv)
            if t1 < TT:
                return
            po = post[ci % 2]
            if ci < 4:
                wc = [self.col("a_conv_w", k * 4 + ci, 1) for k in range(4)]
                conv4(self, po, p, wc, bias=self.col("a_conv_b", ci, 1))
                S.dma("sp", U0("c%d" % ci)[ci, :, :], po[:])
            elif 8 <= ci < 20:
                wc = [self.col("b_conv_w", k * 12 + (ci - 8), 1) for k in range(4)]
                conv4(self, po, p, wc)
                S.act(po[:], po[:], AF.Silu)
                S.dma("sp", U0("c%d" % ci)[ci, :, :], po[:])
            else:
                S.dma("sp", U0("c%d" % ci)[ci, 0:m, :], p[0:m, :])

        proj(self, xn, w_d, E_IN, consumer)
        S.barrier()


def rev(t, a, b):
    return V(t.h[:, a:b][:, ::-1], (t.name, None))


def rglru(self, XM):
    S, es = self.S, self.es
    U0 = self.U0
    gw_d = self.din("e_a_gate_w", [1, 2, 2, 8, 64, 64])[0]
    with S.scope() as es2:
        gwf = S.sb(es2, "gwf", [128, 16, 128], F32)
        gwb = S.sb(es2, "gwb", [128, 16, 128], BF16)
        nsp = S.sb(es2, "nsp", [128, 8], F32)
        ua = S.sb(es2, "ua", [128, TT], F32)
        uab = S.sb(es2, "uab", [128, TT], BF16)
        ra = S.sb(es2, "ra", [128, TT], F32)
        ii = S.sb(es2, "ii", [128, TT], F32)
        mx = S.sb(es2, "mx", [128, TT], F32)
        hh = [S.sb(es2, "hh%d" % i, [128, TT], F32) for i in range(2)]
        S.memset("pool", gwf[:], 0.0)
        for d in range(2):
            for g in range(2):
                for c in range(4):
                    for half in range(2):
                        S.dma("sp", gwf[half * 64:(half + 1) * 64, (d * 2 + g) * 4 + c, half * 64:(half + 1) * 64],
                              gw_d[d, g, 2 * c + half, :, :])
        S.cp("dve", gwb[:], gwf[:])
        S.act(nsp[:], self.col("a_lambda", 0, 8), AF.Exp, scale=-1.0)
        S.act(nsp[:], nsp[:], AF.Ln, bias=1.0)
        S.ts("dve", nsp[:], nsp[:], -8.0, None, op0=ALU.mult)
        wbm = [S.sb(es2, "adawr%d" % i, [128, KC, 256], F32) for i in range(2)]
        interleave(rglru_body(self, XM, U0, ua, uab, ra, ii, mx, hh, gwb, nsp), self.mods_gen(1, wbm))
        S.barrier()


def rglru_body(self, XM, U0, ua, uab, ra, ii, mx, hh, gwb, nsp):
    S = self.S
    if True:
        for c in range(4):
            S.dma("sp", ua[:], U0("c%d" % c)[c, :, :])
            S.cp("pool", uab[:], ua[:])
            yield
            for d in range(2):
                for (t0, t1) in BLKS:
                    n = t1 - t0
                    pr, pi = self.pb[4], self.pb[5]
                    S.mm(pr[:, :n], gwb[:, (d * 2 + 0) * 4 + c, :], uab[:, t0:t1])
                    S.mm(pi[:, :n], gwb[:, (d * 2 + 1) * 4 + c, :], uab[:, t0:t1])
                    S.act(ra[:, t0:t1], pr[:, :n], AF.Sigmoid, bias=self.col("a_gate_b", (d * 2 + 0) * 4 + c, 1))
                    S.act(ii[:, t0:t1], pi[:, :n], AF.Sigmoid, bias=self.col("a_gate_b", (d * 2 + 1) * 4 + c, 1))
                    yield
                S.act(ra[:], ra[:], AF.Exp, scale=nsp[:, d * 4 + c:d * 4 + c + 1])
                yield
                S.tt("pool", mx[:], ra[:], ra[:], ALU.mult)
                S.act(mx[:], mx[:], AF.Sqrt, bias=1.0, scale=-1.0)
                yield
                S.tt("dve", mx[:], mx[:], ii[:], ALU.mult)
                S.tt("dve", mx[:], mx[:], ua[:], ALU.mult)
                h = hh[d]
                if d == 0:
                    S.scan(h[:], ra[:], mx[:], 0.0)
                else:
                    S.scan(rev(h, 0, NCTX), rev(ra, 0, NCTX), rev(mx, 0, NCTX), 0.0)
                    S.scan(rev(h, NCTX, TT), rev(ra, NCTX, TT), rev(mx, NCTX, TT), h[:, 0:1])
                yield
            S.tt("dve", hh[0][:], hh[0][:], hh[1][:], ALU.add)
            self.tap("ha%d" % c, hh[0][:], [128, TT])
            S.dma("sp", ii[:], U0("c%d" % (4 + c))[4 + c, :, :])
            gelu_tanh(self, mx, ii, ra)
            S.tt("dve", XM[:, c, :], hh[0][:], mx[:], ALU.mult)
            yield


def gelu_tanh(self, out, x, tmp):
    S = self.S
    S.tt("pool", tmp[:], x[:], x[:], ALU.mult)
    S.ts("dve", tmp[:], tmp[:], 0.044715, 1.0, op0=ALU.mult, op1=ALU.add)
    S.tt("dve", tmp[:], tmp[:], x[:], ALU.mult)
    S.act(tmp[:], tmp[:], AF.Tanh, scale=0.7978845608028654)
    S.ts("dve", tmp[:], tmp[:], 1.0, 0.5, op0=ALU.add, op1=ALU.mult)
    S.tt("dve", out[:], tmp[:], x[:], ALU.mult)


def layer(self, l):
    S, es = self.S, self.es
    with S.scope() as es2:
        XM = S.sb(es2, "XM", [128, KC, TT], BF16)
        if l == 0:
            modulate(self, 0, 0, XM)
            self.tap("xn0", XM[:], [128, KC, TT])
            if "inproj0" in self.stages:
                inproj0(self, XM)
            if "rglru" in self.stages:
                rglru(self, XM)
            if "deltanet" in self.stages:
                deltanet(self, XM)
                S.dead = False
                S.barrier()
            if "outproj" in self.stages:
                outproj(self, 0, XM, self.din("e_w_out", [1, D, D])[0])
                S.barrier()
                self.tap("hmid0", self.hT[:], [128, KC, TT])
            if "ffn0" in self.stages:
                ffn0(self, XM)
                self.tap("h1", self.hT[:], [128, KC, TT])
        if l == 1:
            modulate(self, 1, 0, XM, perm=True)
            inproj1(self, XM)
            mixer1(self, XM)
            S.dead = False
            outproj(self, 1, XM, self.din("o_w_out", [1, D, D])[0], perm=True, nblk=4)
            S.barrier()
            self.tap("hmid1", self.hT[:], [128, KC, TT])
            if "nomoe" not in self.stages:
                moe(self, XM)
                self.tap("h2", self.hT[:], [128, KC, TT])
        S.barrier()


Prog.layer = layer


def interleave(*gens):
    gens = [g for g in gens if g is not None]
    while gens:
        for g in list(gens):
            try:
                next(g)
            except StopIteration:
                gens.remove(g)


def chunk_order(nck_ctx, nck_all, d):
    if d == 0:
        return list(range(nck_all))
    return list(range(nck_ctx - 1, -1, -1)) + list(range(nck_all - 1, nck_ctx - 1, -1))


def deltanet(self, XM):
    S, es = self.S, self.es
    U0 = self.U0
    cst = self.cst
    NCK = TT // 128
    with S.scope() as es2:
        ROWS = S.sb(es2, "dnROWS", [128, TT], F32)
        GTb = S.sb(es2, "dnGTb", [128, 8, NCK], F32)
        oacc = S.sb(es2, "oacc", [128, TT], F32)
        with S.scope() as es3:
            G = S.sb(es3, "dnG", [64, TT], F32)
            BETA = S.sb(es3, "dnBeta", [64, TT], F32)
            GCS = S.sb(es3, "dnGCS", [64, TT], F32)
            TLS = S.sb(es3, "dnTLS", [64, TT], F32)
            sc = S.sb(es3, "dnsc", [64, 4], F32)
            sel = S.sb(es3, "dnsel", [128, 8, 128], F32)
            PF = S.sb(es3, "dnPF", [64, TT], F32)
            SF = S.sb(es3, "dnSF", [64, TT], F32)
            mk = S.sb(es3, "dnmk", [64, TT], F32)
            S.memset("pool", G[:], 0.0)
            S.memset("pool", BETA[:], 0.0)
            S.memset("pool", sc[:], 0.0)
            S.memset("pool", ROWS[:], 0.0)
            dn_sc = self.din("dn_sc", [8, 2])
            for d in range(2):
                S.dma("sp", G[32 * d:32 * d + 4, :], U0("c24")[24, 8 + 4 * d:12 + 4 * d, :])
                S.dma("sp", BETA[32 * d:32 * d + 4, :], U0("c24")[24, 4 * d:4 * d + 4, :])
                S.dma("sp", sc[32 * d:32 * d + 4, 0:2], dn_sc[4 * d:4 * d + 4, :])
            S.act(G[:], G[:], AF.Exp, bias=sc[:, 1:2])
            S.act(G[:], G[:], AF.Ln, bias=1.0)
            S.act(sc[:, 2:3], sc[:, 0:1], AF.Exp)
            S.ts("dve", sc[:, 2:3], sc[:, 2:3], -1.0, None, op0=ALU.mult)
            S.ts("dve", G[:], G[:], sc[:, 2:3], None, op0=ALU.mult)
            S.act(BETA[:], BETA[:], AF.Sigmoid)
            if "dn_stopA" in self.stages:
                self.tap("dnG", G[:], [64, TT])
                S.barrier()
                return
            S.memset("pool", mk[:], 1.0)
            S.memset("pool", V(mk.h[:, 0:TT:128], (mk.name, None)), 0.0)
            S.scan(PF[:], mk[:], G[:], 0.0)
            S.memset("pool", mk[:], 1.0)
            S.memset("pool", V(mk.h[:, 127:TT:128], (mk.name, None)), 0.0)
            S.scan(rev(SF, 0, TT), rev(mk, 0, TT), rev(G, 0, TT), 0.0)
            S.cp("dve", GCS[0:32, :], PF[0:32, :])
            S.cp("dve", GCS[32:64, :], SF[32:64, :])
            S.tt("dve", TLS[0:32, :], SF[0:32, :], G[0:32, :], ALU.subtract)
            S.tt("dve", TLS[32:64, :], PF[32:64, :], G[32:64, :], ALU.subtract)
            S.act(TLS[:], TLS[:], AF.Exp)
            if "dn_stopB" in self.stages:
                self.tap("dnG", GCS[:], [64, TT])
                S.barrier()
                return
            for d in range(2):
                for kind, src in enumerate((GCS, BETA, TLS)):
                    S.dma("sp", ROWS[32 * d + 4 * kind:32 * d + 4 * kind + 4, :], src[32 * d:32 * d + 4, :])
            if "dn_stopC" in self.stages:
                self.tap("dnG", ROWS[:], [64, TT])
                S.barrier()
                return
            for r8 in range(8):
                r = (r8 // 4) * 32 + (r8 % 4)
                S.ts("dve", sel[:, r8, :], cst[:, 1, :], cst[:, 0, r:r + 1], None, op0=ALU.mult)
            gte = S.sb(es3, "dngte", [128, NCK], F32)
            S.memset("dve", gte[:], 0.0)
            S.cp("dve", gte[0:64, :], V(PF.h[:, 127:TT:128], (PF.name, None)))
            for r8 in range(8):
                pt = self.pb[0]
                S.mm(pt[:, 0:NCK], sel[:, r8, :], gte[:])
                S.act(GTb[:, r8, :], pt[:, 0:NCK], AF.Exp)
            S.barrier()
        self.tap("dnrows", ROWS[:], [128, TT])
        self.tap("dngtb", GTb[:], [128, 8, NCK])
        if "dn_stop1" in self.stages:
            return

        for h in range(4):
            for d in range(2):
                r8 = d * 4 + h
                rr = d * 32 + h
                Mi = cst[:, 2, :] if d == 0 else cst[:, 4, :]
                Ms = self.ncst[:, 0, :] if d == 0 else self.ncst[:, 1, :]
                MsT = self.ncst[:, 1, :] if d == 0 else self.ncst[:, 0, :]
                with S.scope() as es3:
                    kdec = S.sb(es3, "kdec", [128, TT], BF16)
                    qdec = S.sb(es3, "qdec", [128, TT], BF16)
                    AT = S.sb(es3, "AT", [128, NCK, 128], BF16)
                    TTt = S.sb(es3, "TTt", [128, NCK, 128], F32)
                    vb = S.sb(es3, "vb", [128, NCK, 128], BF16)
                    kntm = S.sb(es3, "kntm", [128, NCK, 128], BF16)
                    cols = S.sb(es3, "cols", [128, NCK, 4], F32)
                    Sf = S.sb(es3, "Sf", [128, 128], F32)
                    Sb = S.sb(es3, "Sb", [128, 128], BF16)
                    sel2 = S.sb(es3, "sel2", [128, 2, 128], F32)
                    mk3 = S.sb(es3, "mk3", [128, 3, 128], F32)
                    qc = S.sb(es3, "qc", [128, 512], F32)
                    kc = S.sb(es3, "kc", [128, 512], F32)
                    vc = S.sb(es3, "vc", [128, 512], F32)
                    sqb = S.sb(es3, "sqb", [128, 512], BF16)
                    rs = S.sb(es3, "rs", [128, 512], F32)
                    GCb = S.sb(es3, "GCb", [128, 512], F32)
                    Eb = S.sb(es3, "Eb", [128, 512], F32)
                    knb = S.sb(es3, "knb", [128, 512], BF16)
                    kbb = S.sb(es3, "kbb", [128, 512], BF16)
                    qnb = S.sb(es3, "qnb", [128, 512], BF16)
                    Dm = S.sb(es3, "Dm", [128, 4, 128], F32)
                    Ds = S.sb(es3, "Ds", [128, 4, 128], F32)
                    DT = S.sb(es3, "DTm", [128, 4, 128], F32)
                    Mm = [S.sb(es3, "Mm%d" % i, [128, 2, 4, 128], DBL_DT) for i in range(2)]
                    Xx = [S.sb(es3, "Xx%d" % i, [128, 4, 128], DBL_DT) for i in range(2)]
                    rb = [S.sb(es3, "rb%d" % i, [128, 128], F32) for i in range(2)]
                    vnb = [S.sb(es3, "vnb%d" % i, [128, 128], BF16) for i in range(2)]
                    vns = [S.sb(es3, "vns%d" % i, [128, 128], BF16) for i in range(2)]
                    S.ts("dve", sel2[:, 0, :], cst[:, 1, :], cst[:, 0, rr:rr + 1], None, op0=ALU.mult)
                    S.ts("dve", sel2[:, 1, :], cst[:, 1, :], cst[:, 0, rr + 4:rr + 5], None, op0=ALU.mult)
                    Mi_, Ms_, MsT_ = (cst[:, 2, :], cst[:, 3, :], cst[:, 5, :]) if d == 0 else (cst[:, 4, :], cst[:, 5, :], cst[:, 3, :])
                    S.ts("dve", mk3[:, 0, :], Mi_, 1e30, -1e30, op0=ALU.mult, op1=ALU.add)
                    S.ts("dve", mk3[:, 1, :], Ms_, 1e30, -1e30, op0=ALU.mult, op1=ALU.add)
                    S.ts("dve", mk3[:, 2, :], MsT_, -1e30, 1e30, op0=ALU.mult, op1=ALU.add)
                    S.memset("dve", Sf[:], 0.0)
                    S.memset("dve", Sb[:], 0.0)

                    def pv3(pt, nck):
                        return V(pt.h[:, 0:nck * 128].rearrange("p (c t) -> p c t", c=nck), (pt.name, None))

                    def bc2(v, nck):
                        return V(_ap(v).unsqueeze(1).to_broadcast([128, nck, 128]), v.dep)

                    def prep_block(bi):
                        t0, t1 = BLKS[bi]
                        n = t1 - t0
                        nck = n // 128
                        c0 = t0 // 128
                        q_, k_, v_ = qc, kc, vc
                        S.dma("sp", q_[:, :n], U0("c%d" % (8 + h))[8 + h, :, t0:t1])
                        S.dma("sp", k_[:, :n], U0("c%d" % (12 + h))[12 + h, :, t0:t1])
                        S.dma("sp", v_[:, :n], U0("c%d" % (16 + h))[16 + h, :, t0:t1])
                        p0, p1, p2 = self.pb[0], self.pb[1], self.pb[2]
                        S.mm(p1[:, :n], sel2[:, 0, :], ROWS[:, t0:t1])
                        S.mm(p2[:, :n], sel2[:, 1, :], ROWS[:, t0:t1])
                        for cc in range(nck):
                            S.mm(p0[:, cc * 64:(cc + 1) * 64], ROWS[:, t0 + cc * 128:t0 + cc * 128 + 128], cst[:, 0, 0:64])
                        yield
                        S.cp("act", GCb[:, :n], p1[:, :n])
                        S.act(Eb[:, :n], p1[:, :n], AF.Exp)
                        colp = V(p0.h[:, 0:nck * 64].rearrange("p (c t) -> p c t", c=nck), (p0.name, None))
                        S.cp("dve", cols[:, c0:c0 + nck, 0], V(colp.ap[:, :, rr], colp.dep))
                        S.ts("dve", cols[:, c0:c0 + nck, 1], V(colp.ap[:, :, rr + 4], colp.dep), -1.0, None, op0=ALU.mult)
                        S.cp("dve", cols[:, c0:c0 + nck, 2], V(colp.ap[:, :, rr + 8], colp.dep))
                        S.cp("dve", cols[:, c0:c0 + nck, 3], V(colp.ap[:, :, rr + 4], colp.dep))
                        S.tt("dve", sqb[:, :n], k_[:, :n], k_[:, :n], ALU.mult)
                        yield
                        S.mm(p0[:, :n], self.onesb, sqb[:, :n])
                        yield
                        S.act(rs[:, :n], p0[:, :n], AF.Ln, bias=EPS)
                        S.act(rs[:, :n], rs[:, :n], AF.Exp, scale=-0.5)
                        yield
                        S.tt("dve", k_[:, :n], k_[:, :n], rs[:, :n], ALU.mult)
                        S.tt("pool", sqb[:, :n], q_[:, :n], q_[:, :n], ALU.mult)
                        S.tt("pool", kdec[:, t0:t1], k_[:, :n], Eb[:, :n], ALU.mult)
                        S.tt("dve", kbb[:, :n], k_[:, :n], p2[:, :n], ALU.mult)
                        yield
                        S.mm(p0[:, :n], self.onesb, sqb[:, :n])
                        S.cp("act", knb[:, :n], k_[:, :n])
                        yield
                        S.act(rs[:, :n], p0[:, :n], AF.Ln, bias=EPS)
                        S.act(rs[:, :n], rs[:, :n], AF.Exp, scale=-0.5)
                        pvt, pkt = self.pb[1], self.pb[2]
                        for cc in range(nck):
                            S.tr(pvt[:, cc * 128:(cc + 1) * 128], v_[:, cc * 128:(cc + 1) * 128], self.ident)
                        for cc in range(nck):
                            S.tr(pkt[:, cc * 128:(cc + 1) * 128], k_[:, cc * 128:(cc + 1) * 128], self.ident)
                        yield
                        S.stt("dve", q_[:, :n], q_[:, :n], 128.0 ** -0.5, rs[:, :n], ALU.mult, ALU.mult)
                        S.tt("pool", qdec[:, t0:t1], q_[:, :n], Eb[:, :n], ALU.mult)
                        bcol = V(cols.h[:, c0:c0 + nck, 3:4].to_broadcast([128, nck, 128]), (cols.name, None))
                        S.tt("dve", vb[:, c0:c0 + nck, :], pv3(pvt, nck), bcol, ALU.mult)
                        S.cp("act", qnb[:, :n], q_[:, :n])
                        S.cp("act", kntm[:, c0:c0 + nck, :], pv3(pkt, nck))
                        yield
                        pg1, pg2, pg3 = self.pb[3], self.pb[4], self.pb[5]
                        for cc in range(nck):
                            a_, b_ = cc * 128, cc * 128 + 128
                            S.mm(pg1[:, a_:b_], knb[:, a_:b_], kbb[:, a_:b_])
                        for cc in range(nck):
                            a_, b_ = cc * 128, cc * 128 + 128
                            S.mm(pg2[:, a_:b_], kbb[:, a_:b_], knb[:, a_:b_])
                        for cc in range(nck):
                            a_, b_ = cc * 128, cc * 128 + 128
                            S.mm(pg3[:, a_:b_], knb[:, a_:b_], qnb[:, a_:b_])
                        gcol = V(cols.h[:, c0:c0 + nck, 0:1].to_broadcast([128, nck, 128]), (cols.name, None))
                        gcb3 = V(GCb.h[:, 0:n].rearrange("p (c t) -> p c t", c=nck), (GCb.name, None))
                        S.tt("dve", Dm[:, 0:nck, :], gcb3, gcol, ALU.subtract)
                        S.stt("dve", DT[:, 0:nck, :], Dm[:, 0:nck, :], 0.0, bc2(mk3[:, 2, :], nck), ALU.max, ALU.add)
                        S.stt("dve", Ds[:, 0:nck, :], Dm[:, 0:nck, :], 0.0, bc2(mk3[:, 1, :], nck), ALU.min, ALU.add)
                        S.stt("dve", Dm[:, 0:nck, :], Dm[:, 0:nck, :], 0.0, bc2(mk3[:, 0, :], nck), ALU.min, ALU.add)
                        yield
                        S.act(DT[:, 0:nck, :], DT[:, 0:nck, :], AF.Exp, scale=-1.0)
                        S.act(Ds[:, 0:nck, :], Ds[:, 0:nck, :], AF.Exp)
                        S.act(Dm[:, 0:nck, :], Dm[:, 0:nck, :], AF.Exp)
                        yield
                        M0 = Mm[0]
                        S.stt("dve", r32v(M0[:, 0, 0:nck, :]), pv3(pg1, nck), -1.0, Ds[:, 0:nck, :], ALU.mult, ALU.mult)
                        S.stt("dve", r32v(M0[:, 1, 0:nck, :]), pv3(pg2, nck), -1.0, DT[:, 0:nck, :], ALU.mult, ALU.mult)
                        S.tt("dve", AT[:, c0:c0 + nck, :], pv3(pg3, nck), Dm[:, 0:nck, :], ALU.mult)
                        S.tt("dve", r32v(Xx[0][:, 0:nck, :]), M0[:, 0, 0:nck, :], bc2(cst[:, 0, :], nck), ALU.add)
                        yield
                        cur = 0
                        for lev in range(6):
                            Mc, Mn = Mm[cur], Mm[1 - cur]
                            pa, pbk, px = self.pb[3], self.pb[4], self.pb[5]
                            for cc in range(nck):
                                S.mm(pbk[:, cc * 128:(cc + 1) * 128], Mc[:, 0, cc, :], Mc[:, 1, cc, :], r32=R32)
                            if lev < 5:
                                for cc in range(nck):
                                    S.mm(pa[:, cc * 128:(cc + 1) * 128], Mc[:, 1, cc, :], Mc[:, 0, cc, :], r32=R32)
                            yield
                            S.cp("act", r32v(Mn[:, 1, 0:nck, :]), pv3(pbk, nck))
                            if lev < 5:
                                S.cp("act", r32v(Mn[:, 0, 0:nck, :]), pv3(pa, nck))
                            yield
                            Xc, Xn = Xx[lev % 2], Xx[1 - lev % 2]
                            for cc in range(nck):
                                S.mm(px[:, cc * 128:(cc + 1) * 128], Mn[:, 1, cc, :], Xc[:, cc, :], r32=R32)
                            yield
                            if lev < 5:
                                S.tt("dve", r32v(Xn[:, 0:nck, :]), pv3(px, nck), Xc[:, 0:nck, :], ALU.add)
                            else:
                                S.tt("dve", TTt[:, c0:c0 + nck, :], pv3(px, nck), Xc[:, 0:nck, :], ALU.add)
                            cur = 1 - cur
                            yield

                    step = [0]

                    def rec_chunk(c):
                        si = step[0]
                        step[0] += 1
                        ta, tb = c * 128, c * 128 + 128
                        p6, p7 = self.pb[6], self.pb[7]
                        S.mm(p6[:, 0:128], kdec[:, ta:tb], Sb[:])
                        yield
                        r_ = rb[si % 2]
                        S.stt("dve", r_[:], p6[:, 0:128], cols[:, c, 1:2], vb[:, c, :], ALU.mult, ALU.add)
                        yield
                        S.mm(p6[:, 128:256], TTt[:, c, :], r_[:])
                        yield
                        vn, vs = vnb[si % 2], vns[si % 2]
                        S.cp("dve", vn[:], p6[:, 128:256])
                        S.ts("dve", vs[:], p6[:, 128:256], cols[:, c, 2:3], None, op0=ALU.mult)
                        yield
                        S.mm(p7[:, 128:256], kntm[:, c, :], vs[:])
                        S.mm(p7[:, 0:128], Sb[:], qdec[:, ta:tb], start=True, stop=False)
                        S.mm(p7[:, 0:128], vn[:], AT[:, c, :], start=False, stop=True)
                        yield
                        S.stt("dve", Sb[:], Sf[:], GTb[:, r8, c:c + 1], p7[:, 128:256], ALU.mult, ALU.add)
                        S.stt("dve", Sf[:], Sf[:], GTb[:, r8, c:c + 1], p7[:, 128:256], ALU.mult, ALU.add)
                        if d == 0:
                            S.cp("dve", oacc[:, ta:tb], p7[:, 0:128])
                        else:
                            S.tt("dve", oacc[:, ta:tb], oacc[:, ta:tb], p7[:, 0:128], ALU.add)
                        yield

                    def rec_block(bi):
                        t0, t1 = BLKS[bi]
                        cs = list(range(t0 // 128, t1 // 128))
                        if d == 1:
                            cs = cs[::-1]
                        for c in cs:
                            yield from rec_chunk(c)

                    border = [0, 1, 2, 3, 4] if d == 0 else [0, 4, 3, 2, 1]
                    for k_i in range(len(border) + 1):
                        gp = prep_block(border[k_i]) if k_i < len(border) else None
                        gr = rec_block(border[k_i - 1]) if k_i > 0 else None
                        interleave(gp, gr)
            self.tap("ob%d" % h, oacc[:], [128, TT])
            with S.scope() as es3:
                zb = [S.sb(es3, "zb%d" % i, [128, 512], F32) for i in range(2)]
                sqb = S.sb(es3, "gsq", [128, 512], BF16)
                rs = S.sb(es3, "grs", [128, 512], F32)
                for bi, (t0, t1) in enumerate(BLKS):
                    n = t1 - t0
                    z_ = zb[bi % 2]
                    S.dma("sp", z_[:, :n], U0("c%d" % (20 + h))[20 + h, :, t0:t1])
                    S.act(sqb[:, :n], oacc[:, t0:t1], AF.Square)
                    p0 = self.pb[0]
                    S.mm(p0[:, :n], self.onesb, sqb[:, :n])
                    S.ts("dve", rs[:, :n], p0[:, :n], 1.0 / 128.0, EPS, op0=ALU.mult, op1=ALU.add)
                    S.act(rs[:, :n], rs[:, :n], AF.Sqrt)
                    S.recip(rs[:, :n], rs[:, :n])
                    S.act(z_[:, :n], z_[:, :n], AF.Silu)
                    S.stt("dve", rs[:, :n], oacc[:, t0:t1], self.col("b_norm_g", 0, 1), rs[:, :n], ALU.mult, ALU.mult)
                    S.tt("dve", XM[:, 4 + h, t0:t1], rs[:, :n], z_[:, :n], ALU.mult)
                S.barrier()


def outproj(self, l, XM, w_d, perm=False, nblk=5):
    S = self.S
    hT = self.hT
    md = self.mod[l]

    def consumer(ci, m, blk, pv):
        t0, t1 = blk
        w = 1 if t0 < NCTX else 0
        if perm and t0 >= NCTX:
            c0 = (t0 - NCTX) // 32
            c1 = (t1 - NCTX) // 32
            hv = V(hT.h[:, ci, NCTX:TT].rearrange("p (r c) -> p c r", r=32, c=64)[:, c0:c1, :], (hT.name, None))
            pvv = V(_ap(pv).rearrange("p (c r) -> p c r", r=32), pv.dep)
            S.stt("dve", hv, pvv, md[:, 16 + ci, w:w + 1], hv, ALU.mult, ALU.add)
        else:
            S.stt("dve", hT[:, ci, t0:t1], pv, md[:, 16 + ci, w:w + 1], hT[:, ci, t0:t1], ALU.mult, ALU.add)

    proj(self, XM, w_d, D, consumer, nblk=nblk)


FGROUPS = [(0, 4), (4, 8), (8, 12), (12, 16), (16, 20), (20, 22)]


def swiglu(self, l, xn, wg_d, wu_d, wd_d, nblk=5, gate_fn=None, wbufs=None):
    S = self.S
    hT = self.hT
    md = self.mod[l]
    wgb, wub, wdb, actb, sgb = wbufs
    for gi, (c0, c1) in enumerate(FGROUPS):
        nf = c1 - c0
        u = self.wuse
        self.wuse += 1
        wg, wu, wd = wgb[u % 2], wub[u % 2], wdb[u % 2]
        S.dma("pool", wg[:, :, 0:nf * 128], wg_d[:, c0 * 128:c1 * 128].rearrange("(kc p) n -> p kc n", p=128))
        S.dma("pool", wu[:, :, 0:nf * 128], wu_d[:, c0 * 128:c1 * 128].rearrange("(kc p) n -> p kc n", p=128))
        S.dma("pool", wd[:, 0:nf, :], wd_d[c0 * 128:c1 * 128, :].rearrange("(fc p) n -> p fc n", p=128))
        if gi == 0 and l == 0:
            self.tap("wgtap", wg[:], [128, KC, 512])
        def gu(bi, t0, t1):
            n = t1 - t0
            gv = gate_fn(bi, (t0, t1)) if gate_fn is not None else None
            ab = actb[self.ause % 2]
            self.ause += 1
            for fc in range(nf):
                pg = self.pb[self.puse % 2]
                pu = self.pb[2 + self.puse % 2]
                self.puse += 1
                for kc in range(KC):
                    S.mm(pg[:, :n], wg[:, kc, fc * 128:(fc + 1) * 128], xn[:, kc, t0:t1], start=(kc == 0), stop=(kc == KC - 1))
                for kc in range(KC):
                    S.mm(pu[:, :n], wu[:, kc, fc * 128:(fc + 1) * 128], xn[:, kc, t0:t1], start=(kc == 0), stop=(kc == KC - 1))
                sg = sgb[fc % 2]
                S.act(sg[:, :n], pg[:, :n], AF.Silu)
                if gv is not None:
                    S.tt("dve", sg[:, :n], sg[:, :n], gv, ALU.mult)
                S.tt("dve", ab[:, fc, :n], sg[:, :n], pu[:, :n], ALU.mult)
            return ab

        def down(ab, t0, t1):
            n = t1 - t0
            w = 1 if t0 < NCTX else 0
            for nc_ in range(KC):
                py = self.pb[4 + self.yuse % 4]
                self.yuse += 1
                for fc in range(nf):
                    S.mm(py[:, :n], wd[:, fc, nc_ * 128:(nc_ + 1) * 128], ab[:, fc, :n], start=(fc == 0), stop=(fc == nf - 1))
                S.stt("dve", hT[:, nc_, t0:t1], py[:, :n], md[:, 40 + nc_, w:w + 1], hT[:, nc_, t0:t1], ALU.mult, ALU.add)

        prev = None
        for bi, (t0, t1) in enumerate(BLKS[5 - nblk:]):
            ab = gu(bi, t0, t1)
            if prev is not None:
                down(*prev)
            prev = (ab, t0, t1)
        down(*prev)


def alloc_wbufs(self, es2):
    S = self.S
    wgb = [S.sb(es2, "wgb%d" % i, [128, KC, 512], BF16) for i in range(2)]
    wub = [S.sb(es2, "wub%d" % i, [128, KC, 512], BF16) for i in range(2)]
    wdb = [S.sb(es2, "wdb%d" % i, [128, 4, D], BF16) for i in range(2)]
    actb = [S.sb(es2, "actb%d" % i, [128, 4, 512], BF16) for i in range(2)]
    sgb = [S.sb(es2, "sgb%d" % i, [128, 512], F32) for i in range(2)]
    self.wuse = self.ause = self.puse = self.yuse = 0
    return wgb, wub, wdb, actb, sgb


def ffn0(self, XM):
    S = self.S
    modulate(self, 0, 1, XM)
    self.tap("xn1", XM[:], [128, KC, TT])
    with S.scope() as es2:
        wb = alloc_wbufs(self, es2)
        swiglu(self, 0, XM, self.din("e_ffn_w_gate", [1, D, DFF])[0], self.din("e_ffn_w_up", [1, D, DFF])[0],
               self.din("e_ffn_w_down", [1, DFF, D])[0], wbufs=wb)
        S.barrier()


O_CQ, O_CFF, O_CFB, O_CI, O_CG, O_DQ, O_DK, O_DV, O_DG, O_DLR = 0, 4, 8, 12, 16, 20, 22, 24, 28, 32


def inproj1(self, xn):
    S = self.S
    U1 = self.U1 = S.dram("U1", [33, 128, TT], F32)
    w_d = self.din("o_w_in", [1, D, O_IN])[0]
    with S.scope() as es2:
        buf = [S.sb(es2, "u1b%d" % i, [128, TT], F32) for i in range(3)]

        def consumer(ci, m, blk, pv):
            t0, t1 = blk
            p = buf[ci % 3]
            S.cp("act" if (t0 // 512) % 2 == 0 else "dve", p[0:m, t0:t1], pv)
            if t1 == TT:
                S.dma("sp", U1("c%d" % ci)[ci, 0:m, :], p[0:m, :])

        proj(self, xn, w_d, O_IN, consumer)


def gla_unit(self, XM, kind, idx, masks):
    S = self.S
    U1 = self.U1
    cst = self.cst
    NT = TT // 128
    nh = 1 if kind == "c" else 2
    mkf, mkb = masks
    with S.scope() as es2:
        vtm = S.sb(es2, "vtm", [128, NT, nh, 128], BF16)
        oacc = S.sb(es2, "oacc1", [128, nh, NLAT], F32)
        lbc = S.sb(es2, "lbc", [128, 4], F32)
        if kind == "c":
            S.tt("dve", lbc[:, 0:1], self.col("lb_logits", 4 + idx, 1), self.col("lb_logits", idx, 1), ALU.subtract)
            S.act(lbc[:, 0:1], lbc[:, 0:1], AF.Sigmoid)
            S.ts("dve", lbc[:, 1:2], lbc[:, 0:1], -1.0, 1.0, op0=ALU.mult, op1=ALU.add)
        else:
            for d in range(2):
                S.ts("dve", lbc[:, 2 + d:3 + d], self.col("d_gate_b2", d * 2 + idx, 1), -1.0, None, op0=ALU.mult)
        with S.scope() as es3:
            A = []
            for d in range(2):
                A.append(dict(
                    qdec=S.sb(es3, "qdec1", [128, TT], BF16), kinv=S.sb(es3, "kinv1", [128, TT], BF16),
                    ktm=S.sb(es3, "ktm", [128, NT, 128], BF16), dtot=S.sb(es3, "dtot", [128, TT // 64], F32)))
            with S.scope() as es4:
                T_ = []
                for d in range(2):
                    T_.append(dict(qb=S.sb(es4, "qb", [128, 512], F32), kb=S.sb(es4, "kb", [128, 512], F32),
                                   ld=S.sb(es4, "ld", [128, 512], F32), PF=S.sb(es4, "PF1", [128, 512], F32),
                                   SF=S.sb(es4, "SF1", [128, 512], F32), tmp=S.sb(es4, "tmp1", [128, 512], F32)))
                vb_ = [S.sb(es4, "vb1_0", [128, 512], F32)] * nh
                if kind == "d":
                    w2b = S.sb(es4, "w2b", [128, 2, 128], BF16)
                    dlrb = S.sb(es4, "dlrb", [128, TT], BF16)
                    with S.scope() as es5:
                        w2f = S.sb(es5, "w2f", [128, 2, 128], F32)
                        dlr = S.sb(es5, "dlr", [128, 384], F32)
                        S.memset("pool", w2f[:], 0.0)
                        w2_d = self.din("o_d_gate_w2", [1, 2, 16, 256])[0]
                        for d in range(2):
                            S.dma("sp", w2f[16 * d:16 * d + 16, d, :], w2_d[d, :, idx * 128:(idx + 1) * 128])
                        S.cp("dve", w2b[:], w2f[:])
                        S.memset("pool", dlr[:], 0.0)
                        for hf in range(6):
                            S.dma("sp", dlr[0:32, :], U1("c32")[32, 0:32, hf * 384:(hf + 1) * 384])
                            S.cp("dve", dlrb[:, hf * 384:(hf + 1) * 384], dlr[:])

                def prep_dir(d):
                    a_ = A[d]
                    t_ = T_[d]
                    qdec, kinv, ktm, dtot = a_["qdec"], a_["kinv"], a_["ktm"], a_["dtot"]
                    q_, k_, ld, PF, SF, tmp = t_["qb"], t_["kb"], t_["ld"], t_["PF"], t_["SF"], t_["tmp"]
                    pl = self.pb[d]
                    ptr = self.pb[2 + d]
                    for bi, (t0, t1) in enumerate(BLKS):
                        n = t1 - t0
                        if kind == "c":
                            S.dma("sp", q_[:, :n], U1("c%d" % (O_CQ + idx))[O_CQ + idx, :, t0:t1])
                            S.dma("sp", k_[:, :n], U1("c%d" % ((O_CFF if d == 0 else O_CFB) + idx))[(O_CFF if d == 0 else O_CFB) + idx, :, t0:t1])
                            yield
                            S.act(q_[:, :n], q_[:, :n], AF.Silu)
                            S.act(k_[:, :n], k_[:, :n], AF.Sigmoid)
                            yield
                            S.ts("dve", q_[:, :n], q_[:, :n], 128.0 ** -0.5, None, op0=ALU.mult)
                            S.ts("dve", k_[:, :n], k_[:, :n], lbc[:, 1:2], lbc[:, 0:1], op0=ALU.mult, op1=ALU.add)
                            yield
                            S.act(ld[:, :n], k_[:, :n], AF.Ln)
                            yield
                            S.ts("dve", k_[:, :n], k_[:, :n], -1.0, 1.0, op0=ALU.mult, op1=ALU.add)
                        else:
                            S.dma("sp", q_[:, :n], U1("c%d" % (O_DQ + idx))[O_DQ + idx, :, t0:t1])
                            S.dma("sp", k_[:, :n], U1("c%d" % (O_DK + idx))[O_DK + idx, :, t0:t1])
                            S.mm(pl[:, :n], w2b[:, d, :], dlrb[:, t0:t1])
                            yield
                            S.act(ld[:, :n], pl[:, :n], AF.Exp, bias=lbc[:, 2 + d:3 + d], scale=-1.0)
                            S.act(ld[:, :n], ld[:, :n], AF.Ln, bias=1.0)
                            yield
                            S.ts("dve", q_[:, :n], q_[:, :n], 64.0 ** -0.5, None, op0=ALU.mult)
                            S.ts("dve", ld[:, :n], ld[:, :n], -1.0 / 16.0, None, op0=ALU.mult)
                        S.scan(PF[:, :n], mkf[:, :n], ld[:, :n], 0.0)
                        S.scan(rev(SF, 0, n), rev(mkb, 0, n), rev(ld, 0, n), 0.0)
                        bc, oth = (PF, SF) if d == 0 else (SF, PF)
                        yield
                        S.act(dtot[:, t0 // 64:t1 // 64], V(PF.h[:, 63:n:64], (PF.name, None)), AF.Exp)
                        S.act(tmp[:, :n], bc[:, :n], AF.Exp)
                        yield
                        S.tt("dve", oth[:, :n], oth[:, :n], ld[:, :n], ALU.subtract)
                        S.tt("dve", qdec[:, t0:t1], q_[:, :n], tmp[:, :n], ALU.mult)
                        yield
                        S.act(tmp[:, :n], bc[:, :n], AF.Exp, scale=-1.0)
                        S.act(oth[:, :n], oth[:, :n], AF.Exp)
                        yield
                        S.tt("dve", kinv[:, t0:t1], k_[:, :n], tmp[:, :n], ALU.mult)
                        S.tt("dve", tmp[:, :n], k_[:, :n], oth[:, :n], ALU.mult)
                        yield
                        for cc in range(n // 128):
                            S.tr(ptr[:, cc * 128:(cc + 1) * 128], tmp[:, cc * 128:(cc + 1) * 128], self.ident)
                        yield
                        S.cp("act", ktm[:, t0 // 128:t1 // 128, :], V(ptr.h[:, 0:n].rearrange("p (c t) -> p c t", t=128), (ptr.name, None)))
                        yield
                        if d == 0:
                            for hh in range(nh):
                                v_ = vb_[hh]
                                vch = (O_CI + idx) if kind == "c" else (O_DV + idx * 2 + hh)
                                S.dma("sp", v_[:, :n], U1("c%d" % vch)[vch, :, t0:t1])
                                for cc in range(n // 128):
                                    S.tr(ptr[:, cc * 128:(cc + 1) * 128], v_[:, cc * 128:(cc + 1) * 128], self.ident)
                                yield
                                S.cp("act", vtm[:, t0 // 128:t1 // 128, hh, :], V(ptr.h[:, 0:n].rearrange("p (c t) -> p c t", t=128), (ptr.name, None)))
                                yield

                interleave(prep_dir(0), prep_dir(1))
            with S.scope() as es4:
                NP = TT // 64
                Uall = S.sb(es4, "Uall", [128, 128, NP], F32)
                Sball = S.sb(es4, "Sball", [128, NP, 128], BF16)
                Dq = S.sb(es4, "Dq", [128, 32, NP], F32)
                dperm = S.sb(es4, "dperm", [128, NP], F32)
                ATb = [S.sb(es4, "ATb%d" % i, [128, 128], BF16) for i in range(2)]
                for d in range(2):
                    a_ = A[d]
                    qdec, kinv, ktm, dtot = a_["qdec"], a_["kinv"], a_["ktm"], a_["dtot"]
                    mask = cst[:, 6, :] if d == 0 else cst[:, 7, :]
                    order_tiles = chunk_order(NCTX // 128, NT, d)
                    pos = {}
                    for si, ti in enumerate(order_tiles):
                        for q in range(2):
                            ci = q if d == 0 else 1 - q
                            pos[(ti, ci)] = 2 * si + q
                    if d == 0:
                        S.cp("pool", dperm[:], dtot[:])
                    else:
                        nc4 = NCTX // 64
                        S.cp("pool", dperm[:, 0:nc4], V(dtot.h[:, 0:nc4][:, ::-1], (dtot.name, None)))
                        S.cp("pool", dperm[:, nc4:NP], V(dtot.h[:, nc4:NP][:, ::-1], (dtot.name, None)))
                    S.cp("dve", Dq[:], V(dperm.h[:, :].unsqueeze(1).to_broadcast([128, 32, NP]), (dperm.name, None)))
                    S.memset("dve", Dq[:, :, 0:1], 0.0)
                    for hh in range(nh):
                        r0, r1 = (0, 128) if kind == "c" else (64 * hh, 64 * hh + 64)
                        for gi, g in enumerate(range(0, NT, 4)):
                            tiles = order_tiles[g:g + 4]
                            nt = len(tiles)
                            banks = [self.pb[(gi % 2) * 2], self.pb[(gi % 2) * 2 + 1]]
                            for j, ti in enumerate(tiles):
                                for q in range(2):
                                    ci = q if d == 0 else 1 - q
                                    S.mm(banks[ci][:, j * 128:(j + 1) * 128], ktm[ci * 64:ci * 64 + 64, ti, :], vtm[ci * 64:ci * 64 + 64, ti, hh, :])
                            for ci in range(2):
                                q = ci if d == 0 else 1 - ci
                                p0 = 2 * g + q
                                ov = V(Uall.h[:, :, p0:min(p0 + 2 * nt, NP):2], (Uall.name, None))
                                iv = V(banks[ci].h[:, 0:nt * 128].rearrange("p (c v) -> p v c", c=nt), (banks[ci].name, None))
                                S.cp("act" if ci == 0 else "dve", ov, iv)
                        for vq in range(4):
                            u2 = V(Uall.h[:, vq * 32:(vq + 1) * 32, :].rearrange("p v c -> p (v c)"), (Uall.name, None))
                            d2 = V(Dq.h[:, :, :].rearrange("p v c -> p (v c)"), (Dq.name, None))
                            S.scan(u2, d2, u2, 0.0)
                        for half in range(2):
                            ov = Sball[:, half * 18:(half + 1) * 18, :]
                            iv = V(Uall.h[:, :, half * 18:(half + 1) * 18].rearrange("p v c -> p c v"), (Uall.name, None))
                            S.cp("act" if half == 0 else "dve", ov, iv)
                        for si, ti in enumerate(order_tiles):
                            if ti < NCTX // 128:
                                continue
                            ta = ti * 128
                            la = ta - NCTX
                            pA = self.pb[4 + 3 * (si % 2)]
                            pO = self.pb[5 + si % 2]
                            AT = ATb[si % 2]
                            S.mm(pA[:, 0:128], kinv[r0:r1, ta:ta + 128], qdec[r0:r1, ta:ta + 128])
                            S.tt("dve", AT[:], pA[:, 0:128], mask, ALU.mult)
                            S.mm(pO[:, 0:128], vtm[:, ti, hh, :], AT[:], start=True, stop=False)
                            for q in range(2):
                                ci = q if d == 0 else 1 - q
                                p = pos[(ti, ci)]
                                ca = ta + ci * 64
                                S.mm(pO[:, ci * 64:ci * 64 + 64], Sball[r0:r1, p - 1, :], qdec[r0:r1, ca:ca + 64], start=False, stop=(q == 1))
                            if d == 0:
                                S.cp("act", oacc[:, hh, la:la + 128], pO[:, 0:128])
                            else:
                                S.tt("dve", oacc[:, hh, la:la + 128], oacc[:, hh, la:la + 128], pO[:, 0:128], ALU.add)
                self.ck("g_rec")
        with S.scope() as es3:
            zb = [S.sb(es3, "zb1%d" % i, [128, 512], F32) for i in range(2)]
            sqb = S.sb(es3, "gsq1", [128, 512], BF16)
            rs = S.sb(es3, "grs1", [128, 512], F32)
            for hh in range(nh):
                if kind == "c":
                    gch, xch, gname = O_CG + idx, idx, "c_norm_g"
                else:
                    gch, xch, gname = O_DG + idx * 2 + hh, 4 + idx * 2 + hh, "d_norm_g"
                if hh == 0:
                    self.tap("oc0" if kind == "c" else "od0", oacc[:, 0, :], [128, NLAT])
                for bi, (t0, t1) in enumerate(BLKS[1:]):
                    n = t1 - t0
                    la = t0 - NCTX
                    z_ = zb[bi % 2]
                    S.dma("sp", z_[:, :n], U1("c%d" % gch)[gch, :, t0:t1])
                    S.act(sqb[:, :n], oacc[:, hh, la:la + n], AF.Square)
                    p0 = self.pb[0]
                    S.mm(p0[:, :n], self.onesb, sqb[:, :n])
                    S.ts("dve", rs[:, :n], p0[:, :n], 1.0 / 128.0, EPS, op0=ALU.mult, op1=ALU.add)
                    S.act(rs[:, :n], rs[:, :n], AF.Sqrt)
                    S.recip(rs[:, :n], rs[:, :n])
                    S.act(z_[:, :n], z_[:, :n], AF.Silu)
                    S.stt("dve", rs[:, :n], oacc[:, hh, la:la + n], self.col(gname, 0, 1), rs[:, :n], ALU.mult, ALU.mult)
                    S.tt("dve", XM[:, xch, t0:t1], rs[:, :n], z_[:, :n], ALU.mult)


def mixer1(self, XM):
    S = self.S
    with S.scope() as es2:
        mkf = S.sb(es2, "mkf", [128, 512], F32)
        mkb = S.sb(es2, "mkb", [128, 512], F32)
        S.memset("pool", mkf[:], 1.0)
        S.memset("pool", V(mkf.h[:, 0:512:64], (mkf.name, None)), 0.0)
        S.memset("pool", mkb[:], 1.0)
        S.memset("pool", V(mkb.h[:, 63:512:64], (mkb.name, None)), 0.0)
        for h in range(4):
            gla_unit(self, XM, "c", h, (mkf, mkb))
        for hp in range(2):
            gla_unit(self, XM, "d", hp, (mkf, mkb))


AX = mybir.AxisListType


def moe(self, XM):
    S = self.S
    hT = self.hT
    md = self.mod[1]
    NTL = NLAT // 128
    with S.scope() as es2:
        gtm = S.sb(es2, "gtm", [128, NTL, NEXP], F32)
        rw = S.sb(es2, "rw", [128, KC, NEXP], F32)
        rb = S.sb(es2, "rb", [128, NEXP], F32)
        S.dma("sp", rw[:], self.din("o_router_w", [1, D, NEXP])[0].rearrange("(kc p) e -> p kc e", p=128))
        S.dma("sp", rb[:], self.din("rb128", [128, NEXP])[:, :])
        g0 = 3
        with S.scope() as es3:
            gs = S.sb(es3, "gs2", [128, KC], F32)
            sq = [S.sb(es3, "sq2%d" % i, [128, 512], BF16) for i in range(2)]
            rsb = S.sb(es3, "rsb2", [128, 512], F32)
            tmp = [S.sb(es3, "mtmp2%d" % i, [128, 512], F32) for i in range(2)]
            xr = S.sb(es3, "xr", [128, KC, 512], F32)
            sm = S.sb(es3, "sm", [128, 8, NEXP], F32)
            sc = S.sb(es3, "smc", [128, 8], F32)
            S.ts("dve", gs[:], md[:, (g0 + 1) * 8:(g0 + 2) * 8, 0], 1.0, 32.0, op0=ALU.add, op1=ALU.mult)
            S.tt("dve", gs[:], gs[:], self.col("nffn1", 0, 8), ALU.mult)
            for (t0, t1) in BLKS[1:]:
                n = t1 - t0
                ss = self.pb[0]
                for kc in range(KC):
                    s = sq[kc % 2]
                    S.act(s[:, :n], hT[:, kc, t0:t1], AF.Square)
                    S.mm(ss[:, :n], self.onesb, s[:, :n], start=(kc == 0), stop=(kc == KC - 1))
                S.ts("dve", rsb[:, :n], ss[:, :n], 1024.0 * EPS, None, op0=ALU.add)
                S.act(rsb[:, :n], rsb[:, :n], AF.Sqrt)
                S.recip(rsb[:, :n], rsb[:, :n])
                for kc in range(KC):
                    tm = tmp[kc % 2]
                    S.tt("dve", tm[:, :n], hT[:, kc, t0:t1], rsb[:, :n], ALU.mult)
                    S.act(XM[:, kc, t0:t1], tm[:, :n], AF.Identity, bias=md[:, g0 * 8 + kc, 0:1], scale=gs[:, kc:kc + 1])
                    S.ts("dve", xr[:, kc, :n], tm[:, :n], gs[:, kc:kc + 1], md[:, g0 * 8 + kc, 0:1], op0=ALU.mult, op1=ALU.add)
                for q in range(4):
                    ti = (t0 - NCTX) // 128 + q
                    pl = self.pb[1]
                    for kc in range(KC):
                        S.mm(pl[:, 0:NEXP], xr[:, kc, q * 128:(q + 1) * 128], rw[:, kc, :], start=(kc == 0), stop=(kc == KC - 1))
                    lg, eq, l2, e_ = sm[:, 0, :], sm[:, 1, :], sm[:, 2, :], sm[:, 3, :]
                    m1, m2, nm1, dd = sc[:, 0:1], sc[:, 1:2], sc[:, 2:3], sc[:, 3:4]
                    S.tt("dve", lg, pl[:, 0:NEXP], rb[:], ALU.add)
                    S.op("dve", lambda: self.nc.vector.reduce_max(out=_ap(m1), in_=_ap(lg), axis=AX.X), [lg], [m1])
                    S.ts("dve", eq, lg, m1, None, op0=ALU.is_equal)
                    S.stt("dve", l2, eq, -1e30, lg, ALU.mult, ALU.add)
                    S.op("dve", lambda: self.nc.vector.reduce_max(out=_ap(m2), in_=_ap(l2), axis=AX.X), [l2], [m2])
                    S.ts("dve", eq, lg, m2, None, op0=ALU.is_ge)
                    S.ts("dve", nm1, m1, -1.0, None, op0=ALU.mult)
                    S.act(e_, lg, AF.Exp, bias=nm1)
                    S.tt("dve", dd, m2, m1, ALU.subtract)
                    S.act(dd, dd, AF.Exp)
                    S.ts("dve", dd, dd, 1.0, None, op0=ALU.add)
                    S.recip(dd, dd)
                    S.tt("dve", e_, e_, eq, ALU.mult)
                    S.ts("dve", gtm[:, ti, :], e_, dd, None, op0=ALU.mult)
        self.tap("gates", gtm[:], [128, NTL, NEXP])
        with S.scope() as es3:
            wb = alloc_wbufs(self, es3)
            Gb = S.sb(es3, "Gb", [128, NLAT], F32)
            dg = [S.sb(es3, "dg%d" % i, [128, 128], F32) for i in range(2)]
            wg_d = self.din("o_moe_w_gate", [1, NEXP, D, DFF])[0]
            wu_d = self.din("o_moe_w_up", [1, NEXP, D, DFF])[0]
            wd_d = self.din("o_moe_w_down", [1, NEXP, DFF, D])[0]
            for e in range(NEXP):
                if ("moe_e%d" % e) in self.stages:
                    continue
                for b4 in range(4):
                    pG = self.pb[6 + b4 % 2]
                    for q in range(4):
                        ti = b4 * 4 + q
                        dq_ = dg[q % 2]
                        S.ts("dve", dq_[:], self.cst[:, 0, :], gtm[:, ti, e:e + 1], None, op0=ALU.mult)
                        S.mm(pG[:, q * 128:(q + 1) * 128], self.cst[:, 1, :], dq_[:])
                    S.cp("act", Gb[:, b4 * 512:(b4 + 1) * 512], pG[:, :])
                swiglu(self, 1, XM, wg_d[e], wu_d[e], wd_d[e], nblk=4, wbufs=wb,
                       gate_fn=lambda bi, blk: Gb[:, blk[0] - NCTX:blk[1] - NCTX])


def final(self):
    S = self.S
    hT = self.hT
    out_d = self.nc.dram_tensor("out", [NLAT, D], F32, kind="ExternalOutput").ap()
    with S.scope() as es2:
        g32 = S.sb(es2, "g32", [128, KC], F32)
        sq = [S.sb(es2, "sqf%d" % i, [128, 512], BF16) for i in range(2)]
        rsb = S.sb(es2, "rsbf", [128, 512], F32)
        yT = [S.sb(es2, "yT%d" % i, [128, 512], F32) for i in range(2)]
        ot = [S.sb(es2, "ot%d" % i, [128, D], F32) for i in range(4)]
        S.ts("dve", g32[:], self.col("nfin", 0, 8), 32.0, None, op0=ALU.mult)
        for bi, (t0, t1) in enumerate(BLKS[1:]):
            n = t1 - t0
            ss = self.pb[0]
            for kc in range(KC):
                s = sq[kc % 2]
                S.act(s[:, :n], hT[:, kc, t0:t1], AF.Square)
                S.mm(ss[:, :n], self.onesb, s[:, :n], start=(kc == 0), stop=(kc == KC - 1))
            S.ts("dve", rsb[:, :n], ss[:, :n], 1024.0 * EPS, None, op0=ALU.add)
            S.act(rsb[:, :n], rsb[:, :n], AF.Sqrt)
            S.recip(rsb[:, :n], rsb[:, :n])
            for kc in range(KC):
                y = yT[kc % 2]
                S.stt("dve", y[:, :n], hT[:, kc, t0:t1], g32[:, kc:kc + 1], rsb[:, :n], ALU.mult, ALU.mult)
                pt = self.pb[2 + kc % 4]
                for q in range(4):
                    S.tr(pt[:, q * 128:(q + 1) * 128], y[:, q * 128:(q + 1) * 128], self.ident)
                for q in range(4):
                    S.cp("act" if q % 2 == 0 else "dve", ot[q][:, kc * 128:(kc + 1) * 128], pt[:, q * 128:(q + 1) * 128])
            for q in range(4):
                r0 = t0 - NCTX + q * 128
                tok = S.dma("sp", out_d[r0:r0 + 128, :], ot[q][:])
                S.out_tokens.append(tok)


Prog.final = final


ALL_STAGES = ["L0", "inproj0", "rglru", "deltanet", "outproj", "ffn0", "L1", "FIN"]
_CACHE = {}


def kernel(**inputs):
    inp = {k: np.asarray(v) for k, v in inputs.items()}
    if "prog" not in _CACHE:
        P = Prog(ALL_STAGES)
        P.build()
        _CACHE["prog"] = P
    P = _CACHE["prog"]
    B = inp["x"].shape[0]
    maps = [in_map(inp, b, P.inputs) for b in range(B)]
    res = run_bass_kernel_spmd(P.nc, maps, core_ids=list(range(B)))
    return np.stack([np.asarray(res.results[b]["out"]) for b in range(B)], 0).astype(np.float32)
```

```python
import numpy as np
from contextlib import ExitStack
import concourse.bass as bass
import concourse.mybir as mybir
from concourse.bass_utils import run_bass_kernel_spmd

F32 = mybir.dt.float32
F32R = mybir.dt.float32r
BF16 = mybir.dt.bfloat16
AF = mybir.ActivationFunctionType
ALU = mybir.AluOpType

D = 1024
KC = 8
NCTX = 256
NLAT = 2048
TT = NCTX + NLAT
EPS = 1e-6
E_IN = 3088
O_IN = 4128
DFF = 2816
FC = DFF // 128
NEXP = 8
BLKS = [(0, 256), (256, 768), (768, 1280), (1280, 1792), (1792, 2304)]


class V:
    __slots__ = ("ap", "dep")

    def __init__(self, ap, dep):
        self.ap = ap
        self.dep = dep


class _Idx:
    def __init__(self, t, key):
        self.t = t
        self.key = key

    def __getitem__(self, sl):
        return V(self.t.h[sl], (self.t.name, self.key))


class TT_:
    def __init__(self, h, name):
        self.h = h
        self.name = name

    def __getitem__(self, sl):
        return V(self.h[sl], (self.name, None))

    def __call__(self, key):
        return _Idx(self, key)


def _ap(x):
    return x.ap if isinstance(x, V) else x


def r32v(v):
    return V(v.ap.bitcast(F32R), v.dep) if R32 else v


class Sched:
    ENG = ("pe", "dve", "act", "pool", "sp")
    NDS = 24

    def __init__(self, nc, es):
        self.nc = nc
        self.es = es
        self.eng = dict(pe=nc.tensor, dve=nc.vector, act=nc.scalar, pool=nc.gpsimd, sp=nc.sync)
        self.sem = {e: es.enter_context(nc.semaphore("s_" + e)) for e in self.ENG}
        self.cnt = {e: 0 for e in self.ENG}
        self.seen = {e: {} for e in self.ENG}
        self.dsem = [es.enter_context(nc.semaphore("d%d" % i)) for i in range(self.NDS)]
        self.dval = [0] * self.NDS
        self.dnext = 0
        self.state = {}
        self.uid = 0
        self.out_tokens = []
        self.excl = set()

    def sb(self, es, name, shape, dt):
        self.uid += 1
        nm = "%s_%d" % (name, self.uid)
        return TT_(es.enter_context(self.nc.sbuf_tensor(nm, list(shape), dt)), nm)

    def ps(self, es, name, shape, dt=F32):
        self.uid += 1
        nm = "%s_%d" % (name, self.uid)
        self.excl.add(nm)
        return TT_(es.enter_context(self.nc.psum_tensor(nm, list(shape), dt)), nm)

    def dram(self, name, shape, dt, kind="Internal"):
        t = self.nc.dram_tensor(name, list(shape), dt, kind=kind)
        return TT_(t.ap(), name)

    def _entries(self, dep):
        tid, key = dep
        st = self.state.setdefault(tid, {})
        if None not in st:
            st[None] = [None, {}]
        if key is None:
            return list(st.values())
        if key not in st:
            st[key] = [None, {}]
        return [st[key], st[None]]

    def _wait(self, e, tok):
        if tok is None:
            return
        k, v = tok
        if k == e and e in ("pe", "sp"):
            return
        if self.seen[e].get(k, 0) >= v:
            return
        sem = self.sem[k] if isinstance(k, str) else self.dsem[k[1]]
        self.eng[e].wait_ge(sem, v)
        self.seen[e][k] = v

    def _pre(self, e, R, W):
        for v in R:
            if isinstance(v, V):
                for ent in self._entries(v.dep):
                    self._wait(e, ent[0])
        for v in W:
            if isinstance(v, V):
                for ent in self._entries(v.dep):
                    self._wait(e, ent[0])
                    for k, val in ent[1].items():
                        self._wait(e, (k, val))

    def _post(self, tok, R, W):
        k, val = tok
        for v in R:
            if isinstance(v, V):
                tid, key = v.dep
                ents = self._entries(v.dep)
                tgt = ents if key is None else ents[:1]
                for ent in tgt:
                    if ent[1].get(k, 0) < val:
                        ent[1][k] = val
        for v in W:
            if isinstance(v, V):
                tid, key = v.dep
                ents = self._entries(v.dep)
                tgt = ents if key is None else ents[:1]
                for ent in tgt:
                    ent[0] = tok
                    ent[1] = {}

    dead = False
    last_pool_dma = None

    def op(self, e, fn, R, W):
        if self.dead:
            return
        Rx = [v for v in R if isinstance(v, V) and v.dep[0] in self.excl]
        if Rx:
            R = [v for v in R if not (isinstance(v, V) and v.dep[0] in self.excl)]
            W = list(W) + Rx
        self._pre(e, R, W)
        ins = fn()
        self.cnt[e] += 1
        ins.then_inc(self.sem[e], 1)
        self._post((e, self.cnt[e]), R, W)

    def dma(self, q, out, in_, **kw):
        if self.dead:
            return None
        i = self.dnext
        self.dnext = (i + 1) % self.NDS
        if self.dval[i] > 0:
            self._wait(q, (("d", i), self.dval[i]))
        if q == "pool" and self.last_pool_dma is not None:
            self._wait(q, self.last_pool_dma)
        self._pre(q, [in_], [out])
        ins = self.eng[q].dma_start(out=_ap(out), in_=_ap(in_), **kw)
        self.dval[i] += 16
        ins.then_inc(self.dsem[i], 16)
        tok = (("d", i), self.dval[i])
        if q == "pool":
            self.last_pool_dma = tok
        self._post(tok, [in_], [out])
        return tok

    def scope(self):
        sched = self

        class _Scope(ExitStack):
            def __exit__(self, *a):
                sched.barrier()
                return super().__exit__(*a)

        return _Scope()

    def barrier(self):
        for e in self.ENG:
            for f in self.ENG:
                if f != e and self.cnt[f] > 0:
                    self._wait(e, (f, self.cnt[f]))
            for i in range(self.NDS):
                if self.dval[i] > 0:
                    self._wait(e, (("d", i), self.dval[i]))

    def mm(self, out, lhsT, rhs, start=True, stop=True, r32=False):
        if r32:
            self.op("pe", lambda: self.nc.tensor.matmul(_ap(out), lhsT=_ap(lhsT).bitcast(F32R), rhs=_ap(rhs).bitcast(F32R), start=start, stop=stop),
                    [lhsT, rhs], [out])
            return
        self.op("pe", lambda: self.nc.tensor.matmul(_ap(out), lhsT=_ap(lhsT), rhs=_ap(rhs), start=start, stop=stop),
                [lhsT, rhs], [out])

    def tr(self, out, in_, ident):
        self.op("pe", lambda: self.nc.tensor.transpose(_ap(out), _ap(in_), _ap(ident)), [in_, ident], [out])

    def act(self, out, in_, func, bias=None, scale=None, accum=None):
        kw = {}
        R = [in_]
        if bias is not None:
            kw["bias"] = _ap(bias)
            R.append(bias)
        if scale is not None:
            kw["scale"] = _ap(scale)
            R.append(scale)
        W = [out]
        if accum is not None:
            kw["accum_out"] = _ap(accum)
            W.append(accum)
        self.op("act", lambda: self.nc.scalar.activation(out=_ap(out), in_=_ap(in_), func=func, **kw), R, W)

    def tt(self, e, out, in0, in1, op):
        self.op(e, lambda: self.eng[e].tensor_tensor(out=_ap(out), in0=_ap(in0), in1=_ap(in1), op=op), [in0, in1], [out])

    def ts(self, e, out, in0, s1, s2=None, op0=ALU.mult, op1=None, accum=None):
        kw = {}
        if op1 is not None:
            kw["op1"] = op1
        W = [out]
        if accum is not None:
            kw["accum_out"] = _ap(accum)
            W.append(accum)
        self.op(e, lambda: self.eng[e].tensor_scalar(out=_ap(out), in0=_ap(in0), scalar1=_ap(s1), scalar2=_ap(s2), op0=op0, **kw),
                [in0, s1, s2], W)

    def stt(self, e, out, in0, scalar, in1, op0, op1):
        self.op(e, lambda: self.eng[e].scalar_tensor_tensor(out=_ap(out), in0=_ap(in0), scalar=_ap(scalar), in1=_ap(in1), op0=op0, op1=op1),
                [in0, scalar, in1], [out])

    def cp(self, e, out, in_):
        if e == "act":
            self.op(e, lambda: self.nc.scalar.copy(out=_ap(out), in_=_ap(in_)), [in_], [out])
        else:
            self.op(e, lambda: self.eng[e].tensor_copy(out=_ap(out), in_=_ap(in_)), [in_], [out])

    def scan(self, out, d0, d1, init, op0=ALU.mult, op1=ALU.add):
        self.op("dve", lambda: self.nc.vector.tensor_tensor_scan(out=_ap(out), data0=_ap(d0), data1=_ap(d1), initial=_ap(init), op0=op0, op1=op1),
                [d0, d1, init], [out])

    def memset(self, e, out, val):
        self.op(e, lambda: self.eng[e].memset(_ap(out), val), [], [out])

    def recip(self, out, in_):
        self.op("dve", lambda: self.nc.vector.reciprocal(out=_ap(out), in_=_ap(in_)), [in_], [out])

    def iota_like_select(self, out, in_, pattern, cmp, fill, base, cm):
        self.op("pool", lambda: self.nc.gpsimd.affine_select(out=_ap(out), in_=_ap(in_), pattern=pattern, compare_op=cmp, fill=fill,
                                                              base=base, channel_multiplier=cm), [in_], [out])


def _consts():
    j = np.arange(128)[:, None]
    i = np.arange(128)[None, :]
    c = np.zeros((128, 8, 128), np.float32)
    c[:, 0] = (j == i)
    c[:, 1] = 1.0
    c[:, 2] = (j <= i)
    c[:, 3] = (j < i)
    c[:, 4] = (j >= i)
    c[:, 5] = (j > i)
    same = (j // 64) == (i // 64)
    c[:, 6] = same & (j <= i)
    c[:, 7] = same & (j >= i)
    return c


PROWS = {}


def _pack_rows(inp, b):
    parts = []
    off = [0]

    def add(name, arr):
        a = np.ascontiguousarray(arr, dtype=np.float32).reshape(-1, 128)
        PROWS[name] = (off[0], a.shape[0])
        off[0] += a.shape[0]
        parts.append(a)

    add("c", inp["c"][b])
    add("c_ctx", inp["c_ctx"])
    add("ada_b0", inp["ada_b"][0])
    add("ada_b1", inp["ada_b"][1])
    add("nmix0", inp["norm_mix_g"][0])
    add("nmix1", inp["norm_mix_g"][1])
    add("nffn0", inp["norm_ffn_g"][0])
    add("nffn1", inp["norm_ffn_g"][1])
    add("nfin", inp["final_norm_g"])
    add("a_conv_w", inp["e_a_conv_w"][0])
    add("a_conv_b", inp["e_a_conv_b"][0])
    add("a_gate_b", inp["e_a_gate_b"][0])
    add("a_lambda", inp["e_a_lambda"][0])
    add("b_conv_w", inp["e_b_conv_w"][0])
    add("b_norm_g", inp["e_b_norm_g"][0])
    add("lb_logits", inp["o_lb_logits"])
    add("c_norm_g", inp["o_c_norm_g"][0])
    add("d_gate_b2", inp["o_d_gate_b2"][0])
    add("d_norm_g", inp["o_d_norm_g"][0])
    rows = np.concatenate(parts, 0)
    pad = (-rows.shape[0]) % 128
    if pad:
        rows = np.concatenate([rows, np.zeros((pad, 128), np.float32)], 0)
    return rows


NPROW = 384
R32 = False
DBL_DT = F32
PROWS_STATIC = {'c': (0, 8), 'c_ctx': (8, 8), 'ada_b0': (16, 48), 'ada_b1': (64, 48), 'nmix0': (112, 8), 'nmix1': (120, 8),
                'nffn0': (128, 8), 'nffn1': (136, 8), 'nfin': (144, 8), 'a_conv_w': (152, 16), 'a_conv_b': (168, 4),
                'a_gate_b': (172, 16), 'a_lambda': (188, 8), 'b_conv_w': (196, 48), 'b_norm_g': (244, 1), 'lb_logits': (245, 8),
                'c_norm_g': (253, 1), 'd_gate_b2': (254, 4), 'd_norm_g': (258, 1)}


TAP_SHAPES = {"wgtap": [128, KC, 512], "h0": [128, KC, TT], "mod0": [128, 48, 2], "mod1": [128, 48, 2], "xn0": [128, KC, TT], "rsb": [128, 512],
              "gs": [128, KC, 2], "ha0": [128, TT], "ha3": [128, TT], "ob0": [128, TT], "ob3": [128, TT], "dnrows": [128, TT],
              "dngtb": [128, 8, 18], "dnG": [64, TT], "hmid0": [128, KC, TT], "h1": [128, KC, TT], "xn1": [128, KC, TT],
              "hmid1": [128, KC, TT], "oc0": [128, NLAT], "od0": [128, NLAT], "h2": [128, KC, TT], "gates": [128, 16, 8]}


class _Stop(Exception):
    pass


class Prog:
    cut = None

    ckn = {}

    def ck(self, name):
        k = self.ckn.get(name, 0)
        self.ckn[name] = k + 1
        if self.cut == "%s#%d" % (name, k):
            self.S.dead = True

    def __init__(self, stages, taps=()):
        self.stages = stages
        self.taps = set(taps)
        self.nc = bass.Bass("TRN2", target_bir_lowering=False)
        self.inputs = {}
        self.tap_out = {}

    def din(self, name, shape, dt=F32):
        if name not in self.inputs:
            self.inputs[name] = self.nc.dram_tensor(name, list(shape), dt, kind="ExternalInput").ap()
        return self.inputs[name]

    def tap(self, name, src, shape):
        if name not in self.taps:
            return
        S = self.S
        t = self.tap_out[name]
        q = "sp" if _ap(src).tensor.dtype == F32 else "pool"
        tok = S.dma(q, t, src)
        S.out_tokens.append(tok)

    def build(self):
        nc = self.nc
        with ExitStack() as es:
            S = self.S = Sched(nc, es)
            self.es = es
            for name in sorted(self.taps):
                self.tap_out[name] = nc.dram_tensor("tap_" + name, list(TAP_SHAPES[name]), F32, kind="ExternalOutput").ap()
            try:
                self.setup()
                for l in range(2):
                    if ("L%d" % l) not in self.stages:
                        continue
                    self.layer(l)
                if "FIN" in self.stages:
                    self.final()
            except _Stop:
                pass
            S.barrier()
        return nc

    def setup(self):
        S, nc, es = self.S, self.nc, self.es
        self.pb = [S.ps(es, "pb%d" % i, [128, 512]) for i in range(8)]
        cst = self.cst = S.sb(es, "cst", [128, 8, 128], F32)
        S.dma("sp", cst[:], self.din("consts", [128, 8, 128])[:, :, :])
        cstb = self.cstb = S.sb(es, "cstb", [128, 8, 128], BF16)
        S.cp("dve", cstb[:], cst[:])
        self.ident = cst[:, 0, :]
        ncst = self.ncst = S.sb(es, "ncst", [128, 2, 128], F32)
        S.ts("dve", ncst[:, 0, :], cst[:, 3, :], -1.0, None, op0=ALU.mult)
        S.ts("dve", ncst[:, 1, :], cst[:, 5, :], -1.0, None, op0=ALU.mult)
        self.identb = cstb[:, 0, :]
        self.onesb = cstb[:, 1, :]
        pc = self.pc = S.sb(es, "pc", [128, NPROW], F32)
        prow_d = self.din("prows", [NPROW, 128])
        with S.scope() as es2:
            stg = S.sb(es2, "pstg", [128, NPROW // 128, 128], F32)
            for g in range(NPROW // 128):
                S.dma("sp", stg[:, g, :], prow_d[g * 128:(g + 1) * 128, :])
                pt = self.pb[g % 2]
                S.tr(pt[:, 0:128], stg[:, g, :], self.ident)
                S.cp("dve", pc[:, g * 128:(g + 1) * 128], pt[:, 0:128])
            S.barrier()
        hT = self.hT = S.sb(es, "hT", [128, KC, TT], F32)
        x_d = self.din("x", [NLAT, D])
        ctx_d = self.din("ctx", [NCTX, D])
        with S.scope() as es2:
            xin = [S.sb(es2, "xin%d" % i, [128, D], F32) for i in range(2)]
            for ti in range(TT // 128):
                t0 = ti * 128
                src = ctx_d[t0:t0 + 128, :] if t0 < NCTX else x_d[t0 - NCTX:t0 - NCTX + 128, :]
                xt = xin[ti % 2]
                S.dma("sp", xt[:], src)
                for half in range(2):
                    pt = self.pb[(ti * 2 + half) % 4]
                    for q in range(4):
                        kc = half * 4 + q
                        S.tr(pt[:, q * 128:(q + 1) * 128], xt[:, kc * 128:(kc + 1) * 128], self.ident)
                    S.cp("dve" if half == 0 else "act", hT("t%d" % ti)[:, half * 4:half * 4 + 4, t0:t0 + 128],
                         V(pt.h[:].rearrange("p (q t) -> p q t", q=4), (pt.name, None)))
            S.barrier()
        self.tap("h0", hT[:], [128, KC, TT])
        self.mods()

    def col(self, name, r=0, n=1):
        off, cnt = PROWS_STATIC[name]
        return self.pc[:, off + r:off + r + n]

    def mods(self):
        S, nc, es = self.S, self.nc, self.es
        sT = self.sT = S.sb(es, "sT", [128, KC, 2], F32)
        S.act(sT[:, :, 0], self.col("c", 0, 8), AF.Silu)
        S.act(sT[:, :, 1], self.col("c_ctx", 0, 8), AF.Silu)
        self.mod = [S.sb(es, "mod%d" % l, [128, 48, 2], F32) for l in range(2)]
        with S.scope() as es2:
            wb = [S.sb(es2, "adaw%d" % i, [128, KC, 256], F32) for i in range(2)]
            interleave(self.mods_gen(0, wb))
            if "rglru" not in self.stages:
                interleave(self.mods_gen(1, wb))
        for l in range(2):
            self.tap("mod%d" % l, self.mod[l][:], [128, 48, 2])

    def mods_gen(self, l, wb):
        S = self.S
        ada_d = self.din("ada_w", [2, D, 6 * D])
        md = self.mod[l]
        sT = self.sT
        for g in range(24):
            w = wb[g % 2]
            S.dma("sp", w[:], ada_d[l, :, g * 256:(g + 1) * 256].rearrange("(kc p) n -> p kc n", p=128))
            pt = self.pb[6 + (g % 2)]
            for q in range(2):
                for kc in range(KC):
                    S.mm(pt[:, q * 2:q * 2 + 2], w[:, kc, q * 128:(q + 1) * 128], sT[:, kc, :], start=(kc == 0), stop=(kc == KC - 1))
            yield
            for q in range(2):
                j = g * 2 + q
                S.ts("dve", md[:, j, :], pt[:, q * 2:q * 2 + 2], self.col("ada_b%d" % l, j, 1), None, op0=ALU.add)
            yield


def in_map(inp, b, needed):
    m = {}
    for name in needed:
        if name == "consts":
            m[name] = _consts()
        elif name == "prows":
            m[name] = _pack_rows(inp, b)
        elif name == "x":
            m[name] = np.ascontiguousarray(inp["x"][b])
        elif name == "ctx":
            m[name] = np.ascontiguousarray(inp["ctx"][b])
        elif name == "rb128":
            m[name] = np.ascontiguousarray(np.broadcast_to(inp["o_router_b"][0][None, :], (128, NEXP)))
        elif name == "dn_sc":
            m[name] = np.ascontiguousarray(np.stack([inp["e_b_a_log"][0].reshape(8), inp["e_b_dt_bias"][0].reshape(8)], 1))
        else:
            a = inp[name]
            m[name] = np.ascontiguousarray(a)
    return m


def _layer_methods():
    pass


def modulate(self, l, which, xn, perm=False, nblk=5):
    S, es = self.S, self.es
    hT = self.hT
    g0 = 0 if which == 0 else 3
    gname = ("nmix%d" if which == 0 else "nffn%d") % l
    md = self.mod[l]
    with S.scope() as es2:
        gs = S.sb(es2, "gs", [128, KC, 2], F32)
        sq = [S.sb(es2, "sq%d" % i, [128, 512], BF16) for i in range(2)]
        rsb = S.sb(es2, "rsb", [128, 512], F32)
        tmp = [S.sb(es2, "mtmp%d" % i, [128, 512], F32) for i in range(2)]
        for w in range(2):
            S.ts("dve", gs[:, :, w], md[:, (g0 + 1) * 8:(g0 + 2) * 8, w], 1.0, 32.0, op0=ALU.add, op1=ALU.mult)
            S.tt("dve", gs[:, :, w], gs[:, :, w], self.col(gname, 0, 8), ALU.mult)
        for (t0, t1) in BLKS[5 - nblk:]:
            w = 1 if t0 < NCTX else 0
            n = t1 - t0
            ss = self.pb[0]
            for kc in range(KC):
                s = sq[kc % 2]
                S.act(s[:, :n], hT[:, kc, t0:t1], AF.Square)
                S.mm(ss[:, :n], self.onesb, s[:, :n], start=(kc == 0), stop=(kc == KC - 1))
            S.ts("dve", rsb[:, :n], ss[:, :n], 1024.0 * EPS, None, op0=ALU.add)
            S.act(rsb[:, :n], rsb[:, :n], AF.Sqrt)
            S.recip(rsb[:, :n], rsb[:, :n])
            if t1 == TT:
                self.tap("rsb", rsb[:], [128, 512])
                self.tap("gs", gs[:], [128, KC, 2])
            for kc in range(KC):
                tm = tmp[kc % 2]
                S.tt("dve", tm[:, :n], hT[:, kc, t0:t1], rsb[:, :n], ALU.mult)
                if perm and t0 >= NCTX:
                    r0 = (t0 - NCTX) // 64
                    r1 = (t1 - NCTX) // 64
                    o = V(xn.h[:, kc, NCTX:TT].rearrange("p (c r) -> p r c", c=64, r=32)[:, r0:r1, :], (xn.name, None))
                    i_ = V(tm.h[:, :n].rearrange("p (r c) -> p r c", c=64), (tm.name, None))
                else:
                    o = xn[:, kc, t0:t1]
                    i_ = tm[:, :n]
                S.act(o, i_, AF.Identity, bias=md[:, g0 * 8 + kc, w:w + 1], scale=gs[:, kc, w:w + 1])


def proj(self, xn, w_d, N, consumer, nblk=5, wq="pool"):
    S, es = self.S, self.es
    with S.scope() as es2:
        wb = [S.sb(es2, "pw%d" % i, [128, KC, 512], BF16) for i in range(2)]
        ng = (N + 511) // 512
        pi = 0
        for g in range(ng):
            n0 = g * 512
            nn = min(512, N - n0)
            w = wb[g % 2]
            S.dma(wq, w[:, :, :nn], w_d[:, n0:n0 + nn].rearrange("(kc p) n -> p kc n", p=128))
            for q in range((nn + 127) // 128):
                m = min(128, nn - q * 128)
                for (t0, t1) in BLKS[5 - nblk:]:
                    n = t1 - t0
                    pt = self.pb[2 + (pi % 4)]
                    pi += 1
                    for kc in range(KC):
                        S.mm(pt[0:m, :n], w[:, kc, q * 128:q * 128 + m], xn[:, kc, t0:t1], start=(kc == 0), stop=(kc == KC - 1))
                    consumer(g * 4 + q, m, (t0, t1), pt[0:m, :n])


def conv4(self, out, x, wcols, bias=None):
    S = self.S
    for (a, b) in ((0, NCTX), (NCTX, TT)):
        if bias is None:
            S.ts("dve", out[:, a:b], x[:, a:b], wcols[2], None, op0=ALU.mult)
        else:
            S.ts("dve", out[:, a:b], x[:, a:b], wcols[2], bias, op0=ALU.mult, op1=ALU.add)
        S.stt("dve", out[:, a + 2:b], x[:, a:b - 2], wcols[0], out[:, a + 2:b], ALU.mult, ALU.add)
        S.stt("dve", out[:, a + 1:b], x[:, a:b - 1], wcols[1], out[:, a + 1:b], ALU.mult, ALU.add)
        S.stt("dve", out[:, a:b - 1], x[:, a + 1:b], wcols[3], out[:, a:b - 1], ALU.mult, ALU.add)


def inproj0(self, xn):
    S, es = self.S, self.es
    U0 = self.U0 = S.dram("U0", [25, 128, TT], F32)
    w_d = self.din("e_w_in", [1, D, E_IN])[0]
    with S.scope() as es2:
        pre = [S.sb(es2, "pre%d" % i, [128, TT], F32) for i in range(2)]
        post = [S.sb(es2, "post%d" % i, [128, TT], F32) for i in range(2)]
        cnt = [0]

        def consumer(ci, m, blk, pv):
            t0, t1 = blk
            p = pre[ci % 2]
            S.cp("act", p[0:m, t0:t1], pv)
            if t1 < TT:
                return
            po = post[ci % 2]
            if ci < 4:
                wc = [self.col("a_conv_w", k * 4 + ci, 1) for k in range(4)]
                conv4(self, po, p, wc, bias=self.col("a_conv_b", ci, 1))
                S.dma("sp", U0("c%d" % ci)[ci, :, :], po[:])
            elif 8 <= ci < 20:
                wc = [self.col("b_conv_w", k * 12 + (ci - 8), 1) for k in range(4)]
                conv4(self, po, p, wc)
                S.act(po[:], po[:], AF.Silu)
                S.dma("sp", U0("c%d" % ci)[ci, :, :], po[:])
            else:
                S.dma("sp", U0("c%d" % ci)[ci, 0:m, :], p[0:m, :])

        proj(self, xn, w_d, E_IN, consumer)
        S.barrier()


def rev(t, a, b):
    return V(t.h[:, a:b][:, ::-1], (t.name, None))


def rglru(self, XM):
    S, es = self.S, self.es
    U0 = self.U0
    gw_d = self.din("e_a_gate_w", [1, 2, 2, 8, 64, 64])[0]
    with S.scope() as es2:
        gwf = S.sb(es2, "gwf", [128, 16, 128], F32)
        gwb = S.sb(es2, "gwb", [128, 16, 128], BF16)
        nsp = S.sb(es2, "nsp", [128, 8], F32)
        ua = S.sb(es2, "ua", [128, TT], F32)
        uab = S.sb(es2, "uab", [128, TT], BF16)
        ra = S.sb(es2, "ra", [128, TT], F32)
        ii = S.sb(es2, "ii", [128, TT], F32)
        mx = S.sb(es2, "mx", [128, TT], F32)
        hh = [S.sb(es2, "hh%d" % i, [128, TT], F32) for i in range(2)]
        S.memset("pool", gwf[:], 0.0)
        for d in range(2):
            for g in range(2):
                for c in range(4):
                    for half in range(2):
                        S.dma("sp", gwf[half * 64:(half + 1) * 64, (d * 2 + g) * 4 + c, half * 64:(half + 1) * 64],
                              gw_d[d, g, 2 * c + half, :, :])
        S.cp("dve", gwb[:], gwf[:])
        S.act(nsp[:], self.col("a_lambda", 0, 8), AF.Exp, scale=-1.0)
        S.act(nsp[:], nsp[:], AF.Ln, bias=1.0)
        S.ts("dve", nsp[:], nsp[:], -8.0, None, op0=ALU.mult)
        wbm = [S.sb(es2, "adawr%d" % i, [128, KC, 256], F32) for i in range(2)]
        interleave(rglru_body(self, XM, U0, ua, uab, ra, ii, mx, hh, gwb, nsp), self.mods_gen(1, wbm))
        S.barrier()


def rglru_body(self, XM, U0, ua, uab, ra, ii, mx, hh, gwb, nsp):
    S = self.S
    if True:
        for c in range(4):
            S.dma("sp", ua[:], U0("c%d" % c)[c, :, :])
            S.cp("pool", uab[:], ua[:])
            yield
            for d in range(2):
                for (t0, t1) in BLKS:
                    n = t1 - t0
                    pr, pi = self.pb[4], self.pb[5]
                    S.mm(pr[:, :n], gwb[:, (d * 2 + 0) * 4 + c, :], uab[:, t0:t1])
                    S.mm(pi[:, :n], gwb[:, (d * 2 + 1) * 4 + c, :], uab[:, t0:t1])
                    S.act(ra[:, t0:t1], pr[:, :n], AF.Sigmoid, bias=self.col("a_gate_b", (d * 2 + 0) * 4 + c, 1))
                    S.act(ii[:, t0:t1], pi[:, :n], AF.Sigmoid, bias=self.col("a_gate_b", (d * 2 + 1) * 4 + c, 1))
                    yield
                S.act(ra[:], ra[:], AF.Exp, scale=nsp[:, d * 4 + c:d * 4 + c + 1])
                yield
                S.tt("pool", mx[:], ra[:], ra[:], ALU.mult)
                S.act(mx[:], mx[:], AF.Sqrt, bias=1.0, scale=-1.0)
                yield
                S.tt("dve", mx[:], mx[:], ii[:], ALU.mult)
                S.tt("dve", mx[:], mx[:], ua[:], ALU.mult)
                h = hh[d]
                if d == 0:
                    S.scan(h[:], ra[:], mx[:], 0.0)
                else:
                    S.scan(rev(h, 0, NCTX), rev(ra, 0, NCTX), rev(mx, 0, NCTX), 0.0)
                    S.scan(rev(h, NCTX, TT), rev(ra, NCTX, TT), rev(mx, NCTX, TT), h[:, 0:1])
                yield
            S.tt("dve", hh[0][:], hh[0][:], hh[1][:], ALU.add)
            self.tap("ha%d" % c, hh[0][:], [128, TT])
            S.dma("sp", ii[:], U0("c%d" % (4 + c))[4 + c, :, :])
            gelu_tanh(self, mx, ii, ra)
            S.tt("dve", XM[:, c, :], hh[0][:], mx[:], ALU.mult)
            yield


def gelu_tanh(self, out, x, tmp):
    S = self.S
    S.tt("pool", tmp[:], x[:], x[:], ALU.mult)
    S.ts("dve", tmp[:], tmp[:], 0.044715, 1.0, op0=ALU.mult, op1=ALU.add)
    S.tt("dve", tmp[:], tmp[:], x[:], ALU.mult)
    S.act(tmp[:], tmp[:], AF.Tanh, scale=0.7978845608028654)
    S.ts("dve", tmp[:], tmp[:], 1.0, 0.5, op0=ALU.add, op1=ALU.mult)
    S.tt("dve", out[:], tmp[:], x[:], ALU.mult)


def layer(self, l):
    S, es = self.S, self.es
    with S.scope() as es2:
        XM = S.sb(es2, "XM", [128, KC, TT], BF16)
        if l == 0:
            modulate(self, 0, 0, XM)
            self.tap("xn0", XM[:], [128, KC, TT])
            if "inproj0" in self.stages:
                inproj0(self, XM)
            if "rglru" in self.stages:
                rglru(self, XM)
            if "deltanet" in self.stages:
                deltanet(self, XM)
                S.dead = False
                S.barrier()
            if "outproj" in self.stages:
                outproj(self, 0, XM, self.din("e_w_out", [1, D, D])[0])
                S.barrier()
                self.tap("hmid0", self.hT[:], [128, KC, TT])
            if "ffn0" in self.stages:
                ffn0(self, XM)
                self.tap("h1", self.hT[:], [128, KC, TT])
        if l == 1:
            modulate(self, 1, 0, XM, perm=True)
            inproj1(self, XM)
            mixer1(self, XM)
            S.dead = False
            outproj(self, 1, XM, self.din("o_w_out", [1, D, D])[0], perm=True, nblk=4)
            S.barrier()
            self.tap("hmid1", self.hT[:], [128, KC, TT])
            if "nomoe" not in self.stages:
                moe(self, XM)
                self.tap("h2", self.hT[:], [128, KC, TT])
        S.barrier()


Prog.layer = layer


def interleave(*gens):
    gens = [g for g in gens if g is not None]
    while gens:
        for g in list(gens):
            try:
                next(g)
            except StopIteration:
                gens.remove(g)


def chunk_order(nck_ctx, nck_all, d):
    if d == 0:
        return list(range(nck_all))
    return list(range(nck_ctx - 1, -1, -1)) + list(range(nck_all - 1, nck_ctx - 1, -1))


def deltanet(self, XM):
    S, es = self.S, self.es
    U0 = self.U0
    cst = self.cst
    NCK = TT // 128
    with S.scope() as es2:
        ROWS = S.sb(es2, "dnROWS", [128, TT], F32)
        GTb = S.sb(es2, "dnGTb", [128, 8, NCK], F32)
        oacc = S.sb(es2, "oacc", [128, TT], F32)
        with S.scope() as es3:
            G = S.sb(es3, "dnG", [64, TT], F32)
            BETA = S.sb(es3, "dnBeta", [64, TT], F32)
            GCS = S.sb(es3, "dnGCS", [64, TT], F32)
            TLS = S.sb(es3, "dnTLS", [64, TT], F32)
            sc = S.sb(es3, "dnsc", [64, 4], F32)
            sel = S.sb(es3, "dnsel", [128, 8, 128], F32)
            PF = S.sb(es3, "dnPF", [64, TT], F32)
            SF = S.sb(es3, "dnSF", [64, TT], F32)
            mk = S.sb(es3, "dnmk", [64, TT], F32)
            S.memset("pool", G[:], 0.0)
            S.memset("pool", BETA[:], 0.0)
            S.memset("pool", sc[:], 0.0)
            S.memset("pool", ROWS[:], 0.0)
            dn_sc = self.din("dn_sc", [8, 2])
            for d in range(2):
                S.dma("sp", G[32 * d:32 * d + 4, :], U0("c24")[24, 8 + 4 * d:12 + 4 * d, :])
                S.dma("sp", BETA[32 * d:32 * d + 4, :], U0("c24")[24, 4 * d:4 * d + 4, :])
                S.dma("sp", sc[32 * d:32 * d + 4, 0:2], dn_sc[4 * d:4 * d + 4, :])
            S.act(G[:], G[:], AF.Exp, bias=sc[:, 1:2])
            S.act(G[:], G[:], AF.Ln, bias=1.0)
            S.act(sc[:, 2:3], sc[:, 0:1], AF.Exp)
            S.ts("dve", sc[:, 2:3], sc[:, 2:3], -1.0, None, op0=ALU.mult)
            S.ts("dve", G[:], G[:], sc[:, 2:3], None, op0=ALU.mult)
            S.act(BETA[:], BETA[:], AF.Sigmoid)
            if "dn_stopA" in self.stages:
                self.tap("dnG", G[:], [64, TT])
                S.barrier()
                return
            S.memset("pool", mk[:], 1.0)
            S.memset("pool", V(mk.h[:, 0:TT:128], (mk.name, None)), 0.0)
            S.scan(PF[:], mk[:], G[:], 0.0)
            S.memset("pool", mk[:], 1.0)
            S.memset("pool", V(mk.h[:, 127:TT:128], (mk.name, None)), 0.0)
            S.scan(rev(SF, 0, TT), rev(mk, 0, TT), rev(G, 0, TT), 0.0)
            S.cp("dve", GCS[0:32, :], PF[0:32, :])
            S.cp("dve", GCS[32:64, :], SF[32:64, :])
            S.tt("dve", TLS[0:32, :], SF[0:32, :], G[0:32, :], ALU.subtract)
            S.tt("dve", TLS[32:64, :], PF[32:64, :], G[32:64, :], ALU.subtract)
            S.act(TLS[:], TLS[:], AF.Exp)
            if "dn_stopB" in self.stages:
                self.tap("dnG", GCS[:], [64, TT])
                S.barrier()
                return
            for d in range(2):
                for kind, src in enumerate((GCS, BETA, TLS)):
                    S.dma("sp", ROWS[32 * d + 4 * kind:32 * d + 4 * kind + 4, :], src[32 * d:32 * d + 4, :])
            if "dn_stopC" in self.stages:
                self.tap("dnG", ROWS[:], [64, TT])
                S.barrier()
                return
            for r8 in range(8):
                r = (r8 // 4) * 32 + (r8 % 4)
                S.ts("dve", sel[:, r8, :], cst[:, 1, :], cst[:, 0, r:r + 1], None, op0=ALU.mult)
            gte = S.sb(es3, "dngte", [128, NCK], F32)
            S.memset("dve", gte[:], 0.0)
            S.cp("dve", gte[0:64, :], V(PF.h[:, 127:TT:128], (PF.name, None)))
            for r8 in range(8):
                pt = self.pb[0]
                S.mm(pt[:, 0:NCK], sel[:, r8, :], gte[:])
                S.act(GTb[:, r8, :], pt[:, 0:NCK], AF.Exp)
            S.barrier()
        self.tap("dnrows", ROWS[:], [128, TT])
        self.tap("dngtb", GTb[:], [128, 8, NCK])
        if "dn_stop1" in self.stages:
            return

        for h in range(4):
            for d in range(2):
                r8 = d * 4 + h
                rr = d * 32 + h
                Mi = cst[:, 2, :] if d == 0 else cst[:, 4, :]
                Ms = self.ncst[:, 0, :] if d == 0 else self.ncst[:, 1, :]
                MsT = self.ncst[:, 1, :] if d == 0 else self.ncst[:, 0, :]
                with S.scope() as es3:
                    kdec = S.sb(es3, "kdec", [128, TT], BF16)
                    qdec = S.sb(es3, "qdec", [128, TT], BF16)
                    AT = S.sb(es3, "AT", [128, NCK, 128], BF16)
                    TTt = S.sb(es3, "TTt", [128, NCK, 128], F32)
                    vb = S.sb(es3, "vb", [128, NCK, 128], BF16)
                    kntm = S.sb(es3, "kntm", [128, NCK, 128], BF16)
                    cols = S.sb(es3, "cols", [128, NCK, 4], F32)
                    Sf = S.sb(es3, "Sf", [128, 128], F32)
                    Sb = S.sb(es3, "Sb", [128, 128], BF16)
                    sel2 = S.sb(es3, "sel2", [128, 2, 128], F32)
                    mk3 = S.sb(es3, "mk3", [128, 3, 128], F32)
                    qc = S.sb(es3, "qc", [128, 512], F32)
                    kc = S.sb(es3, "kc", [128, 512], F32)
                    vc = S.sb(es3, "vc", [128, 512], F32)
                    sqb = S.sb(es3, "sqb", [128, 512], BF16)
                    rs = S.sb(es3, "rs", [128, 512], F32)
                    GCb = S.sb(es3, "GCb", [128, 512], F32)
                    Eb = S.sb(es3, "Eb", [128, 512], F32)
                    knb = S.sb(es3, "knb", [128, 512], BF16)
                    kbb = S.sb(es3, "kbb", [128, 512], BF16)
                    qnb = S.sb(es3, "qnb", [128, 512], BF16)
                    Dm = S.sb(es3, "Dm", [128, 4, 128], F32)
                    Ds = S.sb(es3, "Ds", [128, 4, 128], F32)
                    DT = S.sb(es3, "DTm", [128, 4, 128], F32)
                    Mm = [S.sb(es3, "Mm%d" % i, [128, 2, 4, 128], DBL_DT) for i in range(2)]
                    Xx = [S.sb(es3, "Xx%d" % i, [128, 4, 128], DBL_DT) for i in range(2)]
                    rb = [S.sb(es3, "rb%d" % i, [128, 128], F32) for i in range(2)]
                    vnb = [S.sb(es3, "vnb%d" % i, [128, 128], BF16) for i in range(2)]
                    vns = [S.sb(es3, "vns%d" % i, [128, 128], BF16) for i in range(2)]
                    S.ts("dve", sel2[:, 0, :], cst[:, 1, :], cst[:, 0, rr:rr + 1], None, op0=ALU.mult)
                    S.ts("dve", sel2[:, 1, :], cst[:, 1, :], cst[:, 0, rr + 4:rr + 5], None, op0=ALU.mult)
                    Mi_, Ms_, MsT_ = (cst[:, 2, :], cst[:, 3, :], cst[:, 5, :]) if d == 0 else (cst[:, 4, :], cst[:, 5, :], cst[:, 3, :])
                    S.ts("dve", mk3[:, 0, :], Mi_, 1e30, -1e30, op0=ALU.mult, op1=ALU.add)
                    S.ts("dve", mk3[:, 1, :], Ms_, 1e30, -1e30, op0=ALU.mult, op1=ALU.add)
                    S.ts("dve", mk3[:, 2, :], MsT_, -1e30, 1e30, op0=ALU.mult, op1=ALU.add)
                    S.memset("dve", Sf[:], 0.0)
                    S.memset("dve", Sb[:], 0.0)

                    def pv3(pt, nck):
                        return V(pt.h[:, 0:nck * 128].rearrange("p (c t) -> p c t", c=nck), (pt.name, None))

                    def bc2(v, nck):
                        return V(_ap(v).unsqueeze(1).to_broadcast([128, nck, 128]), v.dep)

                    def prep_block(bi):
                        t0, t1 = BLKS[bi]
                        n = t1 - t0
                        nck = n // 128
                        c0 = t0 // 128
                        q_, k_, v_ = qc, kc, vc
                        S.dma("sp", q_[:, :n], U0("c%d" % (8 + h))[8 + h, :, t0:t1])
                        S.dma("sp", k_[:, :n], U0("c%d" % (12 + h))[12 + h, :, t0:t1])
                        S.dma("sp", v_[:, :n], U0("c%d" % (16 + h))[16 + h, :, t0:t1])
                        p0, p1, p2 = self.pb[0], self.pb[1], self.pb[2]
                        S.mm(p1[:, :n], sel2[:, 0, :], ROWS[:, t0:t1])
                        S.mm(p2[:, :n], sel2[:, 1, :], ROWS[:, t0:t1])
                        for cc in range(nck):
                            S.mm(p0[:, cc * 64:(cc + 1) * 64], ROWS[:, t0 + cc * 128:t0 + cc * 128 + 128], cst[:, 0, 0:64])
                        yield
                        S.cp("act", GCb[:, :n], p1[:, :n])
                        S.act(Eb[:, :n], p1[:, :n], AF.Exp)
                        colp = V(p0.h[:, 0:nck * 64].rearrange("p (c t) -> p c t", c=nck), (p0.name, None))
                        S.cp("dve", cols[:, c0:c0 + nck, 0], V(colp.ap[:, :, rr], colp.dep))
                        S.ts("dve", cols[:, c0:c0 + nck, 1], V(colp.ap[:, :, rr + 4], colp.dep), -1.0, None, op0=ALU.mult)
                        S.cp("dve", cols[:, c0:c0 + nck, 2], V(colp.ap[:, :, rr + 8], colp.dep))
                        S.cp("dve", cols[:, c0:c0 + nck, 3], V(colp.ap[:, :, rr + 4], colp.dep))
                        S.tt("dve", sqb[:, :n], k_[:, :n], k_[:, :n], ALU.mult)
                        yield
                        S.mm(p0[:, :n], self.onesb, sqb[:, :n])
                        yield
                        S.act(rs[:, :n], p0[:, :n], AF.Ln, bias=EPS)
                        S.act(rs[:, :n], rs[:, :n], AF.Exp, scale=-0.5)
                        yield
                        S.tt("dve", k_[:, :n], k_[:, :n], rs[:, :n], ALU.mult)
                        S.tt("pool", sqb[:, :n], q_[:, :n], q_[:, :n], ALU.mult)
                        S.tt("pool", kdec[:, t0:t1], k_[:, :n], Eb[:, :n], ALU.mult)
                        S.tt("dve", kbb[:, :n], k_[:, :n], p2[:, :n], ALU.mult)
                        yield
                        S.mm(p0[:, :n], self.onesb, sqb[:, :n])
                        S.cp("act", knb[:, :n], k_[:, :n])
                        yield
                        S.act(rs[:, :n], p0[:, :n], AF.Ln, bias=EPS)
                        S.act(rs[:, :n], rs[:, :n], AF.Exp, scale=-0.5)
                        pvt, pkt = self.pb[1], self.pb[2]
                        for cc in range(nck):
                            S.tr(pvt[:, cc * 128:(cc + 1) * 128], v_[:, cc * 128:(cc + 1) * 128], self.ident)
                        for cc in range(nck):
                            S.tr(pkt[:, cc * 128:(cc + 1) * 128], k_[:, cc * 128:(cc + 1) * 128], self.ident)
                        yield
                        S.stt("dve", q_[:, :n], q_[:, :n], 128.0 ** -0.5, rs[:, :n], ALU.mult, ALU.mult)
                        S.tt("pool", qdec[:, t0:t1], q_[:, :n], Eb[:, :n], ALU.mult)
                        bcol = V(cols.h[:, c0:c0 + nck, 3:4].to_broadcast([128, nck, 128]), (cols.name, None))
                        S.tt("dve", vb[:, c0:c0 + nck, :], pv3(pvt, nck), bcol, ALU.mult)
                        S.cp("act", qnb[:, :n], q_[:, :n])
                        S.cp("act", kntm[:, c0:c0 + nck, :], pv3(pkt, nck))
                        yield
                        pg1, pg2, pg3 = self.pb[3], self.pb[4], self.pb[5]
                        for cc in range(nck):
                            a_, b_ = cc * 128, cc * 128 + 128
                            S.mm(pg1[:, a_:b_], knb[:, a_:b_], kbb[:, a_:b_])
                        for cc in range(nck):
                            a_, b_ = cc * 128, cc * 128 + 128
                            S.mm(pg2[:, a_:b_], kbb[:, a_:b_], knb[:, a_:b_])
                        for cc in range(nck):
                            a_, b_ = cc * 128, cc * 128 + 128
                            S.mm(pg3[:, a_:b_], knb[:, a_:b_], qnb[:, a_:b_])
                        gcol = V(cols.h[:, c0:c0 + nck, 0:1].to_broadcast([128, nck, 128]), (cols.name, None))
                        gcb3 = V(GCb.h[:, 0:n].rearrange("p (c t) -> p c t", c=nck), (GCb.name, None))
                        S.tt("dve", Dm[:, 0:nck, :], gcb3, gcol, ALU.subtract)
                        S.stt("dve", DT[:, 0:nck, :], Dm[:, 0:nck, :], 0.0, bc2(mk3[:, 2, :], nck), ALU.max, ALU.add)
                        S.stt("dve", Ds[:, 0:nck, :], Dm[:, 0:nck, :], 0.0, bc2(mk3[:, 1, :], nck), ALU.min, ALU.add)
                        S.stt("dve", Dm[:, 0:nck, :], Dm[:, 0:nck, :], 0.0, bc2(mk3[:, 0, :], nck), ALU.min, ALU.add)
                        yield
                        S.act(DT[:, 0:nck, :], DT[:, 0:nck, :], AF.Exp, scale=-1.0)
                        S.act(Ds[:, 0:nck, :], Ds[:, 0:nck, :], AF.Exp)
                        S.act(Dm[:, 0:nck, :], Dm[:, 0:nck, :], AF.Exp)
                        yield
                        M0 = Mm[0]
                        S.stt("dve", r32v(M0[:, 0, 0:nck, :]), pv3(pg1, nck), -1.0, Ds[:, 0:nck, :], ALU.mult, ALU.mult)
                        S.stt("dve", r32v(M0[:, 1, 0:nck, :]), pv3(pg2, nck), -1.0, DT[:, 0:nck, :], ALU.mult, ALU.mult)
                        S.tt("dve", AT[:, c0:c0 + nck, :], pv3(pg3, nck), Dm[:, 0:nck, :], ALU.mult)
                        S.tt("dve", r32v(Xx[0][:, 0:nck, :]), M0[:, 0, 0:nck, :], bc2(cst[:, 0, :], nck), ALU.add)
                        yield
                        cur = 0
                        for lev in range(6):
                            Mc, Mn = Mm[cur], Mm[1 - cur]
                            pa, pbk, px = self.pb[3], self.pb[4], self.pb[5]
                            for cc in range(nck):
                                S.mm(pbk[:, cc * 128:(cc + 1) * 128], Mc("m")[:, 0, cc, :], Mc("t")[:, 1, cc, :], r32=R32)
                            if lev < 5:
                                for cc in range(nck):
                                    S.mm(pa[:, cc * 128:(cc + 1) * 128], Mc("t")[:, 1, cc, :], Mc("m")[:, 0, cc, :], r32=R32)
                            yield
                            S.cp("act", r32v(Mn("t")[:, 1, 0:nck, :]), pv3(pbk, nck))
                            yield
                            Xc, Xn = Xx[lev % 2], Xx[1 - lev % 2]
                            for cc in range(nck):
                                S.mm(px[:, cc * 128:(cc + 1) * 128], Mn("t")[:, 1, cc, :], Xc[:, cc, :], r32=R32)
                            if lev < 5:
                                S.cp("act", r32v(Mn("m")[:, 0, 0:nck, :]), pv3(pa, nck))
                            yield
                            if lev < 5:
                                S.tt("dve", r32v(Xn[:, 0:nck, :]), pv3(px, nck), Xc[:, 0:nck, :], ALU.add)
                            else:
                                S.tt("dve", TTt[:, c0:c0 + nck, :], pv3(px, nck), Xc[:, 0:nck, :], ALU.add)
                            cur = 1 - cur
                            yield

                    step = [0]

                    def rec_chunk(c):
                        si = step[0]
                        step[0] += 1
                        ta, tb = c * 128, c * 128 + 128
                        p6, p7 = self.pb[6], self.pb[7]
                        S.mm(p6[:, 0:128], kdec[:, ta:tb], Sb[:])
                        yield
                        r_ = rb[si % 2]
                        S.stt("dve", r_[:], p6[:, 0:128], cols[:, c, 1:2], vb[:, c, :], ALU.mult, ALU.add)
                        yield
                        S.mm(p6[:, 128:256], TTt[:, c, :], r_[:])
                        yield
                        vn, vs = vnb[si % 2], vns[si % 2]
                        S.cp("dve", vn[:], p6[:, 128:256])
                        S.ts("dve", vs[:], p6[:, 128:256], cols[:, c, 2:3], None, op0=ALU.mult)
                        yield
                        S.mm(p7[:, 128:256], kntm[:, c, :], vs[:])
                        S.mm(p7[:, 0:128], Sb[:], qdec[:, ta:tb], start=True, stop=False)
                        S.mm(p7[:, 0:128], vn[:], AT[:, c, :], start=False, stop=True)
                        yield
                        S.stt("dve", Sb[:], Sf[:], GTb[:, r8, c:c + 1], p7[:, 128:256], ALU.mult, ALU.add)
                        S.stt("dve", Sf[:], Sf[:], GTb[:, r8, c:c + 1], p7[:, 128:256], ALU.mult, ALU.add)
                        if d == 0:
                            S.cp("dve", oacc[:, ta:tb], p7[:, 0:128])
                        else:
                            S.tt("dve", oacc[:, ta:tb], oacc[:, ta:tb], p7[:, 0:128], ALU.add)
                        yield

                    def rec_block(bi):
                        t0, t1 = BLKS[bi]
                        cs = list(range(t0 // 128, t1 // 128))
                        if d == 1:
                            cs = cs[::-1]
                        for c in cs:
                            yield from rec_chunk(c)

                    border = [0, 1, 2, 3, 4] if d == 0 else [0, 4, 3, 2, 1]
                    for k_i in range(len(border) + 1):
                        gp = prep_block(border[k_i]) if k_i < len(border) else None
                        gr = rec_block(border[k_i - 1]) if k_i > 0 else None
                        interleave(gp, gr)
            self.tap("ob%d" % h, oacc[:], [128, TT])
            with S.scope() as es3:
                zb = [S.sb(es3, "zb%d" % i, [128, 512], F32) for i in range(2)]
                sqb = S.sb(es3, "gsq", [128, 512], BF16)
                rs = S.sb(es3, "grs", [128, 512], F32)
                for bi, (t0, t1) in enumerate(BLKS):
                    n = t1 - t0
                    z_ = zb[bi % 2]
                    S.dma("sp", z_[:, :n], U0("c%d" % (20 + h))[20 + h, :, t0:t1])
                    S.act(sqb[:, :n], oacc[:, t0:t1], AF.Square)
                    p0 = self.pb[0]
                    S.mm(p0[:, :n], self.onesb, sqb[:, :n])
                    S.ts("dve", rs[:, :n], p0[:, :n], 1.0 / 128.0, EPS, op0=ALU.mult, op1=ALU.add)
                    S.act(rs[:, :n], rs[:, :n], AF.Sqrt)
                    S.recip(rs[:, :n], rs[:, :n])
                    S.act(z_[:, :n], z_[:, :n], AF.Silu)
                    S.stt("dve", rs[:, :n], oacc[:, t0:t1], self.col("b_norm_g", 0, 1), rs[:, :n], ALU.mult, ALU.mult)
                    S.tt("dve", XM[:, 4 + h, t0:t1], rs[:, :n], z_[:, :n], ALU.mult)
                S.barrier()


def outproj(self, l, XM, w_d, perm=False, nblk=5):
    S = self.S
    hT = self.hT
    md = self.mod[l]

    def consumer(ci, m, blk, pv):
        t0, t1 = blk
        w = 1 if t0 < NCTX else 0
        if perm and t0 >= NCTX:
            c0 = (t0 - NCTX) // 32
            c1 = (t1 - NCTX) // 32
            hv = V(hT.h[:, ci, NCTX:TT].rearrange("p (r c) -> p c r", r=32, c=64)[:, c0:c1, :], (hT.name, None))
            pvv = V(_ap(pv).rearrange("p (c r) -> p c r", r=32), pv.dep)
            S.stt("dve", hv, pvv, md[:, 16 + ci, w:w + 1], hv, ALU.mult, ALU.add)
        else:
            S.stt("dve", hT[:, ci, t0:t1], pv, md[:, 16 + ci, w:w + 1], hT[:, ci, t0:t1], ALU.mult, ALU.add)

    proj(self, XM, w_d, D, consumer, nblk=nblk)


FGROUPS = [(0, 4), (4, 8), (8, 12), (12, 16), (16, 20), (20, 22)]


def swiglu(self, l, xn, wg_d, wu_d, wd_d, nblk=5, gate_fn=None, wbufs=None):
    S = self.S
    hT = self.hT
    md = self.mod[l]
    wgb, wub, wdb, actb, sgb = wbufs
    for gi, (c0, c1) in enumerate(FGROUPS):
        nf = c1 - c0
        u = self.wuse
        self.wuse += 1
        wg, wu, wd = wgb[u % 2], wub[u % 2], wdb[u % 2]
        S.dma("pool", wg[:, :, 0:nf * 128], wg_d[:, c0 * 128:c1 * 128].rearrange("(kc p) n -> p kc n", p=128))
        S.dma("pool", wu[:, :, 0:nf * 128], wu_d[:, c0 * 128:c1 * 128].rearrange("(kc p) n -> p kc n", p=128))
        S.dma("pool", wd[:, 0:nf, :], wd_d[c0 * 128:c1 * 128, :].rearrange("(fc p) n -> p fc n", p=128))
        if gi == 0 and l == 0:
            self.tap("wgtap", wg[:], [128, KC, 512])
        def gu(bi, t0, t1):
            n = t1 - t0
            gv = gate_fn(bi, (t0, t1)) if gate_fn is not None else None
            ab = actb[self.ause % 2]
            self.ause += 1
            for fc in range(nf):
                pg = self.pb[self.puse % 2]
                pu = self.pb[2 + self.puse % 2]
                self.puse += 1
                for kc in range(KC):
                    S.mm(pg[:, :n], wg[:, kc, fc * 128:(fc + 1) * 128], xn[:, kc, t0:t1], start=(kc == 0), stop=(kc == KC - 1))
                for kc in range(KC):
                    S.mm(pu[:, :n], wu[:, kc, fc * 128:(fc + 1) * 128], xn[:, kc, t0:t1], start=(kc == 0), stop=(kc == KC - 1))
                sg = sgb[fc % 2]
                S.act(sg[:, :n], pg[:, :n], AF.Silu)
                if gv is not None:
                    S.tt("dve", sg[:, :n], sg[:, :n], gv, ALU.mult)
                S.tt("dve", ab[:, fc, :n], sg[:, :n], pu[:, :n], ALU.mult)
            return ab

        def down(ab, t0, t1):
            n = t1 - t0
            w = 1 if t0 < NCTX else 0
            for nc_ in range(KC):
                py = self.pb[4 + self.yuse % 4]
                self.yuse += 1
                for fc in range(nf):
                    S.mm(py[:, :n], wd[:, fc, nc_ * 128:(nc_ + 1) * 128], ab[:, fc, :n], start=(fc == 0), stop=(fc == nf - 1))
                S.stt("dve", hT[:, nc_, t0:t1], py[:, :n], md[:, 40 + nc_, w:w + 1], hT[:, nc_, t0:t1], ALU.mult, ALU.add)

        prev = None
        for bi, (t0, t1) in enumerate(BLKS[5 - nblk:]):
            ab = gu(bi, t0, t1)
            if prev is not None:
                down(*prev)
            prev = (ab, t0, t1)
        down(*prev)


def alloc_wbufs(self, es2):
    S = self.S
    wgb = [S.sb(es2, "wgb%d" % i, [128, KC, 512], BF16) for i in range(2)]
    wub = [S.sb(es2, "wub%d" % i, [128, KC, 512], BF16) for i in range(2)]
    wdb = [S.sb(es2, "wdb%d" % i, [128, 4, D], BF16) for i in range(2)]
    actb = [S.sb(es2, "actb%d" % i, [128, 4, 512], BF16) for i in range(2)]
    sgb = [S.sb(es2, "sgb%d" % i, [128, 512], F32) for i in range(2)]
    self.wuse = self.ause = self.puse = self.yuse = 0
    return wgb, wub, wdb, actb, sgb


def ffn0(self, XM):
    S = self.S
    modulate(self, 0, 1, XM)
    self.tap("xn1", XM[:], [128, KC, TT])
    with S.scope() as es2:
        wb = alloc_wbufs(self, es2)
        swiglu(self, 0, XM, self.din("e_ffn_w_gate", [1, D, DFF])[0], self.din("e_ffn_w_up", [1, D, DFF])[0],
               self.din("e_ffn_w_down", [1, DFF, D])[0], wbufs=wb)
        S.barrier()


O_CQ, O_CFF, O_CFB, O_CI, O_CG, O_DQ, O_DK, O_DV, O_DG, O_DLR = 0, 4, 8, 12, 16, 20, 22, 24, 28, 32


def inproj1(self, xn):
    S = self.S
    U1 = self.U1 = S.dram("U1", [33, 128, TT], F32)
    w_d = self.din("o_w_in", [1, D, O_IN])[0]
    with S.scope() as es2:
        buf = [S.sb(es2, "u1b%d" % i, [128, TT], F32) for i in range(3)]

        def consumer(ci, m, blk, pv):
            t0, t1 = blk
            p = buf[ci % 3]
            S.cp("act" if (t0 // 512) % 2 == 0 else "dve", p[0:m, t0:t1], pv)
            if t1 == TT:
                S.dma("sp", U1("c%d" % ci)[ci, 0:m, :], p[0:m, :])

        proj(self, xn, w_d, O_IN, consumer)


def gla_unit(self, XM, kind, idx, masks):
    S = self.S
    U1 = self.U1
    cst = self.cst
    NT = TT // 128
    nh = 1 if kind == "c" else 2
    mkf, mkb = masks
    with S.scope() as es2:
        vtm = S.sb(es2, "vtm", [128, NT, nh, 128], BF16)
        oacc = S.sb(es2, "oacc1", [128, nh, NLAT], F32)
        lbc = S.sb(es2, "lbc", [128, 4], F32)
        if kind == "c":
            S.tt("dve", lbc[:, 0:1], self.col("lb_logits", 4 + idx, 1), self.col("lb_logits", idx, 1), ALU.subtract)
            S.act(lbc[:, 0:1], lbc[:, 0:1], AF.Sigmoid)
            S.ts("dve", lbc[:, 1:2], lbc[:, 0:1], -1.0, 1.0, op0=ALU.mult, op1=ALU.add)
        else:
            for d in range(2):
                S.ts("dve", lbc[:, 2 + d:3 + d], self.col("d_gate_b2", d * 2 + idx, 1), -1.0, None, op0=ALU.mult)
        with S.scope() as es3:
            A = []
            for d in range(2):
                A.append(dict(
                    qdec=S.sb(es3, "qdec1", [128, TT], BF16), kinv=S.sb(es3, "kinv1", [128, TT], BF16),
                    ktm=S.sb(es3, "ktm", [128, NT, 128], BF16), dtot=S.sb(es3, "dtot", [128, TT // 64], F32)))
            with S.scope() as es4:
                T_ = []
                for d in range(2):
                    T_.append(dict(qb=S.sb(es4, "qb", [128, 512], F32), kb=S.sb(es4, "kb", [128, 512], F32),
                                   ld=S.sb(es4, "ld", [128, 512], F32), PF=S.sb(es4, "PF1", [128, 512], F32),
                                   SF=S.sb(es4, "SF1", [128, 512], F32), tmp=S.sb(es4, "tmp1", [128, 512], F32)))
                vb_ = [S.sb(es4, "vb1_0", [128, 512], F32)] * nh
                if kind == "d":
                    w2b = S.sb(es4, "w2b", [128, 2, 128], BF16)
                    dlrb = S.sb(es4, "dlrb", [128, TT], BF16)
                    with S.scope() as es5:
                        w2f = S.sb(es5, "w2f", [128, 2, 128], F32)
                        dlr = S.sb(es5, "dlr", [128, 384], F32)
                        S.memset("pool", w2f[:], 0.0)
                        w2_d = self.din("o_d_gate_w2", [1, 2, 16, 256])[0]
                        for d in range(2):
                            S.dma("sp", w2f[16 * d:16 * d + 16, d, :], w2_d[d, :, idx * 128:(idx + 1) * 128])
                        S.cp("dve", w2b[:], w2f[:])
                        S.memset("pool", dlr[:], 0.0)
                        for hf in range(6):
                            S.dma("sp", dlr[0:32, :], U1("c32")[32, 0:32, hf * 384:(hf + 1) * 384])
                            S.cp("dve", dlrb[:, hf * 384:(hf + 1) * 384], dlr[:])

                def prep_dir(d):
                    a_ = A[d]
                    t_ = T_[d]
                    qdec, kinv, ktm, dtot = a_["qdec"], a_["kinv"], a_["ktm"], a_["dtot"]
                    q_, k_, ld, PF, SF, tmp = t_["qb"], t_["kb"], t_["ld"], t_["PF"], t_["SF"], t_["tmp"]
                    pl = self.pb[d]
                    ptr = self.pb[2 + d]
                    for bi, (t0, t1) in enumerate(BLKS):
                        n = t1 - t0
                        if kind == "c":
                            S.dma("sp", q_[:, :n], U1("c%d" % (O_CQ + idx))[O_CQ + idx, :, t0:t1])
                            S.dma("sp", k_[:, :n], U1("c%d" % ((O_CFF if d == 0 else O_CFB) + idx))[(O_CFF if d == 0 else O_CFB) + idx, :, t0:t1])
                            yield
                            S.act(q_[:, :n], q_[:, :n], AF.Silu)
                            S.act(k_[:, :n], k_[:, :n], AF.Sigmoid)
                            yield
                            S.ts("dve", q_[:, :n], q_[:, :n], 128.0 ** -0.5, None, op0=ALU.mult)
                            S.ts("dve", k_[:, :n], k_[:, :n], lbc[:, 1:2], lbc[:, 0:1], op0=ALU.mult, op1=ALU.add)
                            yield
                            S.act(ld[:, :n], k_[:, :n], AF.Ln)
                            yield
                            S.ts("dve", k_[:, :n], k_[:, :n], -1.0, 1.0, op0=ALU.mult, op1=ALU.add)
                        else:
                            S.dma("sp", q_[:, :n], U1("c%d" % (O_DQ + idx))[O_DQ + idx, :, t0:t1])
                            S.dma("sp", k_[:, :n], U1("c%d" % (O_DK + idx))[O_DK + idx, :, t0:t1])
                            S.mm(pl[:, :n], w2b[:, d, :], dlrb[:, t0:t1])
                            yield
                            S.act(ld[:, :n], pl[:, :n], AF.Exp, bias=lbc[:, 2 + d:3 + d], scale=-1.0)
                            S.act(ld[:, :n], ld[:, :n], AF.Ln, bias=1.0)
                            yield
                            S.ts("dve", q_[:, :n], q_[:, :n], 64.0 ** -0.5, None, op0=ALU.mult)
                            S.ts("dve", ld[:, :n], ld[:, :n], -1.0 / 16.0, None, op0=ALU.mult)
                        S.scan(PF[:, :n], mkf[:, :n], ld[:, :n], 0.0)
                        S.scan(rev(SF, 0, n), rev(mkb, 0, n), rev(ld, 0, n), 0.0)
                        bc, oth = (PF, SF) if d == 0 else (SF, PF)
                        yield
                        S.act(dtot[:, t0 // 64:t1 // 64], V(PF.h[:, 63:n:64], (PF.name, None)), AF.Exp)
                        S.act(tmp[:, :n], bc[:, :n], AF.Exp)
                        yield
                        S.tt("dve", oth[:, :n], oth[:, :n], ld[:, :n], ALU.subtract)
                        S.tt("dve", qdec[:, t0:t1], q_[:, :n], tmp[:, :n], ALU.mult)
                        yield
                        S.act(tmp[:, :n], bc[:, :n], AF.Exp, scale=-1.0)
                        S.act(oth[:, :n], oth[:, :n], AF.Exp)
                        yield
                        S.tt("dve", kinv[:, t0:t1], k_[:, :n], tmp[:, :n], ALU.mult)
                        S.tt("dve", tmp[:, :n], k_[:, :n], oth[:, :n], ALU.mult)
                        yield
                        for cc in range(n // 128):
                            S.tr(ptr[:, cc * 128:(cc + 1) * 128], tmp[:, cc * 128:(cc + 1) * 128], self.ident)
                        yield
                        S.cp("act", ktm[:, t0 // 128:t1 // 128, :], V(ptr.h[:, 0:n].rearrange("p (c t) -> p c t", t=128), (ptr.name, None)))
                        yield
                        if d == 0:
                            for hh in range(nh):
                                v_ = vb_[hh]
                                vch = (O_CI + idx) if kind == "c" else (O_DV + idx * 2 + hh)
                                S.dma("sp", v_[:, :n], U1("c%d" % vch)[vch, :, t0:t1])
                                for cc in range(n // 128):
                                    S.tr(ptr[:, cc * 128:(cc + 1) * 128], v_[:, cc * 128:(cc + 1) * 128], self.ident)
                                yield
                                S.cp("act", vtm[:, t0 // 128:t1 // 128, hh, :], V(ptr.h[:, 0:n].rearrange("p (c t) -> p c t", t=128), (ptr.name, None)))
                                yield

                interleave(prep_dir(0), prep_dir(1))
            with S.scope() as es4:
                NP = TT // 64
                Uall = S.sb(es4, "Uall", [128, 128, NP], F32)
                Sball = S.sb(es4, "Sball", [128, NP, 128], BF16)
                Dq = S.sb(es4, "Dq", [128, 32, NP], F32)
                dperm = S.sb(es4, "dperm", [128, NP], F32)
                ATb = [S.sb(es4, "ATb%d" % i, [128, 128], BF16) for i in range(2)]
                for d in range(2):
                    a_ = A[d]
                    qdec, kinv, ktm, dtot = a_["qdec"], a_["kinv"], a_["ktm"], a_["dtot"]
                    mask = cst[:, 6, :] if d == 0 else cst[:, 7, :]
                    order_tiles = chunk_order(NCTX // 128, NT, d)
                    pos = {}
                    for si, ti in enumerate(order_tiles):
                        for q in range(2):
                            ci = q if d == 0 else 1 - q
                            pos[(ti, ci)] = 2 * si + q
                    if d == 0:
                        S.cp("pool", dperm[:], dtot[:])
                    else:
                        nc4 = NCTX // 64
                        S.cp("pool", dperm[:, 0:nc4], V(dtot.h[:, 0:nc4][:, ::-1], (dtot.name, None)))
                        S.cp("pool", dperm[:, nc4:NP], V(dtot.h[:, nc4:NP][:, ::-1], (dtot.name, None)))
                    S.cp("dve", Dq[:], V(dperm.h[:, :].unsqueeze(1).to_broadcast([128, 32, NP]), (dperm.name, None)))
                    S.memset("dve", Dq[:, :, 0:1], 0.0)
                    for hh in range(nh):
                        r0, r1 = (0, 128) if kind == "c" else (64 * hh, 64 * hh + 64)
                        for gi, g in enumerate(range(0, NT, 4)):
                            tiles = order_tiles[g:g + 4]
                            nt = len(tiles)
                            banks = [self.pb[(gi % 2) * 2], self.pb[(gi % 2) * 2 + 1]]
                            for j, ti in enumerate(tiles):
                                for q in range(2):
                                    ci = q if d == 0 else 1 - q
                                    S.mm(banks[ci][:, j * 128:(j + 1) * 128], ktm[ci * 64:ci * 64 + 64, ti, :], vtm[ci * 64:ci * 64 + 64, ti, hh, :])
                            for ci in range(2):
                                q = ci if d == 0 else 1 - ci
                                p0 = 2 * g + q
                                ov = V(Uall.h[:, :, p0:min(p0 + 2 * nt, NP):2], (Uall.name, None))
                                iv = V(banks[ci].h[:, 0:nt * 128].rearrange("p (c v) -> p v c", c=nt), (banks[ci].name, None))
                                S.cp("act" if ci == 0 else "dve", ov, iv)
                        for vq in range(4):
                            u2 = V(Uall.h[:, vq * 32:(vq + 1) * 32, :].rearrange("p v c -> p (v c)"), (Uall.name, None))
                            d2 = V(Dq.h[:, :, :].rearrange("p v c -> p (v c)"), (Dq.name, None))
                            S.scan(u2, d2, u2, 0.0)
                        for half in range(2):
                            ov = Sball[:, half * 18:(half + 1) * 18, :]
                            iv = V(Uall.h[:, :, half * 18:(half + 1) * 18].rearrange("p v c -> p c v"), (Uall.name, None))
                            S.cp("act" if half == 0 else "dve", ov, iv)
                        for si, ti in enumerate(order_tiles):
                            if ti < NCTX // 128:
                                continue
                            ta = ti * 128
                            la = ta - NCTX
                            pA = self.pb[4 + 3 * (si % 2)]
                            pO = self.pb[5 + si % 2]
                            AT = ATb[si % 2]
                            S.mm(pA[:, 0:128], kinv[r0:r1, ta:ta + 128], qdec[r0:r1, ta:ta + 128])
                            S.tt("dve", AT[:], pA[:, 0:128], mask, ALU.mult)
                            S.mm(pO[:, 0:128], vtm[:, ti, hh, :], AT[:], start=True, stop=False)
                            for q in range(2):
                                ci = q if d == 0 else 1 - q
                                p = pos[(ti, ci)]
                                ca = ta + ci * 64
                                S.mm(pO[:, ci * 64:ci * 64 + 64], Sball[r0:r1, p - 1, :], qdec[r0:r1, ca:ca + 64], start=False, stop=(q == 1))
                            if d == 0:
                                S.cp("act", oacc[:, hh, la:la + 128], pO[:, 0:128])
                            else:
                                S.tt("dve", oacc[:, hh, la:la + 128], oacc[:, hh, la:la + 128], pO[:, 0:128], ALU.add)
                self.ck("g_rec")
        with S.scope() as es3:
            zb = [S.sb(es3, "zb1%d" % i, [128, 512], F32) for i in range(2)]
            sqb = S.sb(es3, "gsq1", [128, 512], BF16)
            rs = S.sb(es3, "grs1", [128, 512], F32)
            for hh in range(nh):
                if kind == "c":
                    gch, xch, gname = O_CG + idx, idx, "c_norm_g"
                else:
                    gch, xch, gname = O_DG + idx * 2 + hh, 4 + idx * 2 + hh, "d_norm_g"
                if hh == 0:
                    self.tap("oc0" if kind == "c" else "od0", oacc[:, 0, :], [128, NLAT])
                for bi, (t0, t1) in enumerate(BLKS[1:]):
                    n = t1 - t0
                    la = t0 - NCTX
                    z_ = zb[bi % 2]
                    S.dma("sp", z_[:, :n], U1("c%d" % gch)[gch, :, t0:t1])
                    S.act(sqb[:, :n], oacc[:, hh, la:la + n], AF.Square)
                    p0 = self.pb[0]
                    S.mm(p0[:, :n], self.onesb, sqb[:, :n])
                    S.ts("dve", rs[:, :n], p0[:, :n], 1.0 / 128.0, EPS, op0=ALU.mult, op1=ALU.add)
                    S.act(rs[:, :n], rs[:, :n], AF.Sqrt)
                    S.recip(rs[:, :n], rs[:, :n])
                    S.act(z_[:, :n], z_[:, :n], AF.Silu)
                    S.stt("dve", rs[:, :n], oacc[:, hh, la:la + n], self.col(gname, 0, 1), rs[:, :n], ALU.mult, ALU.mult)
                    S.tt("dve", XM[:, xch, t0:t1], rs[:, :n], z_[:, :n], ALU.mult)


def mixer1(self, XM):
    S = self.S
    with S.scope() as es2:
        mkf = S.sb(es2, "mkf", [128, 512], F32)
        mkb = S.sb(es2, "mkb", [128, 512], F32)
        S.memset("pool", mkf[:], 1.0)
        S.memset("pool", V(mkf.h[:, 0:512:64], (mkf.name, None)), 0.0)
        S.memset("pool", mkb[:], 1.0)
        S.memset("pool", V(mkb.h[:, 63:512:64], (mkb.name, None)), 0.0)
        for h in range(4):
            gla_unit(self, XM, "c", h, (mkf, mkb))
        for hp in range(2):
            gla_unit(self, XM, "d", hp, (mkf, mkb))


AX = mybir.AxisListType


def moe(self, XM):
    S = self.S
    hT = self.hT
    md = self.mod[1]
    NTL = NLAT // 128
    with S.scope() as es2:
        gtm = S.sb(es2, "gtm", [128, NTL, NEXP], F32)
        rw = S.sb(es2, "rw", [128, KC, NEXP], F32)
        rb = S.sb(es2, "rb", [128, NEXP], F32)
        S.dma("sp", rw[:], self.din("o_router_w", [1, D, NEXP])[0].rearrange("(kc p) e -> p kc e", p=128))
        S.dma("sp", rb[:], self.din("rb128", [128, NEXP])[:, :])
        g0 = 3
        with S.scope() as es3:
            gs = S.sb(es3, "gs2", [128, KC], F32)
            sq = [S.sb(es3, "sq2%d" % i, [128, 512], BF16) for i in range(2)]
            rsb = S.sb(es3, "rsb2", [128, 512], F32)
            tmp = [S.sb(es3, "mtmp2%d" % i, [128, 512], F32) for i in range(2)]
            xr = S.sb(es3, "xr", [128, KC, 512], F32)
            sm = S.sb(es3, "sm", [128, 8, NEXP], F32)
            sc = S.sb(es3, "smc", [128, 8], F32)
            S.ts("dve", gs[:], md[:, (g0 + 1) * 8:(g0 + 2) * 8, 0], 1.0, 32.0, op0=ALU.add, op1=ALU.mult)
            S.tt("dve", gs[:], gs[:], self.col("nffn1", 0, 8), ALU.mult)
            for (t0, t1) in BLKS[1:]:
                n = t1 - t0
                ss = self.pb[0]
                for kc in range(KC):
                    s = sq[kc % 2]
                    S.act(s[:, :n], hT[:, kc, t0:t1], AF.Square)
                    S.mm(ss[:, :n], self.onesb, s[:, :n], start=(kc == 0), stop=(kc == KC - 1))
                S.ts("dve", rsb[:, :n], ss[:, :n], 1024.0 * EPS, None, op0=ALU.add)
                S.act(rsb[:, :n], rsb[:, :n], AF.Sqrt)
                S.recip(rsb[:, :n], rsb[:, :n])
                for kc in range(KC):
                    tm = tmp[kc % 2]
                    S.tt("dve", tm[:, :n], hT[:, kc, t0:t1], rsb[:, :n], ALU.mult)
                    S.act(XM[:, kc, t0:t1], tm[:, :n], AF.Identity, bias=md[:, g0 * 8 + kc, 0:1], scale=gs[:, kc:kc + 1])
                    S.ts("dve", xr[:, kc, :n], tm[:, :n], gs[:, kc:kc + 1], md[:, g0 * 8 + kc, 0:1], op0=ALU.mult, op1=ALU.add)
                for q in range(4):
                    ti = (t0 - NCTX) // 128 + q
                    pl = self.pb[1]
                    for kc in range(KC):
                        S.mm(pl[:, 0:NEXP], xr[:, kc, q * 128:(q + 1) * 128], rw[:, kc, :], start=(kc == 0), stop=(kc == KC - 1))
                    lg, eq, l2, e_ = sm[:, 0, :], sm[:, 1, :], sm[:, 2, :], sm[:, 3, :]
                    m1, m2, nm1, dd = sc[:, 0:1], sc[:, 1:2], sc[:, 2:3], sc[:, 3:4]
                    S.tt("dve", lg, pl[:, 0:NEXP], rb[:], ALU.add)
                    S.op("dve", lambda: self.nc.vector.reduce_max(out=_ap(m1), in_=_ap(lg), axis=AX.X), [lg], [m1])
                    S.ts("dve", eq, lg, m1, None, op0=ALU.is_equal)
                    S.stt("dve", l2, eq, -1e30, lg, ALU.mult, ALU.add)
                    S.op("dve", lambda: self.nc.vector.reduce_max(out=_ap(m2), in_=_ap(l2), axis=AX.X), [l2], [m2])
                    S.ts("dve", eq, lg, m2, None, op0=ALU.is_ge)
                    S.ts("dve", nm1, m1, -1.0, None, op0=ALU.mult)
                    S.act(e_, lg, AF.Exp, bias=nm1)
                    S.tt("dve", dd, m2, m1, ALU.subtract)
                    S.act(dd, dd, AF.Exp)
                    S.ts("dve", dd, dd, 1.0, None, op0=ALU.add)
                    S.recip(dd, dd)
                    S.tt("dve", e_, e_, eq, ALU.mult)
                    S.ts("dve", gtm[:, ti, :], e_, dd, None, op0=ALU.mult)
        self.tap("gates", gtm[:], [128, NTL, NEXP])
        with S.scope() as es3:
            wb = alloc_wbufs(self, es3)
            Gb = S.sb(es3, "Gb", [128, NLAT], F32)
            dg = [S.sb(es3, "dg%d" % i, [128, 128], F32) for i in range(2)]
            wg_d = self.din("o_moe_w_gate", [1, NEXP, D, DFF])[0]
            wu_d = self.din("o_moe_w_up", [1, NEXP, D, DFF])[0]
            wd_d = self.din("o_moe_w_down", [1, NEXP, DFF, D])[0]
            for e in range(NEXP):
                if ("moe_e%d" % e) in self.stages:
                    continue
                for b4 in range(4):
                    pG = self.pb[6 + b4 % 2]
                    for q in range(4):
                        ti = b4 * 4 + q
                        dq_ = dg[q % 2]
                        S.ts("dve", dq_[:], self.cst[:, 0, :], gtm[:, ti, e:e + 1], None, op0=ALU.mult)
                        S.mm(pG[:, q * 128:(q + 1) * 128], self.cst[:, 1, :], dq_[:])
                    S.cp("act", Gb[:, b4 * 512:(b4 + 1) * 512], pG[:, :])
                swiglu(self, 1, XM, wg_d[e], wu_d[e], wd_d[e], nblk=4, wbufs=wb,
                       gate_fn=lambda bi, blk: Gb[:, blk[0] - NCTX:blk[1] - NCTX])


def final(self):
    S = self.S
    hT = self.hT
    out_d = self.nc.dram_tensor("out", [NLAT, D], F32, kind="ExternalOutput").ap()
    with S.scope() as es2:
        g32 = S.sb(es2, "g32", [128, KC], F32)
        sq = [S.sb(es2, "sqf%d" % i, [128, 512], BF16) for i in range(2)]
        rsb = S.sb(es2, "rsbf", [128, 512], F32)
        yT = [S.sb(es2, "yT%d" % i, [128, 512], F32) for i in range(2)]
        ot = [S.sb(es2, "ot%d" % i, [128, D], F32) for i in range(4)]
        S.ts("dve", g32[:], self.col("nfin", 0, 8), 32.0, None, op0=ALU.mult)
        for bi, (t0, t1) in enumerate(BLKS[1:]):
            n = t1 - t0
            ss = self.pb[0]
            for kc in range(KC):
                s = sq[kc % 2]
                S.act(s[:, :n], hT[:, kc, t0:t1], AF.Square)
                S.mm(ss[:, :n], self.onesb, s[:, :n], start=(kc == 0), stop=(kc == KC - 1))
            S.ts("dve", rsb[:, :n], ss[:, :n], 1024.0 * EPS, None, op0=ALU.add)
            S.act(rsb[:, :n], rsb[:, :n], AF.Sqrt)
            S.recip(rsb[:, :n], rsb[:, :n])
            for kc in range(KC):
                y = yT[kc % 2]
                S.stt("dve", y[:, :n], hT[:, kc, t0:t1], g32[:, kc:kc + 1], rsb[:, :n], ALU.mult, ALU.mult)
                pt = self.pb[2 + kc % 4]
                for q in range(4):
                    S.tr(pt[:, q * 128:(q + 1) * 128], y[:, q * 128:(q + 1) * 128], self.ident)
                for q in range(4):
                    S.cp("act" if q % 2 == 0 else "dve", ot[q][:, kc * 128:(kc + 1) * 128], pt[:, q * 128:(q + 1) * 128])
            for q in range(4):
                r0 = t0 - NCTX + q * 128
                tok = S.dma("sp", out_d[r0:r0 + 128, :], ot[q][:])
                S.out_tokens.append(tok)


Prog.final = final


ALL_STAGES = ["L0", "inproj0", "rglru", "deltanet", "outproj", "ffn0", "L1", "FIN"]
_CACHE = {}


def kernel(**inputs):
    inp = {k: np.asarray(v) for k, v in inputs.items()}
    if "prog" not in _CACHE:
        P = Prog(ALL_STAGES)
        P.build()
        _CACHE["prog"] = P
    P = _CACHE["prog"]
    B = inp["x"].shape[0]
    maps = [in_map(inp, b, P.inputs) for b in range(B)]
    res = run_bass_kernel_spmd(P.nc, maps, core_ids=list(range(B)))
    return np.stack([np.asarray(res.results[b]["out"]) for b in range(B)], 0).astype(np.float32)
```

```python
import numpy as np
from contextlib import ExitStack
import concourse.bass as bass
import concourse.mybir as mybir
from concourse.bass_utils import run_bass_kernel_spmd

F32 = mybir.dt.float32
F32R = mybir.dt.float32r
BF16 = mybir.dt.bfloat16
AF = mybir.ActivationFunctionType
ALU = mybir.AluOpType

D = 1024
KC = 8
NCTX = 256
NLAT = 2048
TT = NCTX + NLAT
EPS = 1e-6
E_IN = 3088
O_IN = 4128
DFF = 2816
FC = DFF // 128
NEXP = 8
BLKS = [(0, 256), (256, 768), (768, 1280), (1280, 1792), (1792, 2304)]


class V:
    __slots__ = ("ap", "dep")

    def __init__(self, ap, dep):
        self.ap = ap
        self.dep = dep


class _Idx:
    def __init__(self, t, key):
        self.t = t
        self.key = key

    def __getitem__(self, sl):
        return V(self.t.h[sl], (self.t.name, self.key))


class TT_:
    def __init__(self, h, name):
        self.h = h
        self.name = name

    def __getitem__(self, sl):
        return V(self.h[sl], (self.name, None))

    def __call__(self, key):
        return _Idx(self, key)


def _ap(x):
    return x.ap if isinstance(x, V) else x


def r32v(v):
    return V(v.ap.bitcast(F32R), v.dep) if R32 else v


class Sched:
    ENG = ("pe", "dve", "act", "pool", "sp")
    NDS = 24

    def __init__(self, nc, es):
        self.nc = nc
        self.es = es
        self.eng = dict(pe=nc.tensor, dve=nc.vector, act=nc.scalar, pool=nc.gpsimd, sp=nc.sync)
        self.sem = {e: es.enter_context(nc.semaphore("s_" + e)) for e in self.ENG}
        self.cnt = {e: 0 for e in self.ENG}
        self.seen = {e: {} for e in self.ENG}
        self.dsem = [es.enter_context(nc.semaphore("d%d" % i)) for i in range(self.NDS)]
        self.dval = [0] * self.NDS
        self.dnext = 0
        self.state = {}
        self.uid = 0
        self.out_tokens = []
        self.excl = set()

    def sb(self, es, name, shape, dt):
        self.uid += 1
        nm = "%s_%d" % (name, self.uid)
        return TT_(es.enter_context(self.nc.sbuf_tensor(nm, list(shape), dt)), nm)

    def ps(self, es, name, shape, dt=F32):
        self.uid += 1
        nm = "%s_%d" % (name, self.uid)
        self.excl.add(nm)
        return TT_(es.enter_context(self.nc.psum_tensor(nm, list(shape), dt)), nm)

    def dram(self, name, shape, dt, kind="Internal"):
        t = self.nc.dram_tensor(name, list(shape), dt, kind=kind)
        return TT_(t.ap(), name)

    def _entries(self, dep):
        tid, key = dep
        st = self.state.setdefault(tid, {})
        if None not in st:
            st[None] = [None, {}]
        if key is None:
            return list(st.values())
        if key not in st:
            st[key] = [None, {}]
        return [st[key], st[None]]

    def _wait(self, e, tok):
        if tok is None:
            return
        k, v = tok
        if k == e and e in ("pe", "sp"):
            return
        if self.seen[e].get(k, 0) >= v:
            return
        sem = self.sem[k] if isinstance(k, str) else self.dsem[k[1]]
        self.eng[e].wait_ge(sem, v)
        self.seen[e][k] = v

    def _pre(self, e, R, W):
        for v in R:
            if isinstance(v, V):
                for ent in self._entries(v.dep):
                    self._wait(e, ent[0])
        for v in W:
            if isinstance(v, V):
                for ent in self._entries(v.dep):
                    self._wait(e, ent[0])
                    for k, val in ent[1].items():
                        self._wait(e, (k, val))

    def _post(self, tok, R, W):
        k, val = tok
        for v in R:
            if isinstance(v, V):
                tid, key = v.dep
                ents = self._entries(v.dep)
                tgt = ents if key is None else ents[:1]
                for ent in tgt:
                    if ent[1].get(k, 0) < val:
                        ent[1][k] = val
        for v in W:
            if isinstance(v, V):
                tid, key = v.dep
                ents = self._entries(v.dep)
                tgt = ents if key is None else ents[:1]
                for ent in tgt:
                    ent[0] = tok
                    ent[1] = {}

    dead = False
    last_pool_dma = None

    def op(self, e, fn, R, W):
        if self.dead:
            return
        Rx = [v for v in R if isinstance(v, V) and v.dep[0] in self.excl]
        if Rx:
            R = [v for v in R if not (isinstance(v, V) and v.dep[0] in self.excl)]
            W = list(W) + Rx
        self._pre(e, R, W)
        ins = fn()
        self.cnt[e] += 1
        ins.then_inc(self.sem[e], 1)
        self._post((e, self.cnt[e]), R, W)

    def dma(self, q, out, in_, **kw):
        if self.dead:
            return None
        i = self.dnext
        self.dnext = (i + 1) % self.NDS
        if self.dval[i] > 0:
            self._wait(q, (("d", i), self.dval[i]))
        if q == "pool" and self.last_pool_dma is not None:
            self._wait(q, self.last_pool_dma)
        self._pre(q, [in_], [out])
        ins = self.eng[q].dma_start(out=_ap(out), in_=_ap(in_), **kw)
        self.dval[i] += 16
        ins.then_inc(self.dsem[i], 16)
        tok = (("d", i), self.dval[i])
        if q == "pool":
            self.last_pool_dma = tok
        self._post(tok, [in_], [out])
        return tok

    def scope(self):
        sched = self

        class _Scope(ExitStack):
            def __exit__(self, *a):
                sched.barrier()
                return super().__exit__(*a)

        return _Scope()

    def barrier(self):
        for e in self.ENG:
            for f in self.ENG:
                if f != e and self.cnt[f] > 0:
                    self._wait(e, (f, self.cnt[f]))
            for i in range(self.NDS):
                if self.dval[i] > 0:
                    self._wait(e, (("d", i), self.dval[i]))

    def mm(self, out, lhsT, rhs, start=True, stop=True, r32=False):
        if r32:
            self.op("pe", lambda: self.nc.tensor.matmul(_ap(out), lhsT=_ap(lhsT).bitcast(F32R), rhs=_ap(rhs).bitcast(F32R), start=start, stop=stop),
                    [lhsT, rhs], [out])
            return
        self.op("pe", lambda: self.nc.tensor.matmul(_ap(out), lhsT=_ap(lhsT), rhs=_ap(rhs), start=start, stop=stop),
                [lhsT, rhs], [out])

    def tr(self, out, in_, ident):
        self.op("pe", lambda: self.nc.tensor.transpose(_ap(out), _ap(in_), _ap(ident)), [in_, ident], [out])

    def act(self, out, in_, func, bias=None, scale=None, accum=None):
        kw = {}
        R = [in_]
        if bias is not None:
            kw["bias"] = _ap(bias)
            R.append(bias)
        if scale is not None:
            kw["scale"] = _ap(scale)
            R.append(scale)
        W = [out]
        if accum is not None:
            kw["accum_out"] = _ap(accum)
            W.append(accum)
        self.op("act", lambda: self.nc.scalar.activation(out=_ap(out), in_=_ap(in_), func=func, **kw), R, W)

    def tt(self, e, out, in0, in1, op):
        self.op(e, lambda: self.eng[e].tensor_tensor(out=_ap(out), in0=_ap(in0), in1=_ap(in1), op=op), [in0, in1], [out])

    def ts(self, e, out, in0, s1, s2=None, op0=ALU.mult, op1=None, accum=None):
        kw = {}
        if op1 is not None:
            kw["op1"] = op1
        W = [out]
        if accum is not None:
            kw["accum_out"] = _ap(accum)
            W.append(accum)
        self.op(e, lambda: self.eng[e].tensor_scalar(out=_ap(out), in0=_ap(in0), scalar1=_ap(s1), scalar2=_ap(s2), op0=op0, **kw),
                [in0, s1, s2], W)

    def stt(self, e, out, in0, scalar, in1, op0, op1):
        self.op(e, lambda: self.eng[e].scalar_tensor_tensor(out=_ap(out), in0=_ap(in0), scalar=_ap(scalar), in1=_ap(in1), op0=op0, op1=op1),
                [in0, scalar, in1], [out])

    def cp(self, e, out, in_):
        if e == "act":
            self.op(e, lambda: self.nc.scalar.copy(out=_ap(out), in_=_ap(in_)), [in_], [out])
        else:
            self.op(e, lambda: self.eng[e].tensor_copy(out=_ap(out), in_=_ap(in_)), [in_], [out])

    def scan(self, out, d0, d1, init, op0=ALU.mult, op1=ALU.add):
        self.op("dve", lambda: self.nc.vector.tensor_tensor_scan(out=_ap(out), data0=_ap(d0), data1=_ap(d1), initial=_ap(init), op0=op0, op1=op1),
                [d0, d1, init], [out])

    def memset(self, e, out, val):
        self.op(e, lambda: self.eng[e].memset(_ap(out), val), [], [out])

    def recip(self, out, in_):
        self.op("dve", lambda: self.nc.vector.reciprocal(out=_ap(out), in_=_ap(in_)), [in_], [out])

    def iota_like_select(self, out, in_, pattern, cmp, fill, base, cm):
        self.op("pool", lambda: self.nc.gpsimd.affine_select(out=_ap(out), in_=_ap(in_), pattern=pattern, compare_op=cmp, fill=fill,
                                                              base=base, channel_multiplier=cm), [in_], [out])


def _consts():
    j = np.arange(128)[:, None]
    i = np.arange(128)[None, :]
    c = np.zeros((128, 8, 128), np.float32)
    c[:, 0] = (j == i)
    c[:, 1] = 1.0
    c[:, 2] = (j <= i)
    c[:, 3] = (j < i)
    c[:, 4] = (j >= i)
    c[:, 5] = (j > i)
    same = (j // 64) == (i // 64)
    c[:, 6] = same & (j <= i)
    c[:, 7] = same & (j >= i)
    return c


PROWS = {}


def _pack_rows(inp, b):
    parts = []
    off = [0]

    def add(name, arr):
        a = np.ascontiguousarray(arr, dtype=np.float32).reshape(-1, 128)
        PROWS[name] = (off[0], a.shape[0])
        off[0] += a.shape[0]
        parts.append(a)

    add("c", inp["c"][b])
    add("c_ctx", inp["c_ctx"])
    add("ada_b0", inp["ada_b"][0])
    add("ada_b1", inp["ada_b"][1])
    add("nmix0", inp["norm_mix_g"][0])
    add("nmix1", inp["norm_mix_g"][1])
    add("nffn0", inp["norm_ffn_g"][0])
    add("nffn1", inp["norm_ffn_g"][1])
    add("nfin", inp["final_norm_g"])
    add("a_conv_w", inp["e_a_conv_w"][0])
    add("a_conv_b", inp["e_a_conv_b"][0])
    add("a_gate_b", inp["e_a_gate_b"][0])
    add("a_lambda", inp["e_a_lambda"][0])
    add("b_conv_w", inp["e_b_conv_w"][0])
    add("b_norm_g", inp["e_b_norm_g"][0])
    add("lb_logits", inp["o_lb_logits"])
    add("c_norm_g", inp["o_c_norm_g"][0])
    add("d_gate_b2", inp["o_d_gate_b2"][0])
    add("d_norm_g", inp["o_d_norm_g"][0])
    rows = np.concatenate(parts, 0)
    pad = (-rows.shape[0]) % 128
    if pad:
        rows = np.concatenate([rows, np.zeros((pad, 128), np.float32)], 0)
    return rows


NPROW = 384
R32 = False
DBL_DT = F32
PROWS_STATIC = {'c': (0, 8), 'c_ctx': (8, 8), 'ada_b0': (16, 48), 'ada_b1': (64, 48), 'nmix0': (112, 8), 'nmix1': (120, 8),
                'nffn0': (128, 8), 'nffn1': (136, 8), 'nfin': (144, 8), 'a_conv_w': (152, 16), 'a_conv_b': (168, 4),
                'a_gate_b': (172, 16), 'a_lambda': (188, 8), 'b_conv_w': (196, 48), 'b_norm_g': (244, 1), 'lb_logits': (245, 8),
                'c_norm_g': (253, 1), 'd_gate_b2': (254, 4), 'd_norm_g': (258, 1)}


TAP_SHAPES = {"wgtap": [128, KC, 512], "h0": [128, KC, TT], "mod0": [128, 48, 2], "mod1": [128, 48, 2], "xn0": [128, KC, TT], "rsb": [128, 512],
              "gs": [128, KC, 2], "ha0": [128, TT], "ha3": [128, TT], "ob0": [128, TT], "ob3": [128, TT], "dnrows": [128, TT],
              "dngtb": [128, 8, 18], "dnG": [64, TT], "hmid0": [128, KC, TT], "h1": [128, KC, TT], "xn1": [128, KC, TT],
              "hmid1": [128, KC, TT], "oc0": [128, NLAT], "od0": [128, NLAT], "h2": [128, KC, TT], "gates": [128, 16, 8]}


class _Stop(Exception):
    pass


class Prog:
    cut = None

    ckn = {}

    def ck(self, name):
        k = self.ckn.get(name, 0)
        self.ckn[name] = k + 1
        if self.cut == "%s#%d" % (name, k):
            self.S.dead = True

    def __init__(self, stages, taps=()):
        self.stages = stages
        self.taps = set(taps)
        self.nc = bass.Bass("TRN2", target_bir_lowering=False)
        self.inputs = {}
        self.tap_out = {}

    def din(self, name, shape, dt=F32):
        if name not in self.inputs:
            self.inputs[name] = self.nc.dram_tensor(name, list(shape), dt, kind="ExternalInput").ap()
        return self.inputs[name]

    def tap(self, name, src, shape):
        if name not in self.taps:
            return
        S = self.S
        t = self.tap_out[name]
        q = "sp" if _ap(src).tensor.dtype == F32 else "pool"
        tok = S.dma(q, t, src)
        S.out_tokens.append(tok)

    def build(self):
        nc = self.nc
        with ExitStack() as es:
            S = self.S = Sched(nc, es)
            self.es = es
            for name in sorted(self.taps):
                self.tap_out[name] = nc.dram_tensor("tap_" + name, list(TAP_SHAPES[name]), F32, kind="ExternalOutput").ap()
            try:
                self.setup()
                for l in range(2):
                    if ("L%d" % l) not in self.stages:
                        continue
                    self.layer(l)
                if "FIN" in self.stages:
                    self.final()
            except _Stop:
                pass
            S.barrier()
        return nc

    def setup(self):
        S, nc, es = self.S, self.nc, self.es
        self.pb = [S.ps(es, "pb%d" % i, [128, 512]) for i in range(8)]
        cst = self.cst = S.sb(es, "cst", [128, 8, 128], F32)
        S.dma("sp", cst[:], self.din("consts", [128, 8, 128])[:, :, :])
        cstb = self.cstb = S.sb(es, "cstb", [128, 8, 128], BF16)
        S.cp("dve", cstb[:], cst[:])
        self.ident = cst[:, 0, :]
        ncst = self.ncst = S.sb(es, "ncst", [128, 2, 128], F32)
        S.ts("dve", ncst[:, 0, :], cst[:, 3, :], -1.0, None, op0=ALU.mult)
        S.ts("dve", ncst[:, 1, :], cst[:, 5, :], -1.0, None, op0=ALU.mult)
        self.identb = cstb[:, 0, :]
        self.onesb = cstb[:, 1, :]
        pc = self.pc = S.sb(es, "pc", [128, NPROW], F32)
        prow_d = self.din("prows", [NPROW, 128])
        with S.scope() as es2:
            stg = S.sb(es2, "pstg", [128, NPROW // 128, 128], F32)
            for g in range(NPROW // 128):
                S.dma("sp", stg[:, g, :], prow_d[g * 128:(g + 1) * 128, :])
                pt = self.pb[g % 2]
                S.tr(pt[:, 0:128], stg[:, g, :], self.ident)
                S.cp("dve", pc[:, g * 128:(g + 1) * 128], pt[:, 0:128])
            S.barrier()
        hT = self.hT = S.sb(es, "hT", [128, KC, TT], F32)
        x_d = self.din("x", [NLAT, D])
        ctx_d = self.din("ctx", [NCTX, D])
        with S.scope() as es2:
            xin = [S.sb(es2, "xin%d" % i, [128, D], F32) for i in range(2)]
            for ti in range(TT // 128):
                t0 = ti * 128
                src = ctx_d[t0:t0 + 128, :] if t0 < NCTX else x_d[t0 - NCTX:t0 - NCTX + 128, :]
                xt = xin[ti % 2]
                S.dma("sp", xt[:], src)
                for half in range(2):
                    pt = self.pb[(ti * 2 + half) % 4]
                    for q in range(4):
                        kc = half * 4 + q
                        S.tr(pt[:, q * 128:(q + 1) * 128], xt[:, kc * 128:(kc + 1) * 128], self.ident)
                    S.cp("dve" if half == 0 else "act", hT("t%d" % ti)[:, half * 4:half * 4 + 4, t0:t0 + 128],
                         V(pt.h[:].rearrange("p (q t) -> p q t", q=4), (pt.name, None)))
            S.barrier()
        self.tap("h0", hT[:], [128, KC, TT])
        self.mods()

    def col(self, name, r=0, n=1):
        off, cnt = PROWS_STATIC[name]
        return self.pc[:, off + r:off + r + n]

    def mods(self):
        S, nc, es = self.S, self.nc, self.es
        sT = self.sT = S.sb(es, "sT", [128, KC, 2], F32)
        S.act(sT[:, :, 0], self.col("c", 0, 8), AF.Silu)
        S.act(sT[:, :, 1], self.col("c_ctx", 0, 8), AF.Silu)
        self.mod = [S.sb(es, "mod%d" % l, [128, 48, 2], F32) for l in range(2)]
        with S.scope() as es2:
            wb = [S.sb(es2, "adaw%d" % i, [128, KC, 256], F32) for i in range(2)]
            interleave(self.mods_gen(0, wb))
            if "rglru" not in self.stages:
                interleave(self.mods_gen(1, wb))
        for l in range(2):
            self.tap("mod%d" % l, self.mod[l][:], [128, 48, 2])

    def mods_gen(self, l, wb):
        S = self.S
        ada_d = self.din("ada_w", [2, D, 6 * D])
        md = self.mod[l]
        sT = self.sT
        for g in range(24):
            w = wb[g % 2]
            S.dma("sp", w[:], ada_d[l, :, g * 256:(g + 1) * 256].rearrange("(kc p) n -> p kc n", p=128))
            pt = self.pb[6 + (g % 2)]
            for q in range(2):
                for kc in range(KC):
                    S.mm(pt[:, q * 2:q * 2 + 2], w[:, kc, q * 128:(q + 1) * 128], sT[:, kc, :], start=(kc == 0), stop=(kc == KC - 1))
            yield
            for q in range(2):
                j = g * 2 + q
                S.ts("dve", md[:, j, :], pt[:, q * 2:q * 2 + 2], self.col("ada_b%d" % l, j, 1), None, op0=ALU.add)
            yield


def in_map(inp, b, needed):
    m = {}
    for name in needed:
        if name == "consts":
            m[name] = _consts()
        elif name == "prows":
            m[name] = _pack_rows(inp, b)
        elif name == "x":
            m[name] = np.ascontiguousarray(inp["x"][b])
        elif name == "ctx":
            m[name] = np.ascontiguousarray(inp["ctx"][b])
        elif name == "rb128":
            m[name] = np.ascontiguousarray(np.broadcast_to(inp["o_router_b"][0][None, :], (128, NEXP)))
        elif name == "dn_sc":
            m[name] = np.ascontiguousarray(np.stack([inp["e_b_a_log"][0].reshape(8), inp["e_b_dt_bias"][0].reshape(8)], 1))
        else:
            a = inp[name]
            m[name] = np.ascontiguousarray(a)
    return m


def _layer_methods():
    pass


def modulate(self, l, which, xn, perm=False, nblk=5):
    S, es = self.S, self.es
    hT = self.hT
    g0 = 0 if which == 0 else 3
    gname = ("nmix%d" if which == 0 else "nffn%d") % l
    md = self.mod[l]
    with S.scope() as es2:
        gs = S.sb(es2, "gs", [128, KC, 2], F32)
        sq = [S.sb(es2, "sq%d" % i, [128, 512], BF16) for i in range(2)]
        rsb = S.sb(es2, "rsb", [128, 512], F32)
        tmp = [S.sb(es2, "mtmp%d" % i, [128, 512], F32) for i in range(2)]
        for w in range(2):
            S.ts("dve", gs[:, :, w], md[:, (g0 + 1) * 8:(g0 + 2) * 8, w], 1.0, 32.0, op0=ALU.add, op1=ALU.mult)
            S.tt("dve", gs[:, :, w], gs[:, :, w], self.col(gname, 0, 8), ALU.mult)
        for (t0, t1) in BLKS[5 - nblk:]:
            w = 1 if t0 < NCTX else 0
            n = t1 - t0
            ss = self.pb[0]
            for kc in range(KC):
                s = sq[kc % 2]
                S.act(s[:, :n], hT[:, kc, t0:t1], AF.Square)
                S.mm(ss[:, :n], self.onesb, s[:, :n], start=(kc == 0), stop=(kc == KC - 1))
            S.ts("dve", rsb[:, :n], ss[:, :n], 1024.0 * EPS, None, op0=ALU.add)
            S.act(rsb[:, :n], rsb[:, :n], AF.Sqrt)
            S.recip(rsb[:, :n], rsb[:, :n])
            if t1 == TT:
                self.tap("rsb", rsb[:], [128, 512])
                self.tap("gs", gs[:], [128, KC, 2])
            for kc in range(KC):
                tm = tmp[kc % 2]
                S.tt("dve", tm[:, :n], hT[:, kc, t0:t1], rsb[:, :n], ALU.mult)
                if perm and t0 >= NCTX:
                    r0 = (t0 - NCTX) // 64
                    r1 = (t1 - NCTX) // 64
                    o = V(xn.h[:, kc, NCTX:TT].rearrange("p (c r) -> p r c", c=64, r=32)[:, r0:r1, :], (xn.name, None))
                    i_ = V(tm.h[:, :n].rearrange("p (r c) -> p r c", c=64), (tm.name, None))
                else:
                    o = xn[:, kc, t0:t1]
                    i_ = tm[:, :n]
                S.act(o, i_, AF.Identity, bias=md[:, g0 * 8 + kc, w:w + 1], scale=gs[:, kc, w:w + 1])


def proj(self, xn, w_d, N, consumer, nblk=5, wq="pool"):
    S, es = self.S, self.es
    with S.scope() as es2:
        wb = [S.sb(es2, "pw%d" % i, [128, KC, 512], BF16) for i in range(2)]
        ng = (N + 511) // 512
        pi = 0
        for g in range(ng):
            n0 = g * 512
            nn = min(512, N - n0)
            w = wb[g % 2]
            S.dma(wq, w[:, :, :nn], w_d[:, n0:n0 + nn].rearrange("(kc p) n -> p kc n", p=128))
            for q in range((nn + 127) // 128):
                m = min(128, nn - q * 128)
                for (t0, t1) in BLKS[5 - nblk:]:
                    n = t1 - t0
                    pt = self.pb[2 + (pi % 4)]
                    pi += 1
                    for kc in range(KC):
                        S.mm(pt[0:m, :n], w[:, kc, q * 128:q * 128 + m], xn[:, kc, t0:t1], start=(kc == 0), stop=(kc == KC - 1))
                    consumer(g * 4 + q, m, (t0, t1), pt[0:m, :n])


def conv4(self, out, x, wcols, bias=None):
    S = self.S
    for (a, b) in ((0, NCTX), (NCTX, TT)):
        if bias is None:
            S.ts("dve", out[:, a:b], x[:, a:b], wcols[2], None, op0=ALU.mult)
        else:
            S.ts("dve", out[:, a:b], x[:, a:b], wcols[2], bias, op0=ALU.mult, op1=ALU.add)
        S.stt("dve", out[:, a + 2:b], x[:, a:b - 2], wcols[0], out[:, a + 2:b], ALU.mult, ALU.add)
        S.stt("dve", out[:, a + 1:b], x[:, a:b - 1], wcols[1], out[:, a + 1:b], ALU.mult, ALU.add)
        S.stt("dve", out[:, a:b - 1], x[:, a + 1:b], wcols[3], out[:, a:b - 1], ALU.mult, ALU.add)


def inproj0(self, xn):
    S, es = self.S, self.es
    U0 = self.U0 = S.dram("U0", [25, 128, TT], F32)
    w_d = self.din("e_w_in", [1, D, E_IN])[0]
    with S.scope() as es2:
        pre = [S.sb(es2, "pre%d" % i, [128, TT], F32) for i in range(2)]
        post = [S.sb(es2, "post%d" % i, [128, TT], F32) for i in range(2)]
        cnt = [0]

        def consumer(ci, m, blk, pv):
            t0, t1 = blk
            p = pre[ci % 2]
            S.cp("act", p[0:m, t0:t1], pv)
            if t1 < TT:
                return
            po = post[ci % 2]
            if ci < 4:
                wc = [self.col("a_conv_w", k * 4 + ci, 1) for k in range(4)]
                conv4(self, po, p, wc, bias=self.col("a_conv_b", ci, 1))
                S.dma("sp", U0("c%d" % ci)[ci, :, :], po[:])
            elif 8 <= ci < 20:
                wc = [self.col("b_conv_w", k * 12 + (ci - 8), 1) for k in range(4)]
                conv4(self, po, p, wc)
                S.act(po[:], po[:], AF.Silu)
                S.dma("sp", U0("c%d" % ci)[ci, :, :], po[:])
            else:
                S.dma("sp", U0("c%d" % ci)[ci, 0:m, :], p[0:m, :])

        proj(self, xn, w_d, E_IN, consumer)
        S.barrier()


def rev(t, a, b):
    return V(t.h[:, a:b][:, ::-1], (t.name, None))


def rglru(self, XM):
    S, es = self.S, self.es
    U0 = self.U0
    gw_d = self.din("e_a_gate_w", [1, 2, 2, 8, 64, 64])[0]
    with S.scope() as es2:
        gwf = S.sb(es2, "gwf", [128, 16, 128], F32)
        gwb = S.sb(es2, "gwb", [128, 16, 128], BF16)
        nsp = S.sb(es2, "nsp", [128, 8], F32)
        ua = S.sb(es2, "ua", [128, TT], F32)
        uab = S.sb(es2, "uab", [128, TT], BF16)
        ra = S.sb(es2, "ra", [128, TT], F32)
        ii = S.sb(es2, "ii", [128, TT], F32)
        mx = S.sb(es2, "mx", [128, TT], F32)
        hh = [S.sb(es2, "hh%d" % i, [128, TT], F32) for i in range(2)]
        S.memset("pool", gwf[:], 0.0)
        for d in range(2):
            for g in range(2):
                for c in range(4):
                    for half in range(2):
                        S.dma("sp", gwf[half * 64:(half + 1) * 64, (d * 2 + g) * 4 + c, half * 64:(half + 1) * 64],
                              gw_d[d, g, 2 * c + half, :, :])
        S.cp("dve", gwb[:], gwf[:])
        S.act(nsp[:], self.col("a_lambda", 0, 8), AF.Exp, scale=-1.0)
        S.act(nsp[:], nsp[:], AF.Ln, bias=1.0)
        S.ts("dve", nsp[:], nsp[:], -8.0, None, op0=ALU.mult)
        wbm = [S.sb(es2, "adawr%d" % i, [128, KC, 256], F32) for i in range(2)]
        interleave(rglru_body(self, XM, U0, ua, uab, ra, ii, mx, hh, gwb, nsp), self.mods_gen(1, wbm))
        S.barrier()


def rglru_body(self, XM, U0, ua, uab, ra, ii, mx, hh, gwb, nsp):
    S = self.S
    if True:
        for c in range(4):
            S.dma("sp", ua[:], U0("c%d" % c)[c, :, :])
            S.cp("act", uab[:], ua[:])
            yield
            for d in range(2):
                for (t0, t1) in BLKS:
                    n = t1 - t0
                    pr, pi = self.pb[4], self.pb[5]
                    S.mm(pr[:, :n], gwb[:, (d * 2 + 0) * 4 + c, :], uab[:, t0:t1])
                    S.mm(pi[:, :n], gwb[:, (d * 2 + 1) * 4 + c, :], uab[:, t0:t1])
                    S.act(ra[:, t0:t1], pr[:, :n], AF.Sigmoid, bias=self.col("a_gate_b", (d * 2 + 0) * 4 + c, 1))
                    S.act(ii[:, t0:t1], pi[:, :n], AF.Sigmoid, bias=self.col("a_gate_b", (d * 2 + 1) * 4 + c, 1))
                    yield
                S.act(ra[:], ra[:], AF.Exp, scale=nsp[:, d * 4 + c:d * 4 + c + 1])
                yield
                S.tt("dve", mx[:], ra[:], ra[:], ALU.mult)
                S.act(mx[:], mx[:], AF.Sqrt, bias=1.0, scale=-1.0)
                yield
                S.tt("dve", mx[:], mx[:], ii[:], ALU.mult)
                S.tt("dve", mx[:], mx[:], ua[:], ALU.mult)
                h = hh[d]
                if d == 0:
                    S.scan(h[:], ra[:], mx[:], 0.0)
                else:
                    S.scan(rev(h, 0, NCTX), rev(ra, 0, NCTX), rev(mx, 0, NCTX), 0.0)
                    S.scan(rev(h, NCTX, TT), rev(ra, NCTX, TT), rev(mx, NCTX, TT), h[:, 0:1])
                yield
            S.tt("dve", hh[0][:], hh[0][:], hh[1][:], ALU.add)
            self.tap("ha%d" % c, hh[0][:], [128, TT])
            S.dma("sp", ii[:], U0("c%d" % (4 + c))[4 + c, :, :])
            gelu_tanh(self, mx, ii, ra)
            S.tt("dve", XM[:, c, :], hh[0][:], mx[:], ALU.mult)
            yield


def gelu_tanh(self, out, x, tmp):
    S = self.S
    S.tt("dve", tmp[:], x[:], x[:], ALU.mult)
    S.ts("dve", tmp[:], tmp[:], 0.044715, 1.0, op0=ALU.mult, op1=ALU.add)
    S.tt("dve", tmp[:], tmp[:], x[:], ALU.mult)
    S.act(tmp[:], tmp[:], AF.Tanh, scale=0.7978845608028654)
    S.ts("dve", tmp[:], tmp[:], 1.0, 0.5, op0=ALU.add, op1=ALU.mult)
    S.tt("dve", out[:], tmp[:], x[:], ALU.mult)


def layer(self, l):
    S, es = self.S, self.es
    with S.scope() as es2:
        XM = S.sb(es2, "XM", [128, KC, TT], BF16)
        if l == 0:
            modulate(self, 0, 0, XM)
            self.tap("xn0", XM[:], [128, KC, TT])
            if "inproj0" in self.stages:
                inproj0(self, XM)
            if "rglru" in self.stages:
                rglru(self, XM)
            if "deltanet" in self.stages:
                deltanet(self, XM)
                S.dead = False
                S.barrier()
            if "outproj" in self.stages:
                outproj(self, 0, XM, self.din("e_w_out", [1, D, D])[0])
                S.barrier()
                self.tap("hmid0", self.hT[:], [128, KC, TT])
            if "ffn0" in self.stages:
                ffn0(self, XM)
                self.tap("h1", self.hT[:], [128, KC, TT])
        if l == 1:
            modulate(self, 1, 0, XM, perm=True)
            inproj1(self, XM)
            mixer1(self, XM)
            S.dead = False
            outproj(self, 1, XM, self.din("o_w_out", [1, D, D])[0], perm=True, nblk=4)
            S.barrier()
            self.tap("hmid1", self.hT[:], [128, KC, TT])
            if "nomoe" not in self.stages:
                moe(self, XM)
                self.tap("h2", self.hT[:], [128, KC, TT])
        S.barrier()


Prog.layer = layer


def interleave(*gens):
    gens = [g for g in gens if g is not None]
    while gens:
        for g in list(gens):
            try:
                next(g)
            except StopIteration:
                gens.remove(g)


def chunk_order(nck_ctx, nck_all, d):
    if d == 0:
        return list(range(nck_all))
    return list(range(nck_ctx - 1, -1, -1)) + list(range(nck_all - 1, nck_ctx - 1, -1))


def deltanet(self, XM):
    S, es = self.S, self.es
    U0 = self.U0
    cst = self.cst
    NCK = TT // 128
    with S.scope() as es2:
        ROWS = S.sb(es2, "dnROWS", [128, TT], F32)
        GTb = S.sb(es2, "dnGTb", [128, 8, NCK], F32)
        oacc = S.sb(es2, "oacc", [128, TT], F32)
        with S.scope() as es3:
            G = S.sb(es3, "dnG", [64, TT], F32)
            BETA = S.sb(es3, "dnBeta", [64, TT], F32)
            GCS = S.sb(es3, "dnGCS", [64, TT], F32)
            TLS = S.sb(es3, "dnTLS", [64, TT], F32)
            sc = S.sb(es3, "dnsc", [64, 4], F32)
            sel = S.sb(es3, "dnsel", [128, 8, 128], F32)
            PF = S.sb(es3, "dnPF", [64, TT], F32)
            SF = S.sb(es3, "dnSF", [64, TT], F32)
            mk = S.sb(es3, "dnmk", [64, TT], F32)
            S.memset("pool", G[:], 0.0)
            S.memset("pool", BETA[:], 0.0)
            S.memset("pool", sc[:], 0.0)
            S.memset("pool", ROWS[:], 0.0)
            dn_sc = self.din("dn_sc", [8, 2])
            for d in range(2):
                S.dma("sp", G[32 * d:32 * d + 4, :], U0("c24")[24, 8 + 4 * d:12 + 4 * d, :])
                S.dma("sp", BETA[32 * d:32 * d + 4, :], U0("c24")[24, 4 * d:4 * d + 4, :])
                S.dma("sp", sc[32 * d:32 * d + 4, 0:2], dn_sc[4 * d:4 * d + 4, :])
            S.act(G[:], G[:], AF.Exp, bias=sc[:, 1:2])
            S.act(G[:], G[:], AF.Ln, bias=1.0)
            S.act(sc[:, 2:3], sc[:, 0:1], AF.Exp)
            S.ts("dve", sc[:, 2:3], sc[:, 2:3], -1.0, None, op0=ALU.mult)
            S.ts("dve", G[:], G[:], sc[:, 2:3], None, op0=ALU.mult)
            S.act(BETA[:], BETA[:], AF.Sigmoid)
            if "dn_stopA" in self.stages:
                self.tap("dnG", G[:], [64, TT])
                S.barrier()
                return
            S.memset("pool", mk[:], 1.0)
            S.memset("pool", V(mk.h[:, 0:TT:128], (mk.name, None)), 0.0)
            S.scan(PF[:], mk[:], G[:], 0.0)
            S.memset("pool", mk[:], 1.0)
            S.memset("pool", V(mk.h[:, 127:TT:128], (mk.name, None)), 0.0)
            S.scan(rev(SF, 0, TT), rev(mk, 0, TT), rev(G, 0, TT), 0.0)
            S.cp("dve", GCS[0:32, :], PF[0:32, :])
            S.cp("dve", GCS[32:64, :], SF[32:64, :])
            S.tt("dve", TLS[0:32, :], SF[0:32, :], G[0:32, :], ALU.subtract)
            S.tt("dve", TLS[32:64, :], PF[32:64, :], G[32:64, :], ALU.subtract)
            S.act(TLS[:], TLS[:], AF.Exp)
            if "dn_stopB" in self.stages:
                self.tap("dnG", GCS[:], [64, TT])
                S.barrier()
                return
            for d in range(2):
                for kind, src in enumerate((GCS, BETA, TLS)):
                    S.dma("sp", ROWS[32 * d + 4 * kind:32 * d + 4 * kind + 4, :], src[32 * d:32 * d + 4, :])
            if "dn_stopC" in self.stages:
                self.tap("dnG", ROWS[:], [64, TT])
                S.barrier()
                return
            for r8 in range(8):
                r = (r8 // 4) * 32 + (r8 % 4)
                S.ts("dve", sel[:, r8, :], cst[:, 1, :], cst[:, 0, r:r + 1], None, op0=ALU.mult)
            gte = S.sb(es3, "dngte", [128, NCK], F32)
            S.memset("dve", gte[:], 0.0)
            S.cp("dve", gte[0:64, :], V(PF.h[:, 127:TT:128], (PF.name, None)))
            for r8 in range(8):
                pt = self.pb[0]
                S.mm(pt[:, 0:NCK], sel[:, r8, :], gte[:])
                S.act(GTb[:, r8, :], pt[:, 0:NCK], AF.Exp)
            S.barrier()
        self.tap("dnrows", ROWS[:], [128, TT])
        self.tap("dngtb", GTb[:], [128, 8, NCK])
        if "dn_stop1" in self.stages:
            return

        for h in range(4):
            for d in range(2):
                r8 = d * 4 + h
                rr = d * 32 + h
                Mi = cst[:, 2, :] if d == 0 else cst[:, 4, :]
                Ms = self.ncst[:, 0, :] if d == 0 else self.ncst[:, 1, :]
                MsT = self.ncst[:, 1, :] if d == 0 else self.ncst[:, 0, :]
                with S.scope() as es3:
                    kdec = S.sb(es3, "kdec", [128, TT], BF16)
                    qdec = S.sb(es3, "qdec", [128, TT], BF16)
                    AT = S.sb(es3, "AT", [128, NCK, 128], BF16)
                    TTt = S.sb(es3, "TTt", [128, NCK, 128], F32)
                    vb = S.sb(es3, "vb", [128, NCK, 128], BF16)
                    kntm = S.sb(es3, "kntm", [128, NCK, 128], BF16)
                    cols = S.sb(es3, "cols", [128, NCK, 4], F32)
                    Sf = S.sb(es3, "Sf", [128, 128], F32)
                    Sb = S.sb(es3, "Sb", [128, 128], BF16)
                    sel2 = S.sb(es3, "sel2", [128, 2, 128], F32)
                    mk3 = S.sb(es3, "mk3", [128, 3, 128], F32)
                    qc = S.sb(es3, "qc", [128, 512], F32)
                    kc = S.sb(es3, "kc", [128, 512], F32)
                    vc = S.sb(es3, "vc", [128, 512], F32)
                    sqb = S.sb(es3, "sqb", [128, 512], BF16)
                    rs = S.sb(es3, "rs", [128, 512], F32)
                    GCb = S.sb(es3, "GCb", [128, 512], F32)
                    Eb = S.sb(es3, "Eb", [128, 512], F32)
                    knb = S.sb(es3, "knb", [128, 512], BF16)
                    kbb = S.sb(es3, "kbb", [128, 512], BF16)
                    qnb = S.sb(es3, "qnb", [128, 512], BF16)
                    Dm = S.sb(es3, "Dm", [128, 4, 128], F32)
                    Ds = S.sb(es3, "Ds", [128, 4, 128], F32)
                    DT = S.sb(es3, "DTm", [128, 4, 128], F32)
                    Mm = [S.sb(es3, "Mm%d" % i, [128, 2, 4, 128], DBL_DT) for i in range(2)]
                    Xx = [S.sb(es3, "Xx%d" % i, [128, 4, 128], DBL_DT) for i in range(2)]
                    rb = [S.sb(es3, "rb%d" % i, [128, 128], F32) for i in range(2)]
                    vnb = [S.sb(es3, "vnb%d" % i, [128, 128], BF16) for i in range(2)]
                    vns = [S.sb(es3, "vns%d" % i, [128, 128], BF16) for i in range(2)]
                    S.ts("dve", sel2[:, 0, :], cst[:, 1, :], cst[:, 0, rr:rr + 1], None, op0=ALU.mult)
                    S.ts("dve", sel2[:, 1, :], cst[:, 1, :], cst[:, 0, rr + 4:rr + 5], None, op0=ALU.mult)
                    Mi_, Ms_, MsT_ = (cst[:, 2, :], cst[:, 3, :], cst[:, 5, :]) if d == 0 else (cst[:, 4, :], cst[:, 5, :], cst[:, 3, :])
                    S.ts("dve", mk3[:, 0, :], Mi_, 1e30, -1e30, op0=ALU.mult, op1=ALU.add)
                    S.ts("dve", mk3[:, 1, :], Ms_, 1e30, -1e30, op0=ALU.mult, op1=ALU.add)
                    S.ts("dve", mk3[:, 2, :], MsT_, -1e30, 1e30, op0=ALU.mult, op1=ALU.add)
                    S.memset("dve", Sf[:], 0.0)
                    S.memset("dve", Sb[:], 0.0)

                    def pv3(pt, nck):
                        return V(pt.h[:, 0:nck * 128].rearrange("p (c t) -> p c t", c=nck), (pt.name, None))

                    def bc2(v, nck):
                        return V(_ap(v).unsqueeze(1).to_broadcast([128, nck, 128]), v.dep)

                    def prep_block(bi):
                        t0, t1 = BLKS[bi]
                        n = t1 - t0
                        nck = n // 128
                        c0 = t0 // 128
                        q_, k_, v_ = qc, kc, vc
                        S.dma("sp", q_[:, :n], U0("c%d" % (8 + h))[8 + h, :, t0:t1])
                        S.dma("sp", k_[:, :n], U0("c%d" % (12 + h))[12 + h, :, t0:t1])
                        S.dma("sp", v_[:, :n], U0("c%d" % (16 + h))[16 + h, :, t0:t1])
                        p0, p1, p2 = self.pb[0], self.pb[1], self.pb[2]
                        S.mm(p1[:, :n], sel2[:, 0, :], ROWS[:, t0:t1])
                        S.mm(p2[:, :n], sel2[:, 1, :], ROWS[:, t0:t1])
                        for cc in range(nck):
                            S.mm(p0[:, cc * 64:(cc + 1) * 64], ROWS[:, t0 + cc * 128:t0 + cc * 128 + 128], cst[:, 0, 0:64])
                        yield
                        S.cp("act", GCb[:, :n], p1[:, :n])
                        S.act(Eb[:, :n], p1[:, :n], AF.Exp)
                        colp = V(p0.h[:, 0:nck * 64].rearrange("p (c t) -> p c t", c=nck), (p0.name, None))
                        S.cp("dve", cols[:, c0:c0 + nck, 0], V(colp.ap[:, :, rr], colp.dep))
                        S.ts("dve", cols[:, c0:c0 + nck, 1], V(colp.ap[:, :, rr + 4], colp.dep), -1.0, None, op0=ALU.mult)
                        S.cp("dve", cols[:, c0:c0 + nck, 2], V(colp.ap[:, :, rr + 8], colp.dep))
                        S.cp("dve", cols[:, c0:c0 + nck, 3], V(colp.ap[:, :, rr + 4], colp.dep))
                        S.tt("dve", sqb[:, :n], k_[:, :n], k_[:, :n], ALU.mult)
                        yield
                        S.mm(p0[:, :n], self.onesb, sqb[:, :n])
                        yield
                        S.act(rs[:, :n], p0[:, :n], AF.Ln, bias=EPS)
                        S.act(rs[:, :n], rs[:, :n], AF.Exp, scale=-0.5)
                        yield
                        S.tt("dve", k_[:, :n], k_[:, :n], rs[:, :n], ALU.mult)
                        S.tt("pool", sqb[:, :n], q_[:, :n], q_[:, :n], ALU.mult)
                        S.tt("pool", kdec[:, t0:t1], k_[:, :n], Eb[:, :n], ALU.mult)
                        S.tt("dve", kbb[:, :n], k_[:, :n], p2[:, :n], ALU.mult)
                        yield
                        S.mm(p0[:, :n], self.onesb, sqb[:, :n])
                        S.cp("act", knb[:, :n], k_[:, :n])
                        yield
                        S.act(rs[:, :n], p0[:, :n], AF.Ln, bias=EPS)
                        S.act(rs[:, :n], rs[:, :n], AF.Exp, scale=-0.5)
                        pvt, pkt = self.pb[1], self.pb[2]
                        for cc in range(nck):
                            S.tr(pvt[:, cc * 128:(cc + 1) * 128], v_[:, cc * 128:(cc + 1) * 128], self.ident)
                        for cc in range(nck):
                            S.tr(pkt[:, cc * 128:(cc + 1) * 128], k_[:, cc * 128:(cc + 1) * 128], self.ident)
                        yield
                        S.stt("dve", q_[:, :n], q_[:, :n], 128.0 ** -0.5, rs[:, :n], ALU.mult, ALU.mult)
                        S.tt("pool", qdec[:, t0:t1], q_[:, :n], Eb[:, :n], ALU.mult)
                        bcol = V(cols.h[:, c0:c0 + nck, 3:4].to_broadcast([128, nck, 128]), (cols.name, None))
                        S.tt("dve", vb[:, c0:c0 + nck, :], pv3(pvt, nck), bcol, ALU.mult)
                        S.cp("act", qnb[:, :n], q_[:, :n])
                        S.cp("act", kntm[:, c0:c0 + nck, :], pv3(pkt, nck))
                        yield
                        pg1, pg2, pg3 = self.pb[3], self.pb[4], self.pb[5]
                        for cc in range(nck):
                            a_, b_ = cc * 128, cc * 128 + 128
                            S.mm(pg1[:, a_:b_], knb[:, a_:b_], kbb[:, a_:b_])
                        for cc in range(nck):
                            a_, b_ = cc * 128, cc * 128 + 128
                            S.mm(pg2[:, a_:b_], kbb[:, a_:b_], knb[:, a_:b_])
                        for cc in range(nck):
                            a_, b_ = cc * 128, cc * 128 + 128
                            S.mm(pg3[:, a_:b_], knb[:, a_:b_], qnb[:, a_:b_])
                        gcol = V(cols.h[:, c0:c0 + nck, 0:1].to_broadcast([128, nck, 128]), (cols.name, None))
                        gcb3 = V(GCb.h[:, 0:n].rearrange("p (c t) -> p c t", c=nck), (GCb.name, None))
                        S.tt("dve", Dm[:, 0:nck, :], gcb3, gcol, ALU.subtract)
                        S.stt("dve", DT[:, 0:nck, :], Dm[:, 0:nck, :], 0.0, bc2(mk3[:, 2, :], nck), ALU.max, ALU.add)
                        S.stt("dve", Ds[:, 0:nck, :], Dm[:, 0:nck, :], 0.0, bc2(mk3[:, 1, :], nck), ALU.min, ALU.add)
                        S.stt("dve", Dm[:, 0:nck, :], Dm[:, 0:nck, :], 0.0, bc2(mk3[:, 0, :], nck), ALU.min, ALU.add)
                        yield
                        S.act(DT[:, 0:nck, :], DT[:, 0:nck, :], AF.Exp, scale=-1.0)
                        S.act(Ds[:, 0:nck, :], Ds[:, 0:nck, :], AF.Exp)
                        S.act(Dm[:, 0:nck, :], Dm[:, 0:nck, :], AF.Exp)
                        yield
                        M0 = Mm[0]
                        S.stt("dve", r32v(M0[:, 0, 0:nck, :]), pv3(pg1, nck), -1.0, Ds[:, 0:nck, :], ALU.mult, ALU.mult)
                        S.stt("dve", r32v(M0[:, 1, 0:nck, :]), pv3(pg2, nck), -1.0, DT[:, 0:nck, :], ALU.mult, ALU.mult)
                        S.tt("dve", AT[:, c0:c0 + nck, :], pv3(pg3, nck), Dm[:, 0:nck, :], ALU.mult)
                        S.tt("dve", r32v(Xx[0][:, 0:nck, :]), M0[:, 0, 0:nck, :], bc2(cst[:, 0, :], nck), ALU.add)
                        yield
                        cur = 0
                        for lev in range(6):
                            Mc, Mn = Mm[cur], Mm[1 - cur]
                            pa, pbk, px = self.pb[3], self.pb[4], self.pb[5]
                            for cc in range(nck):
                                S.mm(pbk[:, cc * 128:(cc + 1) * 128], Mc("m")[:, 0, cc, :], Mc("t")[:, 1, cc, :], r32=R32)
                            if lev < 5:
                                for cc in range(nck):
                                    S.mm(pa[:, cc * 128:(cc + 1) * 128], Mc("t")[:, 1, cc, :], Mc("m")[:, 0, cc, :], r32=R32)
                            yield
                            S.cp("act", r32v(Mn("t")[:, 1, 0:nck, :]), pv3(pbk, nck))
                            yield
                            Xc, Xn = Xx[lev % 2], Xx[1 - lev % 2]
                            for cc in range(nck):
                                S.mm(px[:, cc * 128:(cc + 1) * 128], Mn("t")[:, 1, cc, :], Xc[:, cc, :], r32=R32)
                            if lev < 5:
                                S.cp("act", r32v(Mn("m")[:, 0, 0:nck, :]), pv3(pa, nck))
                            yield
                            if lev < 5:
                                S.tt("dve", r32v(Xn[:, 0:nck, :]), pv3(px, nck), Xc[:, 0:nck, :], ALU.add)
                            else:
                                S.tt("dve", TTt[:, c0:c0 + nck, :], pv3(px, nck), Xc[:, 0:nck, :], ALU.add)
                            cur = 1 - cur
                            yield

                    step = [0]

                    def rec_chunk(c):
                        si = step[0]
                        step[0] += 1
                        ta, tb = c * 128, c * 128 + 128
                        p6, p7 = self.pb[6], self.pb[7]
                        S.mm(p6[:, 0:128], kdec[:, ta:tb], Sb[:])
                        yield
                        r_ = rb[si % 2]
                        S.stt("dve", r_[:], p6[:, 0:128], cols[:, c, 1:2], vb[:, c, :], ALU.mult, ALU.add)
                        yield
                        S.mm(p6[:, 128:256], TTt[:, c, :], r_[:])
                        yield
                        vn, vs = vnb[si % 2], vns[si % 2]
                        S.cp("dve", vn[:], p6[:, 128:256])
                        S.ts("dve", vs[:], p6[:, 128:256], cols[:, c, 2:3], None, op0=ALU.mult)
                        yield
                        S.mm(p7[:, 128:256], kntm[:, c, :], vs[:])
                        S.mm(p7[:, 0:128], Sb[:], qdec[:, ta:tb], start=True, stop=False)
                        S.mm(p7[:, 0:128], vn[:], AT[:, c, :], start=False, stop=True)
                        yield
                        S.stt("dve", Sb[:], Sf[:], GTb[:, r8, c:c + 1], p7[:, 128:256], ALU.mult, ALU.add)
                        S.stt("dve", Sf[:], Sf[:], GTb[:, r8, c:c + 1], p7[:, 128:256], ALU.mult, ALU.add)
                        if d == 0:
                            S.cp("dve", oacc[:, ta:tb], p7[:, 0:128])
                        else:
                            S.tt("dve", oacc[:, ta:tb], oacc[:, ta:tb], p7[:, 0:128], ALU.add)
                        yield

                    def rec_block(bi):
                        t0, t1 = BLKS[bi]
                        cs = list(range(t0 // 128, t1 // 128))
                        if d == 1:
                            cs = cs[::-1]
                        for c in cs:
                            yield from rec_chunk(c)

                    border = [0, 1, 2, 3, 4] if d == 0 else [0, 4, 3, 2, 1]
                    for k_i in range(len(border) + 1):
                        gp = prep_block(border[k_i]) if k_i < len(border) else None
                        gr = rec_block(border[k_i - 1]) if k_i > 0 else None
                        interleave(gp, gr)
            self.tap("ob%d" % h, oacc[:], [128, TT])
            with S.scope() as es3:
                zb = [S.sb(es3, "zb%d" % i, [128, 512], F32) for i in range(2)]
                sqb = S.sb(es3, "gsq", [128, 512], BF16)
                rs = S.sb(es3, "grs", [128, 512], F32)
                for bi, (t0, t1) in enumerate(BLKS):
                    n = t1 - t0
                    z_ = zb[bi % 2]
                    S.dma("sp", z_[:, :n], U0("c%d" % (20 + h))[20 + h, :, t0:t1])
                    S.act(sqb[:, :n], oacc[:, t0:t1], AF.Square)
                    p0 = self.pb[0]
                    S.mm(p0[:, :n], self.onesb, sqb[:, :n])
                    S.ts("dve", rs[:, :n], p0[:, :n], 1.0 / 128.0, EPS, op0=ALU.mult, op1=ALU.add)
                    S.act(rs[:, :n], rs[:, :n], AF.Sqrt)
                    S.recip(rs[:, :n], rs[:, :n])
                    S.act(z_[:, :n], z_[:, :n], AF.Silu)
                    S.stt("dve", rs[:, :n], oacc[:, t0:t1], self.col("b_norm_g", 0, 1), rs[:, :n], ALU.mult, ALU.mult)
                    S.tt("dve", XM[:, 4 + h, t0:t1], rs[:, :n], z_[:, :n], ALU.mult)
                S.barrier()


def outproj(self, l, XM, w_d, perm=False, nblk=5):
    S = self.S
    hT = self.hT
    md = self.mod[l]

    def consumer(ci, m, blk, pv):
        t0, t1 = blk
        w = 1 if t0 < NCTX else 0
        if perm and t0 >= NCTX:
            c0 = (t0 - NCTX) // 32
            c1 = (t1 - NCTX) // 32
            hv = V(hT.h[:, ci, NCTX:TT].rearrange("p (r c) -> p c r", r=32, c=64)[:, c0:c1, :], (hT.name, None))
            pvv = V(_ap(pv).rearrange("p (c r) -> p c r", r=32), pv.dep)
            S.stt("dve", hv, pvv, md[:, 16 + ci, w:w + 1], hv, ALU.mult, ALU.add)
        else:
            S.stt("dve", hT[:, ci, t0:t1], pv, md[:, 16 + ci, w:w + 1], hT[:, ci, t0:t1], ALU.mult, ALU.add)

    proj(self, XM, w_d, D, consumer, nblk=nblk)


FGROUPS = [(0, 4), (4, 8), (8, 12), (12, 16), (16, 20), (20, 22)]


def swiglu(self, l, xn, wg_d, wu_d, wd_d, nblk=5, gate_fn=None, wbufs=None):
    S = self.S
    hT = self.hT
    md = self.mod[l]
    wgb, wub, wdb, actb, sgb = wbufs
    for gi, (c0, c1) in enumerate(FGROUPS):
        nf = c1 - c0
        u = self.wuse
        self.wuse += 1
        wg, wu, wd = wgb[u % 2], wub[u % 2], wdb[u % 2]
        S.dma("pool", wg[:, :, 0:nf * 128], wg_d[:, c0 * 128:c1 * 128].rearrange("(kc p) n -> p kc n", p=128))
        S.dma("pool", wu[:, :, 0:nf * 128], wu_d[:, c0 * 128:c1 * 128].rearrange("(kc p) n -> p kc n", p=128))
        S.dma("pool", wd[:, 0:nf, :], wd_d[c0 * 128:c1 * 128, :].rearrange("(fc p) n -> p fc n", p=128))
        if gi == 0 and l == 0:
            self.tap("wgtap", wg[:], [128, KC, 512])
        def gu(bi, t0, t1):
            n = t1 - t0
            gv = gate_fn(bi, (t0, t1)) if gate_fn is not None else None
            ab = actb[self.ause % 2]
            self.ause += 1
            for fc in range(nf):
                pg = self.pb[self.puse % 2]
                pu = self.pb[2 + self.puse % 2]
                self.puse += 1
                for kc in range(KC):
                    S.mm(pg[:, :n], wg[:, kc, fc * 128:(fc + 1) * 128], xn[:, kc, t0:t1], start=(kc == 0), stop=(kc == KC - 1))
                for kc in range(KC):
                    S.mm(pu[:, :n], wu[:, kc, fc * 128:(fc + 1) * 128], xn[:, kc, t0:t1], start=(kc == 0), stop=(kc == KC - 1))
                sg = sgb[fc % 2]
                S.act(sg[:, :n], pg[:, :n], AF.Silu)
                if gv is not None:
                    S.tt("dve", sg[:, :n], sg[:, :n], gv, ALU.mult)
                S.tt("dve", ab[:, fc, :n], sg[:, :n], pu[:, :n], ALU.mult)
            return ab

        def down(ab, t0, t1):
            n = t1 - t0
            w = 1 if t0 < NCTX else 0
            for nc_ in range(KC):
                py = self.pb[4 + self.yuse % 4]
                self.yuse += 1
                for fc in range(nf):
                    S.mm(py[:, :n], wd[:, fc, nc_ * 128:(nc_ + 1) * 128], ab[:, fc, :n], start=(fc == 0), stop=(fc == nf - 1))
                S.stt("dve", hT[:, nc_, t0:t1], py[:, :n], md[:, 40 + nc_, w:w + 1], hT[:, nc_, t0:t1], ALU.mult, ALU.add)

        prev = None
        for bi, (t0, t1) in enumerate(BLKS[5 - nblk:]):
            ab = gu(bi, t0, t1)
            if prev is not None:
                down(*prev)
            prev = (ab, t0, t1)
        down(*prev)


def alloc_wbufs(self, es2):
    S = self.S
    wgb = [S.sb(es2, "wgb%d" % i, [128, KC, 512], BF16) for i in range(2)]
    wub = [S.sb(es2, "wub%d" % i, [128, KC, 512], BF16) for i in range(2)]
    wdb = [S.sb(es2, "wdb%d" % i, [128, 4, D], BF16) for i in range(2)]
    actb = [S.sb(es2, "actb%d" % i, [128, 4, 512], BF16) for i in range(2)]
    sgb = [S.sb(es2, "sgb%d" % i, [128, 512], F32) for i in range(2)]
    self.wuse = self.ause = self.puse = self.yuse = 0
    return wgb, wub, wdb, actb, sgb


def ffn0(self, XM):
    S = self.S
    modulate(self, 0, 1, XM)
    self.tap("xn1", XM[:], [128, KC, TT])
    with S.scope() as es2:
        wb = alloc_wbufs(self, es2)
        swiglu(self, 0, XM, self.din("e_ffn_w_gate", [1, D, DFF])[0], self.din("e_ffn_w_up", [1, D, DFF])[0],
               self.din("e_ffn_w_down", [1, DFF, D])[0], wbufs=wb)
        S.barrier()


O_CQ, O_CFF, O_CFB, O_CI, O_CG, O_DQ, O_DK, O_DV, O_DG, O_DLR = 0, 4, 8, 12, 16, 20, 22, 24, 28, 32


def inproj1(self, xn):
    S = self.S
    U1 = self.U1 = S.dram("U1", [33, 128, TT], F32)
    w_d = self.din("o_w_in", [1, D, O_IN])[0]
    with S.scope() as es2:
        buf = [S.sb(es2, "u1b%d" % i, [128, TT], F32) for i in range(3)]

        def consumer(ci, m, blk, pv):
            t0, t1 = blk
            p = buf[ci % 3]
            S.cp("act" if (t0 // 512) % 2 == 0 else "dve", p[0:m, t0:t1], pv)
            if t1 == TT:
                S.dma("sp", U1("c%d" % ci)[ci, 0:m, :], p[0:m, :])

        proj(self, xn, w_d, O_IN, consumer)


def gla_unit(self, XM, kind, idx, masks):
    S = self.S
    U1 = self.U1
    cst = self.cst
    NT = TT // 128
    nh = 1 if kind == "c" else 2
    mkf, mkb = masks
    with S.scope() as es2:
        vtm = S.sb(es2, "vtm", [128, NT, nh, 128], BF16)
        oacc = S.sb(es2, "oacc1", [128, nh, NLAT], F32)
        lbc = S.sb(es2, "lbc", [128, 4], F32)
        if kind == "c":
            S.tt("dve", lbc[:, 0:1], self.col("lb_logits", 4 + idx, 1), self.col("lb_logits", idx, 1), ALU.subtract)
            S.act(lbc[:, 0:1], lbc[:, 0:1], AF.Sigmoid)
            S.ts("dve", lbc[:, 1:2], lbc[:, 0:1], -1.0, 1.0, op0=ALU.mult, op1=ALU.add)
        else:
            for d in range(2):
                S.ts("dve", lbc[:, 2 + d:3 + d], self.col("d_gate_b2", d * 2 + idx, 1), -1.0, None, op0=ALU.mult)
        with S.scope() as es3:
            A = []
            for d in range(2):
                A.append(dict(
                    qdec=S.sb(es3, "qdec1", [128, TT], BF16), kinv=S.sb(es3, "kinv1", [128, TT], BF16),
                    ktm=S.sb(es3, "ktm", [128, NT, 128], BF16), dtot=S.sb(es3, "dtot", [128, TT // 64], F32)))
            with S.scope() as es4:
                T_ = []
                for d in range(2):
                    T_.append(dict(qb=S.sb(es4, "qb", [128, 512], F32), kb=S.sb(es4, "kb", [128, 512], F32),
                                   ld=S.sb(es4, "ld", [128, 512], F32), PF=S.sb(es4, "PF1", [128, 512], F32),
                                   SF=S.sb(es4, "SF1", [128, 512], F32), tmp=S.sb(es4, "tmp1", [128, 512], F32)))
                vb_ = [S.sb(es4, "vb1_0", [128, 512], F32)] * nh
                if kind == "d":
                    w2b = S.sb(es4, "w2b", [128, 2, 128], BF16)
                    dlrb = S.sb(es4, "dlrb", [128, TT], BF16)
                    with S.scope() as es5:
                        w2f = S.sb(es5, "w2f", [128, 2, 128], F32)
                        dlr = S.sb(es5, "dlr", [128, 384], F32)
                        S.memset("pool", w2f[:], 0.0)
                        w2_d = self.din("o_d_gate_w2", [1, 2, 16, 256])[0]
                        for d in range(2):
                            S.dma("sp", w2f[16 * d:16 * d + 16, d, :], w2_d[d, :, idx * 128:(idx + 1) * 128])
                        S.cp("dve", w2b[:], w2f[:])
                        S.memset("pool", dlr[:], 0.0)
                        for hf in range(6):
                            S.dma("sp", dlr[0:32, :], U1("c32")[32, 0:32, hf * 384:(hf + 1) * 384])
                            S.cp("dve", dlrb[:, hf * 384:(hf + 1) * 384], dlr[:])

                def prep_dir(d):
                    a_ = A[d]
                    t_ = T_[d]
                    qdec, kinv, ktm, dtot = a_["qdec"], a_["kinv"], a_["ktm"], a_["dtot"]
                    q_, k_, ld, PF, SF, tmp = t_["qb"], t_["kb"], t_["ld"], t_["PF"], t_["SF"], t_["tmp"]
                    pl = self.pb[d]
                    ptr = self.pb[2 + d]
                    for bi, (t0, t1) in enumerate(BLKS):
                        n = t1 - t0
                        if kind == "c":
                            S.dma("sp", q_[:, :n], U1("c%d" % (O_CQ + idx))[O_CQ + idx, :, t0:t1])
                            S.dma("sp", k_[:, :n], U1("c%d" % ((O_CFF if d == 0 else O_CFB) + idx))[(O_CFF if d == 0 else O_CFB) + idx, :, t0:t1])
                            yield
                            S.act(q_[:, :n], q_[:, :n], AF.Silu)
                            S.act(k_[:, :n], k_[:, :n], AF.Sigmoid)
                            yield
                            S.ts("dve", q_[:, :n], q_[:, :n], 128.0 ** -0.5, None, op0=ALU.mult)
                            S.ts("dve", k_[:, :n], k_[:, :n], lbc[:, 1:2], lbc[:, 0:1], op0=ALU.mult, op1=ALU.add)
                            yield
                            S.act(ld[:, :n], k_[:, :n], AF.Ln)
                            yield
                            S.ts("dve", k_[:, :n], k_[:, :n], -1.0, 1.0, op0=ALU.mult, op1=ALU.add)
                        else:
                            S.dma("sp", q_[:, :n], U1("c%d" % (O_DQ + idx))[O_DQ + idx, :, t0:t1])
                            S.dma("sp", k_[:, :n], U1("c%d" % (O_DK + idx))[O_DK + idx, :, t0:t1])
                            S.mm(pl[:, :n], w2b[:, d, :], dlrb[:, t0:t1])
                            yield
                            S.act(ld[:, :n], pl[:, :n], AF.Exp, bias=lbc[:, 2 + d:3 + d], scale=-1.0)
                            S.act(ld[:, :n], ld[:, :n], AF.Ln, bias=1.0)
                            yield
                            S.ts("dve", q_[:, :n], q_[:, :n], 64.0 ** -0.5, None, op0=ALU.mult)
                            S.ts("dve", ld[:, :n], ld[:, :n], -1.0 / 16.0, None, op0=ALU.mult)
                        S.scan(PF[:, :n], mkf[:, :n], ld[:, :n], 0.0)
                        S.scan(rev(SF, 0, n), rev(mkb, 0, n), rev(ld, 0, n), 0.0)
                        bc, oth = (PF, SF) if d == 0 else (SF, PF)
                        yield
                        S.act(dtot[:, t0 // 64:t1 // 64], V(PF.h[:, 63:n:64], (PF.name, None)), AF.Exp)
                        S.act(tmp[:, :n], bc[:, :n], AF.Exp)
                        yield
                        S.tt("dve", oth[:, :n], oth[:, :n], ld[:, :n], ALU.subtract)
                        S.tt("dve", qdec[:, t0:t1], q_[:, :n], tmp[:, :n], ALU.mult)
                        yield
                        S.act(tmp[:, :n], bc[:, :n], AF.Exp, scale=-1.0)
                        S.act(oth[:, :n], oth[:, :n], AF.Exp)
                        yield
                        S.tt("dve", kinv[:, t0:t1], k_[:, :n], tmp[:, :n], ALU.mult)
                        S.tt("dve", tmp[:, :n], k_[:, :n], oth[:, :n], ALU.mult)
                        yield
                        for cc in range(n // 128):
                            S.tr(ptr[:, cc * 128:(cc + 1) * 128], tmp[:, cc * 128:(cc + 1) * 128], self.ident)
                        yield
                        S.cp("act", ktm[:, t0 // 128:t1 // 128, :], V(ptr.h[:, 0:n].rearrange("p (c t) -> p c t", t=128), (ptr.name, None)))
                        yield
                        if d == 0:
                            for hh in range(nh):
                                v_ = vb_[hh]
                                vch = (O_CI + idx) if kind == "c" else (O_DV + idx * 2 + hh)
                                S.dma("sp", v_[:, :n], U1("c%d" % vch)[vch, :, t0:t1])
                                for cc in range(n // 128):
                                    S.tr(ptr[:, cc * 128:(cc + 1) * 128], v_[:, cc * 128:(cc + 1) * 128], self.ident)
                                yield
                                S.cp("act", vtm[:, t0 // 128:t1 // 128, hh, :], V(ptr.h[:, 0:n].rearrange("p (c t) -> p c t", t=128), (ptr.name, None)))
                                yield

                interleave(prep_dir(0), prep_dir(1))
            with S.scope() as es4:
                NP = TT // 64
                Uall = S.sb(es4, "Uall", [128, 128, NP], F32)
                Sball = S.sb(es4, "Sball", [128, NP, 128], BF16)
                Dq = S.sb(es4, "Dq", [128, 32, NP], F32)
                dperm = S.sb(es4, "dperm", [128, NP], F32)
                ATb = [S.sb(es4, "ATb%d" % i, [128, 128], BF16) for i in range(2)]
                for d in range(2):
                    a_ = A[d]
                    qdec, kinv, ktm, dtot = a_["qdec"], a_["kinv"], a_["ktm"], a_["dtot"]
                    mask = cst[:, 6, :] if d == 0 else cst[:, 7, :]
                    order_tiles = chunk_order(NCTX // 128, NT, d)
                    pos = {}
                    for si, ti in enumerate(order_tiles):
                        for q in range(2):
                            ci = q if d == 0 else 1 - q
                            pos[(ti, ci)] = 2 * si + q
                    if d == 0:
                        S.cp("pool", dperm[:], dtot[:])
                    else:
                        nc4 = NCTX // 64
                        S.cp("pool", dperm[:, 0:nc4], V(dtot.h[:, 0:nc4][:, ::-1], (dtot.name, None)))
                        S.cp("pool", dperm[:, nc4:NP], V(dtot.h[:, nc4:NP][:, ::-1], (dtot.name, None)))
                    S.cp("dve", Dq[:], V(dperm.h[:, :].unsqueeze(1).to_broadcast([128, 32, NP]), (dperm.name, None)))
                    S.memset("dve", Dq[:, :, 0:1], 0.0)
                    for hh in range(nh):
                        r0, r1 = (0, 128) if kind == "c" else (64 * hh, 64 * hh + 64)
                        for gi, g in enumerate(range(0, NT, 4)):
                            tiles = order_tiles[g:g + 4]
                            nt = len(tiles)
                            banks = [self.pb[(gi % 2) * 2], self.pb[(gi % 2) * 2 + 1]]
                            for j, ti in enumerate(tiles):
                                for q in range(2):
                                    ci = q if d == 0 else 1 - q
                                    S.mm(banks[ci][:, j * 128:(j + 1) * 128], ktm[ci * 64:ci * 64 + 64, ti, :], vtm[ci * 64:ci * 64 + 64, ti, hh, :])
                            for ci in range(2):
                                q = ci if d == 0 else 1 - ci
                                p0 = 2 * g + q
                                ov = V(Uall.h[:, :, p0:min(p0 + 2 * nt, NP):2], (Uall.name, None))
                                iv = V(banks[ci].h[:, 0:nt * 128].rearrange("p (c v) -> p v c", c=nt), (banks[ci].name, None))
                                S.cp("act" if ci == 0 else "dve", ov, iv)
                        for vq in range(4):
                            u2 = V(Uall.h[:, vq * 32:(vq + 1) * 32, :].rearrange("p v c -> p (v c)"), (Uall.name, None))
                            d2 = V(Dq.h[:, :, :].rearrange("p v c -> p (v c)"), (Dq.name, None))
                            S.scan(u2, d2, u2, 0.0)
                        for half in range(2):
                            ov = Sball[:, half * 18:(half + 1) * 18, :]
                            iv = V(Uall.h[:, :, half * 18:(half + 1) * 18].rearrange("p v c -> p c v"), (Uall.name, None))
                            S.cp("act" if half == 0 else "dve", ov, iv)
                        for si, ti in enumerate(order_tiles):
                            if ti < NCTX // 128:
                                continue
                            ta = ti * 128
                            la = ta - NCTX
                            pA = self.pb[4 + 3 * (si % 2)]
                            pO = self.pb[5 + si % 2]
                            AT = ATb[si % 2]
                            S.mm(pA[:, 0:128], kinv[r0:r1, ta:ta + 128], qdec[r0:r1, ta:ta + 128])
                            S.tt("dve", AT[:], pA[:, 0:128], mask, ALU.mult)
                            S.mm(pO[:, 0:128], vtm[:, ti, hh, :], AT[:], start=True, stop=False)
                            for q in range(2):
                                ci = q if d == 0 else 1 - q
                                p = pos[(ti, ci)]
                                ca = ta + ci * 64
                                S.mm(pO[:, ci * 64:ci * 64 + 64], Sball[r0:r1, p - 1, :], qdec[r0:r1, ca:ca + 64], start=False, stop=(q == 1))
                            if d == 0:
                                S.cp("act", oacc[:, hh, la:la + 128], pO[:, 0:128])
                            else:
                                S.tt("dve", oacc[:, hh, la:la + 128], oacc[:, hh, la:la + 128], pO[:, 0:128], ALU.add)
                self.ck("g_rec")
        with S.scope() as es3:
            zb = [S.sb(es3, "zb1%d" % i, [128, 512], F32) for i in range(2)]
            sqb = S.sb(es3, "gsq1", [128, 512], BF16)
            rs = S.sb(es3, "grs1", [128, 512], F32)
            for hh in range(nh):
                if kind == "c":
                    gch, xch, gname = O_CG + idx, idx, "c_norm_g"
                else:
                    gch, xch, gname = O_DG + idx * 2 + hh, 4 + idx * 2 + hh, "d_norm_g"
                if hh == 0:
                    self.tap("oc0" if kind == "c" else "od0", oacc[:, 0, :], [128, NLAT])
                for bi, (t0, t1) in enumerate(BLKS[1:]):
                    n = t1 - t0
                    la = t0 - NCTX
                    z_ = zb[bi % 2]
                    S.dma("sp", z_[:, :n], U1("c%d" % gch)[gch, :, t0:t1])
                    S.act(sqb[:, :n], oacc[:, hh, la:la + n], AF.Square)
                    p0 = self.pb[0]
                    S.mm(p0[:, :n], self.onesb, sqb[:, :n])
                    S.ts("dve", rs[:, :n], p0[:, :n], 1.0 / 128.0, EPS, op0=ALU.mult, op1=ALU.add)
                    S.act(rs[:, :n], rs[:, :n], AF.Sqrt)
                    S.recip(rs[:, :n], rs[:, :n])
                    S.act(z_[:, :n], z_[:, :n], AF.Silu)
                    S.stt("dve", rs[:, :n], oacc[:, hh, la:la + n], self.col(gname, 0, 1), rs[:, :n], ALU.mult, ALU.mult)
                    S.tt("dve", XM[:, xch, t0:t1], rs[:, :n], z_[:, :n], ALU.mult)


def mixer1(self, XM):
    S = self.S
    with S.scope() as es2:
        mkf = S.sb(es2, "mkf", [128, 512], F32)
        mkb = S.sb(es2, "mkb", [128, 512], F32)
        S.memset("pool", mkf[:], 1.0)
        S.memset("pool", V(mkf.h[:, 0:512:64], (mkf.name, None)), 0.0)
        S.memset("pool", mkb[:], 1.0)
        S.memset("pool", V(mkb.h[:, 63:512:64], (mkb.name, None)), 0.0)
        for h in range(4):
            gla_unit(self, XM, "c", h, (mkf, mkb))
        for hp in range(2):
            gla_unit(self, XM, "d", hp, (mkf, mkb))


AX = mybir.AxisListType


def moe(self, XM):
    S = self.S
    hT = self.hT
    md = self.mod[1]
    NTL = NLAT // 128
    with S.scope() as es2:
        gtm = S.sb(es2, "gtm", [128, NTL, NEXP], F32)
        rw = S.sb(es2, "rw", [128, KC, NEXP], F32)
        rb = S.sb(es2, "rb", [128, NEXP], F32)
        S.dma("sp", rw[:], self.din("o_router_w", [1, D, NEXP])[0].rearrange("(kc p) e -> p kc e", p=128))
        S.dma("sp", rb[:], self.din("rb128", [128, NEXP])[:, :])
        g0 = 3
        with S.scope() as es3:
            gs = S.sb(es3, "gs2", [128, KC], F32)
            sq = [S.sb(es3, "sq2%d" % i, [128, 512], BF16) for i in range(2)]
            rsb = S.sb(es3, "rsb2", [128, 512], F32)
            tmp = [S.sb(es3, "mtmp2%d" % i, [128, 512], F32) for i in range(2)]
            xr = S.sb(es3, "xr", [128, KC, 512], F32)
            sm = S.sb(es3, "sm", [128, 8, NEXP], F32)
            sc = S.sb(es3, "smc", [128, 8], F32)
            S.ts("dve", gs[:], md[:, (g0 + 1) * 8:(g0 + 2) * 8, 0], 1.0, 32.0, op0=ALU.add, op1=ALU.mult)
            S.tt("dve", gs[:], gs[:], self.col("nffn1", 0, 8), ALU.mult)
            for (t0, t1) in BLKS[1:]:
                n = t1 - t0
                ss = self.pb[0]
                for kc in range(KC):
                    s = sq[kc % 2]
                    S.act(s[:, :n], hT[:, kc, t0:t1], AF.Square)
                    S.mm(ss[:, :n], self.onesb, s[:, :n], start=(kc == 0), stop=(kc == KC - 1))
                S.ts("dve", rsb[:, :n], ss[:, :n], 1024.0 * EPS, None, op0=ALU.add)
                S.act(rsb[:, :n], rsb[:, :n], AF.Sqrt)
                S.recip(rsb[:, :n], rsb[:, :n])
                for kc in range(KC):
                    tm = tmp[kc % 2]
                    S.tt("dve", tm[:, :n], hT[:, kc, t0:t1], rsb[:, :n], ALU.mult)
                    S.act(XM[:, kc, t0:t1], tm[:, :n], AF.Identity, bias=md[:, g0 * 8 + kc, 0:1], scale=gs[:, kc:kc + 1])
                    S.ts("dve", xr[:, kc, :n], tm[:, :n], gs[:, kc:kc + 1], md[:, g0 * 8 + kc, 0:1], op0=ALU.mult, op1=ALU.add)
                for q in range(4):
                    ti = (t0 - NCTX) // 128 + q
                    pl = self.pb[1]
                    for kc in range(KC):
                        S.mm(pl[:, 0:NEXP], xr[:, kc, q * 128:(q + 1) * 128], rw[:, kc, :], start=(kc == 0), stop=(kc == KC - 1))
                    lg, eq, l2, e_ = sm[:, 0, :], sm[:, 1, :], sm[:, 2, :], sm[:, 3, :]
                    m1, m2, nm1, dd = sc[:, 0:1], sc[:, 1:2], sc[:, 2:3], sc[:, 3:4]
                    S.tt("dve", lg, pl[:, 0:NEXP], rb[:], ALU.add)
                    S.op("dve", lambda: self.nc.vector.reduce_max(out=_ap(m1), in_=_ap(lg), axis=AX.X), [lg], [m1])
                    S.ts("dve", eq, lg, m1, None, op0=ALU.is_equal)
                    S.stt("dve", l2, eq, -1e30, lg, ALU.mult, ALU.add)
                    S.op("dve", lambda: self.nc.vector.reduce_max(out=_ap(m2), in_=_ap(l2), axis=AX.X), [l2], [m2])
                    S.ts("dve", eq, lg, m2, None, op0=ALU.is_ge)
                    S.ts("dve", nm1, m1, -1.0, None, op0=ALU.mult)
                    S.act(e_, lg, AF.Exp, bias=nm1)
                    S.tt("dve", dd, m2, m1, ALU.subtract)
                    S.act(dd, dd, AF.Exp)
                    S.ts("dve", dd, dd, 1.0, None, op0=ALU.add)
                    S.recip(dd, dd)
                    S.tt("dve", e_, e_, eq, ALU.mult)
                    S.ts("dve", gtm[:, ti, :], e_, dd, None, op0=ALU.mult)
        self.tap("gates", gtm[:], [128, NTL, NEXP])
        with S.scope() as es3:
            wb = alloc_wbufs(self, es3)
            Gb = S.sb(es3, "Gb", [128, NLAT], F32)
            dg = [S.sb(es3, "dg%d" % i, [128, 128], F32) for i in range(2)]
            wg_d = self.din("o_moe_w_gate", [1, NEXP, D, DFF])[0]
            wu_d = self.din("o_moe_w_up", [1, NEXP, D, DFF])[0]
            wd_d = self.din("o_moe_w_down", [1, NEXP, DFF, D])[0]
            for e in range(NEXP):
                if ("moe_e%d" % e) in self.stages:
                    continue
                for b4 in range(4):
                    pG = self.pb[6 + b4 % 2]
                    for q in range(4):
                        ti = b4 * 4 + q
                        dq_ = dg[q % 2]
                        S.ts("dve", dq_[:], self.cst[:, 0, :], gtm[:, ti, e:e + 1], None, op0=ALU.mult)
                        S.mm(pG[:, q * 128:(q + 1) * 128], self.cst[:, 1, :], dq_[:])
                    S.cp("act", Gb[:, b4 * 512:(b4 + 1) * 512], pG[:, :])
                swiglu(self, 1, XM, wg_d[e], wu_d[e], wd_d[e], nblk=4, wbufs=wb,
                       gate_fn=lambda bi, blk: Gb[:, blk[0] - NCTX:blk[1] - NCTX])


def final(self):
    S = self.S
    hT = self.hT
    out_d = self.nc.dram_tensor("out", [NLAT, D], F32, kind="ExternalOutput").ap()
    with S.scope() as es2:
        g32 = S.sb(es2, "g32", [128, KC], F32)
        sq = [S.sb(es2, "sqf%d" % i, [128, 512], BF16) for i in range(2)]
        rsb = S.sb(es2, "rsbf", [128, 512], F32)
        yT = [S.sb(es2, "yT%d" % i, [128, 512], F32) for i in range(2)]
        ot = [S.sb(es2, "ot%d" % i, [128, D], F32) for i in range(4)]
        S.ts("dve", g32[:], self.col("nfin", 0, 8), 32.0, None, op0=ALU.mult)
        for bi, (t0, t1) in enumerate(BLKS[1:]):
            n = t1 - t0
            ss = self.pb[0]
            for kc in range(KC):
                s = sq[kc % 2]
                S.act(s[:, :n], hT[:, kc, t0:t1], AF.Square)
                S.mm(ss[:, :n], self.onesb, s[:, :n], start=(kc == 0), stop=(kc == KC - 1))
            S.ts("dve", rsb[:, :n], ss[:, :n], 1024.0 * EPS, None, op0=ALU.add)
            S.act(rsb[:, :n], rsb[:, :n], AF.Sqrt)
            S.recip(rsb[:, :n], rsb[:, :n])
            for kc in range(KC):
                y = yT[kc % 2]
                S.stt("dve", y[:, :n], hT[:, kc, t0:t1], g32[:, kc:kc + 1], rsb[:, :n], ALU.mult, ALU.mult)
                pt = self.pb[2 + kc % 4]
                for q in range(4):
                    S.tr(pt[:, q * 128:(q + 1) * 128], y[:, q * 128:(q + 1) * 128], self.ident)
                for q in range(4):
                    S.cp("act" if q % 2 == 0 else "dve", ot[q][:, kc * 128:(kc + 1) * 128], pt[:, q * 128:(q + 1) * 128])
            for q in range(4):
                r0 = t0 - NCTX + q * 128
                tok = S.dma("sp", out_d[r0:r0 + 128, :], ot[q][:])
                S.out_tokens.append(tok)


Prog.final = final


ALL_STAGES = ["L0", "inproj0", "rglru", "deltanet", "outproj", "ffn0", "L1", "FIN"]
_CACHE = {}


def kernel(**inputs):
    inp = {k: np.asarray(v) for k, v in inputs.items()}
    if "prog" not in _CACHE:
        P = Prog(ALL_STAGES)
        P.build()
        _CACHE["prog"] = P
    P = _CACHE["prog"]
    B = inp["x"].shape[0]
    maps = [in_map(inp, b, P.inputs) for b in range(B)]
    res = run_bass_kernel_spmd(P.nc, maps, core_ids=list(range(B)))
    return np.stack([np.asarray(res.results[b]["out"]) for b in range(B)], 0).astype(np.float32)
```
